# Optimizing a Trainium2 kernel written in Bass

```python
import math
import jax, jax.numpy as jnp
from jax import lax
import numpy as np

D_MODEL = 4096
BATCH = 4
SEQ = 4096
DEPTH = 1

D_MIX = D_MODEL
MLA_HEADS = 16
QK_NOPE_DIM = 128
QK_ROPE_DIM = 64
QK_HEAD_DIM = QK_NOPE_DIM + QK_ROPE_DIM
V_HEAD_DIM = 128
D_ATTN = MLA_HEADS * V_HEAD_DIM
Q_LORA_RANK = 1024
KV_LORA_RANK = 512
ROPE_THETA = 10000.0
Q_BLOCK = 128
D_SSM = D_MIX - D_ATTN
SSM_GROUP = 16
SSM_GROUPS = D_SSM // SSM_GROUP
SSM_STATE = 64
DT_MIN = 0.001
DT_MAX = 0.1
D_IN_PROJ = Q_LORA_RANK + KV_LORA_RANK + QK_ROPE_DIM + D_SSM
N_EXPERTS = 32
TOP_K = 4
D_EXPERT = D_MODEL // 2
SWIGLU_ALPHA = 1.702
SWIGLU_LIMIT = 7.0
EXPERT_BLOCK = 256
EPS = 1e-6

kernel_name = "hymba_mla_s5_moe_encoder_layer"


def rms_norm(x, g):
    xf = x.astype(jnp.float32)
    y = xf * lax.rsqrt(jnp.mean(xf * xf, axis=-1, keepdims=True) + EPS)
    return (y * g.astype(jnp.float32)).astype(x.dtype)


def rope_tables(positions, dtype):
    half = QK_ROPE_DIM // 2
    inv_freq = ROPE_THETA ** (-jnp.arange(half, dtype=jnp.float32) / half)
    ang = positions.astype(jnp.float32)[..., None] * inv_freq
    return jnp.cos(ang)[:, :, None, :].astype(dtype), jnp.sin(ang)[:, :, None, :].astype(dtype)


def apply_rope(x, cos, sin):
    x1, x2 = jnp.split(x, 2, axis=-1)
    return jnp.concatenate([x1 * cos - x2 * sin, x2 * cos + x1 * sin], axis=-1)


def block_attention(q, k, v):
    B, S, H, dq = q.shape
    nq = S // Q_BLOCK
    scale = QK_HEAD_DIM ** -0.5
    qb = jnp.swapaxes(q.reshape(B, nq, Q_BLOCK, H, dq), 0, 1)

    def one_block(q_blk):
        s = jnp.einsum('bqhd,bkhd->bhqk', q_blk, k).astype(jnp.float32) * scale
        p = jax.nn.softmax(s, axis=-1).astype(v.dtype)
        return jnp.einsum('bhqk,bkhd->bqhd', p, v)

    o = lax.map(one_block, qb)
    return jnp.swapaxes(o, 0, 1).reshape(B, S, H, V_HEAD_DIM)


def mla_mixer(c_q, c_kv, k_pe, cos, sin, q_lora_norm_g, w_q_up, kv_lora_norm_g, w_kv_up,
              q_head_norm_g, k_head_norm_g):
    B, S, _ = c_q.shape
    q = (rms_norm(c_q, q_lora_norm_g) @ w_q_up).reshape(B, S, MLA_HEADS, QK_HEAD_DIM)
    kv = (rms_norm(c_kv, kv_lora_norm_g) @ w_kv_up).reshape(B, S, MLA_HEADS, QK_NOPE_DIM + V_HEAD_DIM)
    k_nope, v = kv[..., :QK_NOPE_DIM], kv[..., QK_NOPE_DIM:]
    k_rot = jnp.broadcast_to(k_pe[:, :, None, :], (B, S, MLA_HEADS, QK_ROPE_DIM))
    k = jnp.concatenate([k_nope, k_rot], axis=-1)
    q = rms_norm(q, q_head_norm_g)
    k = rms_norm(k, k_head_norm_g)
    q = jnp.concatenate([q[..., :QK_NOPE_DIM], apply_rope(q[..., QK_NOPE_DIM:], cos, sin)], axis=-1)
    k = jnp.concatenate([k[..., :QK_NOPE_DIM], apply_rope(k[..., QK_NOPE_DIM:], cos, sin)], axis=-1)
    o = block_attention(q, k, v)
    return o.reshape(B, S, D_ATTN)


def _complex_linear_combine(e1, e2):
    a1r, a1i, b1r, b1i = e1
    a2r, a2i, b2r, b2i = e2
    return (a1r * a2r - a1i * a2i,
            a1r * a2i + a1i * a2r,
            a2r * b1r - a2i * b1i + b2r,
            a2r * b1i + a2i * b1r + b2i)


def s5_scan(u, a_re, a_im, log_step, b_re, b_im, c_re, c_im, reverse):
    f32 = jnp.float32
    a_re, a_im = a_re.astype(f32), a_im.astype(f32)
    b_re, b_im = b_re.astype(f32), b_im.astype(f32)
    c_re, c_im = c_re.astype(f32), c_im.astype(f32)
    dt = jnp.exp(log_step.astype(f32))[:, None]
    mag = jnp.exp(a_re * dt)
    ang = a_im * dt
    ab_re, ab_im = mag * jnp.cos(ang), mag * jnp.sin(ang)
    den = a_re * a_re + a_im * a_im
    zr = ((ab_re - 1.0) * a_re + ab_im * a_im) / den
    zi = (ab_im * a_re - (ab_re - 1.0) * a_im) / den
    bb_re = zr[..., None] * b_re - zi[..., None] * b_im
    bb_im = zr[..., None] * b_im + zi[..., None] * b_re
    bu_re = jnp.einsum('sbgc,gpc->sbgp', u, bb_re)
    bu_im = jnp.einsum('sbgc,gpc->sbgp', u, bb_im)
    S = u.shape[0]
    lam_re = jnp.broadcast_to(ab_re, (S, 1) + ab_re.shape)
    lam_im = jnp.broadcast_to(ab_im, (S, 1) + ab_im.shape)
    _, _, x_re, x_im = lax.associative_scan(
        _complex_linear_combine, (lam_re, lam_im, bu_re, bu_im), reverse=reverse, axis=0)
    return jnp.einsum('sbgp,gcp->sbgc', x_re, c_re) - jnp.einsum('sbgp,gcp->sbgc', x_im, c_im)


def s5_mixer(u, a_re, a_im, log_step, b_re, b_im, c_re, c_im, d_skip, w_glu, b_glu):
    B, S, _ = u.shape
    uf = u.astype(jnp.float32)
    us = jnp.swapaxes(uf, 0, 1).reshape(S, B, SSM_GROUPS, SSM_GROUP)
    y = 0.0
    for d in range(2):
        y = y + s5_scan(us, a_re[d], a_im[d], log_step[d], b_re[d], b_im[d], c_re[d], c_im[d],
                        reverse=(d == 1))
    y = jnp.swapaxes(y.reshape(S, B, D_SSM), 0, 1) + d_skip.astype(jnp.float32) * uf
    g = jax.nn.gelu(y).astype(u.dtype)
    return g * jax.nn.sigmoid(g @ w_glu + b_glu)


def clamped_swiglu(a, lin):
    a = jnp.minimum(a, SWIGLU_LIMIT)
    lin = jnp.clip(lin, -SWIGLU_LIMIT, SWIGLU_LIMIT)
    return a * jax.nn.sigmoid(SWIGLU_ALPHA * a) * (lin + 1.0)


def moe_ffn(h, w_router, b_router, w_gate, b_gate, w_up, b_up, w_down, b_down):
    B, S, D = h.shape
    T = B * S
    TK = T * TOP_K
    hf = h.reshape(T, D)
    logits = (hf @ w_router + b_router).astype(jnp.float32)
    top_logit, top_idx = lax.top_k(logits, TOP_K)
    gates = jax.nn.softmax(top_logit, axis=-1)
    flat_e = top_idx.reshape(TK).astype(jnp.int32)
    flat_tok = jnp.repeat(jnp.arange(T, dtype=jnp.int32), TOP_K)
    flat_g = gates.reshape(TK)
    order = jnp.argsort(flat_e)
    sorted_e = flat_e[order]
    counts = jnp.bincount(flat_e, length=N_EXPERTS)
    starts = jnp.cumsum(counts) - counts
    padded = (counts + EXPERT_BLOCK - 1) // EXPERT_BLOCK * EXPERT_BLOCK
    pad_ends = jnp.cumsum(padded)
    pad_starts = pad_ends - padded
    dest = pad_starts[sorted_e] + jnp.arange(TK, dtype=jnp.int32) - starts[sorted_e]
    n_blocks = -(-TK // EXPERT_BLOCK) + N_EXPERTS
    n_slots = n_blocks * EXPERT_BLOCK
    slot_tok = jnp.zeros((n_slots,), jnp.int32).at[dest].set(flat_tok[order])
    slot_gate = jnp.zeros((n_slots,), jnp.float32).at[dest].set(flat_g[order])
    block_start = jnp.arange(n_blocks, dtype=jnp.int32) * EXPERT_BLOCK
    block_e = jnp.minimum(jnp.searchsorted(pad_ends, block_start, side='right'), N_EXPERTS - 1)

    def expert_block(acc, inp):
        tok, gate, e = inp
        xb = hf[tok]
        act = clamped_swiglu(xb @ w_gate[e] + b_gate[e], xb @ w_up[e] + b_up[e])
        yb = act @ w_down[e] + b_down[e]
        return acc.at[tok].add(yb * gate[:, None].astype(yb.dtype)), None

    acc, _ = lax.scan(expert_block, jnp.zeros_like(hf),
                      (slot_tok.reshape(n_blocks, EXPERT_BLOCK),
                       slot_gate.reshape(n_blocks, EXPERT_BLOCK), block_e))
    return acc.reshape(B, S, D)


def setup_inputs(seed: int = 0) -> dict:
    key = jax.random.key(seed)
    ks = list(jax.random.split(key, 40))

    def nrm(shape, scale):
        return jax.random.normal(ks.pop(), shape, jnp.float32) * scale

    def gain(shape):
        return 1.0 + nrm(shape, 0.02)

    L = DEPTH
    G, P, C = SSM_GROUPS, SSM_STATE, SSM_GROUP
    inp = {}
    inp["x"] = nrm((BATCH, SEQ, D_MODEL), 1.0)
    inp["positions"] = jnp.broadcast_to(jnp.arange(SEQ, dtype=jnp.int32), (BATCH, SEQ))
    inp["attn_norm_g"] = gain((L, D_MODEL))
    inp["w_in"] = nrm((L, D_MODEL, D_IN_PROJ), D_MODEL ** -0.5)
    inp["q_lora_norm_g"] = gain((L, Q_LORA_RANK))
    inp["w_q_up"] = nrm((L, Q_LORA_RANK, MLA_HEADS * QK_HEAD_DIM), Q_LORA_RANK ** -0.5)
    inp["kv_lora_norm_g"] = gain((L, KV_LORA_RANK))
    inp["w_kv_up"] = nrm((L, KV_LORA_RANK, MLA_HEADS * (QK_NOPE_DIM + V_HEAD_DIM)), KV_LORA_RANK ** -0.5)
    inp["q_head_norm_g"] = gain((L, QK_HEAD_DIM))
    inp["k_head_norm_g"] = gain((L, QK_HEAD_DIM))
    inp["ssm_a_re"] = -0.5 + nrm((L, 2, G, P), 0.01)
    inp["ssm_a_im"] = jnp.broadcast_to(math.pi * jnp.arange(P, dtype=jnp.float32), (L, 2, G, P))
    inp["ssm_log_step"] = jax.random.uniform(ks.pop(), (L, 2, G), jnp.float32,
                                             math.log(DT_MIN), math.log(DT_MAX))
    inp["ssm_b_re"] = nrm((L, 2, G, P, C), (2.0 * C) ** -0.5)
    inp["ssm_b_im"] = nrm((L, 2, G, P, C), (2.0 * C) ** -0.5)
    inp["ssm_c_re"] = nrm((L, 2, G, C, P), (2.0 * P) ** -0.5)
    inp["ssm_c_im"] = nrm((L, 2, G, C, P), (2.0 * P) ** -0.5)
    inp["ssm_d"] = nrm((L, D_SSM), 1.0)
    inp["ssm_w_glu"] = nrm((L, D_SSM, D_SSM), D_SSM ** -0.5)
    inp["ssm_b_glu"] = nrm((L, D_SSM), 0.01)
    inp["attn_out_norm_g"] = gain((L, D_ATTN))
    inp["ssm_out_norm_g"] = gain((L, D_SSM))
    inp["w_out"] = nrm((L, D_MIX, D_MODEL), D_MIX ** -0.5)
    inp["ffn_norm_g"] = gain((L, D_MODEL))
    inp["w_router"] = nrm((L, D_MODEL, N_EXPERTS), D_MODEL ** -0.5)
    inp["b_router"] = nrm((L, N_EXPERTS), 0.01)
    inp["w_gate"] = nrm((L, N_EXPERTS, D_MODEL, D_EXPERT), D_MODEL ** -0.5)
    inp["b_gate"] = nrm((L, N_EXPERTS, D_EXPERT), 0.01)
    inp["w_up"] = nrm((L, N_EXPERTS, D_MODEL, D_EXPERT), D_MODEL ** -0.5)
    inp["b_up"] = nrm((L, N_EXPERTS, D_EXPERT), 0.01)
    inp["w_down"] = nrm((L, N_EXPERTS, D_EXPERT, D_MODEL), D_EXPERT ** -0.5)
    inp["b_down"] = nrm((L, N_EXPERTS, D_MODEL), 0.01)
    return inp


def reference(x, positions, attn_norm_g, w_in, q_lora_norm_g, w_q_up, kv_lora_norm_g, w_kv_up,
              q_head_norm_g, k_head_norm_g, ssm_a_re, ssm_a_im, ssm_log_step, ssm_b_re, ssm_b_im,
              ssm_c_re, ssm_c_im, ssm_d, ssm_w_glu, ssm_b_glu, attn_out_norm_g, ssm_out_norm_g,
              w_out, ffn_norm_g, w_router, b_router, w_gate, b_gate, w_up, b_up, w_down, b_down):
    cos, sin = rope_tables(positions, x.dtype)
    split_at = [Q_LORA_RANK, Q_LORA_RANK + KV_LORA_RANK, Q_LORA_RANK + KV_LORA_RANK + QK_ROPE_DIM]
    h = x
    for l in range(DEPTH):
        z = rms_norm(h, attn_norm_g[l])
        proj = z @ w_in[l]
        c_q, c_kv, k_pe, u = jnp.split(proj, split_at, axis=-1)
        o_attn = mla_mixer(c_q, c_kv, k_pe, cos, sin, q_lora_norm_g[l], w_q_up[l],
                           kv_lora_norm_g[l], w_kv_up[l], q_head_norm_g[l], k_head_norm_g[l])
        o_ssm = s5_mixer(u, ssm_a_re[l], ssm_a_im[l], ssm_log_step[l], ssm_b_re[l], ssm_b_im[l],
                         ssm_c_re[l], ssm_c_im[l], ssm_d[l], ssm_w_glu[l], ssm_b_glu[l])
        mixed = jnp.concatenate([rms_norm(o_attn, attn_out_norm_g[l]),
                                 rms_norm(o_ssm, ssm_out_norm_g[l])], axis=-1)
        h = h + mixed @ w_out[l]
        h = h + moe_ffn(rms_norm(h, ffn_norm_g[l]), w_router[l], b_router[l], w_gate[l], b_gate[l],
                        w_up[l], b_up[l], w_down[l], b_down[l])
    return h
```

```python
import math
import os
from contextlib import ExitStack

import numpy as np
import concourse.bass as bass
import concourse.mybir as mybir
from concourse.bass_utils import run_bass_kernel_spmd

F32 = mybir.dt.float32
BF16 = mybir.dt.bfloat16
I32 = mybir.dt.int32
U32 = mybir.dt.uint32
ALU = mybir.AluOpType
AF = mybir.ActivationFunctionType
AX = mybir.AxisListType

N_CORES = 8

FULL_CFG = dict(D=4096, B=4, SEQ=4096, H=16, NOPE=128, ROPE=64, DV=128, QL=1024, KVL=512,
                G=128, P=64, C=16, E=32, DE=2048, TOPK=4, CAP=768, THETA=10000.0,
                ALPHA=1.702, LIMIT=7.0, EPS=1e-6)


class Buf:
    __slots__ = ("name", "w", "r")

    def __init__(self, name=""):
        self.name = name
        self.w = None
        self.r = []


class Sched:
    ENGS = ("pe", "act", "dve", "pool", "sp")
    NDMA = 48
    NPOOL = 8

    def __init__(self, nc, es):
        self.nc = nc
        self.q = {e: [] for e in self.ENGS}
        self.cnt = {e: 0 for e in self.ENGS}
        self.seen = {e: {} for e in self.ENGS}
        self.sem = {e: es.enter_context(nc.semaphore("s_" + e)) for e in ("pe", "act", "dve", "pool")}
        self.dsem = [es.enter_context(nc.semaphore("d%d" % i)) for i in range(self.NDMA)]
        self.dtot = [0] * self.NDMA
        self.drr = 0
        self.prr = 0
        self.nops = 0

    def _semobj(self, key):
        return self.sem[key] if isinstance(key, str) else self.dsem[key]

    def _waits(self, eng, deps):
        need = {}
        for d in deps:
            if d is None:
                continue
            k, v = d
            if eng == "pe" and k == "pe":
                continue
            if self.seen[eng].get(k, 0) >= v:
                continue
            if need.get(k, 0) < v:
                need[k] = v
        for k, v in need.items():
            self.seen[eng][k] = v
            so = self._semobj(k)
            self.q[eng].append(lambda e, so=so, v=v: e.wait_ge(so, v))

    @staticmethod
    def _deps(reads, writes):
        deps = []
        for b in reads:
            deps.append(b.w)
        for b in writes:
            deps.append(b.w)
            deps.extend(b.r)
        return deps

    @staticmethod
    def _mark(tok, reads, writes):
        for b in writes:
            b.w = tok
            b.r = []
        for b in reads:
            if b.w is not tok:
                b.r.append(tok)

    def op(self, eng, fn, reads=(), writes=()):
        self._waits(eng, self._deps(reads, writes))
        self.cnt[eng] += 1
        so = self.sem[eng]
        self.q[eng].append(lambda e, fn=fn, so=so: fn(e).then_inc(so, 1))
        self._mark((eng, self.cnt[eng]), reads, writes)
        self.nops += 1

    def dma(self, eng, fn, reads=(), writes=(), inc=16):
        if eng == "pool":
            k = self.prr
            self.prr = (self.prr + 1) % self.NPOOL
        else:
            k = self.NPOOL + self.drr
            self.drr = (self.drr + 1) % (self.NDMA - self.NPOOL)
        deps = self._deps(reads, writes)
        if self.dtot[k]:
            deps.append((k, self.dtot[k]))
        self._waits(eng, deps)
        self.dtot[k] += inc
        so = self.dsem[k]
        self.q[eng].append(lambda e, fn=fn, so=so, inc=inc: fn(e).then_inc(so, inc))
        self._mark((k, self.dtot[k]), reads, writes)
        self.nops += 1

    def flush(self):
        allk = [(k, self.dtot[k]) for k in range(self.NDMA) if self.dtot[k]]
        allk += [(e, self.cnt[e]) for e in ("pe", "act", "dve", "pool") if self.cnt[e]]
        for eng in self.ENGS:
            need = [d for d in allk if self.seen[eng].get(d[0], 0) < d[1]]
            for k, v in need:
                self.seen[eng][k] = v
                so = self._semobj(k)
                self.q[eng].append(lambda e, so=so, v=v: e.wait_ge(so, v))
        nc = self.nc
        q = self.q
        self.q = {e: [] for e in self.ENGS}
        with nc.Block() as block:
            @block.sync
            def _(e):
                for f in q["sp"]:
                    f(e)

            @block.tensor
            def _(e):
                for f in q["pe"]:
                    f(e)

            @block.scalar
            def _(e):
                for f in q["act"]:
                    f(e)

            @block.vector
            def _(e):
                for f in q["dve"]:
                    f(e)

            @block.gpsimd
            def _(e):
                for f in q["pool"]:
                    f(e)

    def finish(self, final_bufs):
        self.flush()


class Ctx:
    _n = [0]

    def __init__(self, nc, es, S):
        self.nc, self.es, self.S = nc, es, S

    def sb(self, shape, dt, name=None):
        Ctx._n[0] += 1
        t = self.es.enter_context(self.nc.sbuf_tensor("%s_%d" % (name or "t", Ctx._n[0]), list(shape), dt))
        return t, Buf(name or "t")

    def ps(self, shape, dt, name=None):
        Ctx._n[0] += 1
        t = self.es.enter_context(self.nc.psum_tensor("%s_%d" % (name or "p", Ctx._n[0]), list(shape), dt))
        return t, Buf(name or "p")

    def dram(self, shape, dt, name):
        t = self.nc.dram_tensor(name, list(shape), dt, kind="Internal")
        return t.ap(), Buf(name)


class Phase:
    def __init__(self, nc, S):
        self.nc = nc
        self.S = S
        self.es = ExitStack()

    def __enter__(self):
        self.es.__enter__()
        self.cx = Ctx(self.nc, self.es, self.S)
        return self

    def __exit__(self, *a):
        if a[0] is None:
            self.S.flush()
        return self.es.__exit__(*a)

    def identity(self, dt=BF16):
        S, cx = self.S, self.cx
        idf, idf_b = cx.sb([128, 128], F32, "idf")
        S.op("pool", lambda e: e.memset(idf[:], 0.0), writes=[idf_b])
        S.op("pool", lambda e: e.affine_select(out=idf[:], in_=idf[:], pattern=[[-1, 128]],
                                               compare_op=ALU.not_equal, fill=1.0, base=0,
                                               channel_multiplier=1), reads=[idf_b], writes=[idf_b])
        if dt == F32:
            return idf, idf_b
        idb, idb_b = cx.sb([128, 128], BF16, "idb")
        S.op("dve", lambda e: e.tensor_copy(out=idb[:], in_=idf[:]), reads=[idf_b], writes=[idb_b])
        return idb, idb_b

    def bcast_load(self, src_row_ap, n, name="g"):
        t, b = self.cx.sb([128, n], F32, name)
        self.S.dma("sp", lambda e: e.dma_start(out=t[:], in_=src_row_ap.to_broadcast([128, n])), writes=[b])
        return t, b

    def rstd(self, ss, ss_b, n, eps, np_=128):
        S = self.S
        S.op("dve", lambda e: e.tensor_scalar(out=ss, in0=ss, scalar1=1.0 / n, scalar2=eps,
                                              op0=ALU.mult, op1=ALU.add), reads=[ss_b], writes=[ss_b])
        S.op("act", lambda e: e.activation(out=ss, in_=ss, func=AF.Sqrt), reads=[ss_b], writes=[ss_b])
        S.op("dve", lambda e: e.reciprocal(out=ss, in_=ss), reads=[ss_b], writes=[ss_b])

    def rmsnorm(self, x, x_b, n, g, g_b, out, out_b, scr, scr_b, ss, ss_b, eps):
        S = self.S
        S.op("dve", lambda e: e.memset(ss, 0.0), writes=[ss_b])
        S.op("act", lambda e: e.activation(out=scr, in_=x, func=AF.Square, accum_out=ss),
             reads=[x_b, ss_b], writes=[scr_b, ss_b])
        self.rstd(ss, ss_b, n, eps)
        S.op("dve", lambda e: e.scalar_tensor_tensor(out=out, in0=x, scalar=ss, in1=g, op0=ALU.mult,
                                                     op1=ALU.mult), reads=[x_b, ss_b, g_b], writes=[out_b])

    def transpose_chunks(self, src, src_b, nch, dst, dst_b, ident, ident_b, pts, rows=128, evac="act"):
        S = self.S
        for c0 in range(0, nch, 8):
            n = min(8, nch - c0)
            pt, pt_b = pts[self._ptr % len(pts)]
            self._ptr += 1
            for c in range(n):
                S.op("pe", lambda e, pt=pt, c=c, c0=c0: e.transpose(out=pt[:, c, 0:rows],
                                                                  in_=src[:, (c0 + c) * 128:(c0 + c + 1) * 128],
                                                                  identity=ident[0:rows, 0:rows]),
                     reads=[src_b, ident_b], writes=[pt_b])
            if evac == "act":
                S.op("act", lambda e, pt=pt, c0=c0, n=n: e.copy(out=dst[:, c0:c0 + n, :], in_=pt[:, 0:n, 0:rows]),
                     reads=[pt_b], writes=[dst_b])
            else:
                S.op("dve", lambda e, pt=pt, c0=c0, n=n: e.tensor_copy(out=dst[:, c0:c0 + n, :], in_=pt[:, 0:n, 0:rows]),
                     reads=[pt_b], writes=[dst_b])
    _ptr = 0

    def sincos(self, ang, ang_b, shape, sin_out, cos_out, out_b):
        S, cx = self.S, self.cx
        ki, ki_b = cx.sb(shape, I32, "ki")
        kf, kf_b = cx.sb(shape, F32, "kf")
        r, r_b = cx.sb(shape, F32, "r")
        for which, outt in ((0, sin_out), (1, cos_out)):
            sh = 0.0 if which == 0 else 0.25
            S.op("dve", lambda e, sh=sh: e.tensor_scalar(out=r[:], in0=ang, scalar1=1.0 / (2 * math.pi), scalar2=sh,
                                                        op0=ALU.mult, op1=ALU.add), reads=[ang_b], writes=[r_b])
            S.op("dve", lambda e: e.tensor_copy(out=ki[:], in_=r[:]), reads=[r_b], writes=[ki_b])
            S.op("dve", lambda e: e.tensor_copy(out=kf[:], in_=ki[:]), reads=[ki_b], writes=[kf_b])
            S.op("dve", lambda e: e.tensor_tensor(out=r[:], in0=r[:], in1=kf[:], op=ALU.subtract),
                 reads=[r_b, kf_b], writes=[r_b])
            S.op("dve", lambda e: e.tensor_scalar(out=kf[:], in0=r[:], scalar1=0.5, scalar2=None, op0=ALU.is_gt),
                 reads=[r_b], writes=[kf_b])
            S.op("dve", lambda e: e.tensor_tensor(out=r[:], in0=r[:], in1=kf[:], op=ALU.subtract),
                 reads=[r_b, kf_b], writes=[r_b])
            S.op("dve", lambda e: e.tensor_scalar(out=kf[:], in0=r[:], scalar1=-0.5, scalar2=None, op0=ALU.is_lt),
                 reads=[r_b], writes=[kf_b])
            S.op("dve", lambda e: e.tensor_tensor(out=r[:], in0=r[:], in1=kf[:], op=ALU.add),
                 reads=[r_b, kf_b], writes=[r_b])
            S.op("act", lambda e, outt=outt: e.activation(out=outt, in_=r[:], func=AF.Sin, scale=2 * math.pi),
                 reads=[r_b], writes=[out_b])


class RR:
    def __init__(self, items):
        self.items = items
        self.i = 0

    def get(self):
        it = self.items[self.i % len(self.items)]
        self.i += 1
        return it


def build(cfg, debug=False):
    D, SEQ, H, NOPE, ROPE, DV = cfg["D"], cfg["SEQ"], cfg["H"], cfg["NOPE"], cfg["ROPE"], cfg["DV"]
    QL, KVL, G, P, C, E, DE = cfg["QL"], cfg["KVL"], cfg["G"], cfg["P"], cfg["C"], cfg["E"], cfg["DE"]
    EPS = cfg["EPS"]
    QKD = NOPE + ROPE
    DATT = H * DV
    DSSM = G * C
    DMIX = DATT + DSSM
    DIN = QL + KVL + ROPE + DSSM
    OWN = SEQ // 2
    KC = D // 128
    NT = SEQ // 128
    EL = E // N_CORES
    HALF = ROPE // 2
    assert NOPE == 128 and DV == 128 and ROPE == 64 and DMIX == D

    nc = bass.Bass("TRN2", target_bir_lowering=False)

    def din(name, shape, dt=F32):
        return nc.dram_tensor(name, list(shape), dt, kind="ExternalInput").ap()

    x = din("x", [SEQ, D])
    pos = din("pos", [SEQ, 1], I32)
    half_sel = din("half_sel", [1, 1], I32)
    invf = din("invf", [1, HALF])
    attn_norm_g = din("attn_norm_g", [1, D])
    w_in = din("w_in", [D, DIN])
    q_lora_norm_g = din("q_lora_norm_g", [1, QL])
    w_q_up = din("w_q_up", [QL, H * QKD])
    kv_lora_norm_g = din("kv_lora_norm_g", [1, KVL])
    w_kv_up = din("w_kv_up", [KVL, H * (NOPE + DV)])
    q_head_norm_g = din("q_head_norm_g", [1, QKD])
    k_head_norm_g = din("k_head_norm_g", [1, QKD])
    attn_out_norm_g = din("attn_out_norm_g", [1, DATT])
    ssm_out_norm_g = din("ssm_out_norm_g", [1, DSSM])
    w_out = din("w_out", [DMIX, D])
    ssm_a_re = din("ssm_a_re", [2, G, P])
    ssm_a_im = din("ssm_a_im", [2, G, P])
    ssm_log_step = din("ssm_log_step", [1, 2 * G])
    ssm_b_re = din("ssm_b_re", [2, G, P, C])
    ssm_b_im = din("ssm_b_im", [2, G, P, C])
    ssm_c_re = din("ssm_c_re", [2, G, C, P])
    ssm_c_im = din("ssm_c_im", [2, G, C, P])
    ssm_d = din("ssm_d", [1, DSSM])
    ssm_w_glu = din("ssm_w_glu", [DSSM, DSSM])
    ssm_b_glu = din("ssm_b_glu", [1, DSSM])
    halff = din("halff", [1, 1])
    ffn_norm_g = din("ffn_norm_g", [1, D])
    w_router = din("w_router", [D, E])
    b_router = din("b_router", [1, E])
    w_gate = din("w_gate", [E * D, DE])
    w_up = din("w_up", [E * D, DE])
    w_down = din("w_down", [E * DE, D])
    b_gate = din("b_gate", [E, DE])
    b_up = din("b_up", [E, DE])
    b_down = din("b_down", [E, D])
    CAP = cfg["CAP"]
    NSLOT = E * CAP
    own_x = din("own_x", [OWN, D])
    own_pos = din("own_pos", [OWN, 1], I32)
    out = nc.dram_tensor("out", [OWN, D], F32, kind="ExternalOutput").ap()
    cnt_out = nc.dram_tensor("cnt_out", [128, E], F32, kind="ExternalOutput").ap()
    dbg = {}

    def dout(name, shape, dt=F32):
        return nc.dram_tensor(name, list(shape), dt, kind="ExternalOutput").ap()

    ges = ExitStack()
    with ges:
        S = Sched(nc, ges)
        gcx = Ctx(nc, ges, S)
        w_in_bf, w_in_bf_b = gcx.dram([D, DIN], BF16, "w_in_bf")
        w_q_bf, w_q_bf_b = gcx.dram([QL, H * QKD], BF16, "w_q_bf")
        w_kv_bf, w_kv_bf_b = gcx.dram([KVL, H * 256], BF16, "w_kv_bf")
        w_out_bf, w_out_bf_b = gcx.dram([DMIX, D], BF16, "w_out_bf")
        proj, proj_b = gcx.dram([SEQ, KVL + ROPE], F32, "proj")
        projq, projq_b = gcx.dram([OWN, QL], F32, "projq")
        kTn, kTn_b = gcx.dram([H, 128, SEQ], BF16, "kTn")
        kTr, kTr_b = gcx.dram([H, 64, SEQ], BF16, "kTr")
        qTn, qTn_b = gcx.dram([H, 128, OWN], BF16, "qTn")
        qTr, qTr_b = gcx.dram([H, 64, OWN], BF16, "qTr")
        vsc, vsc_b = gcx.dram([SEQ, H * DV], BF16, "vsc")
        if debug:
            mixT = dout("mixT", [DMIX, OWN])
            mixT_b = Buf("mixT")
        else:
            mixT, mixT_b = gcx.dram([DMIX, OWN], F32, "mixT")
        h1, h1_b = gcx.dram([OWN, D], F32, "h1")
        w_glu_bf, w_glu_bf_b = gcx.dram([DSSM, DSSM], BF16, "w_glu_bf")
        Xd, Xd_b = gcx.dram([NSLOT, D], BF16, "Xd")
        Yd, Yd_b = gcx.dram([NSLOT, D], BF16, "Yd")
        idxd, idxd_b = gcx.dram([OWN, 4], I32, "idxd")
        gated, gated_b = gcx.dram([OWN, 4], F32, "gated")
        uT32, uT32_b = gcx.dram([DSSM, SEQ], F32, "uT32")
        uTb, uTb_b = gcx.dram([DSSM, SEQ], BF16, "uTb")
        gT32, gT32_b = gcx.dram([DSSM, OWN], F32, "gT32")
        gTb, gTb_b = gcx.dram([DSSM, OWN], BF16, "gTb")

        with Phase(nc, S) as ph:
            for (src, dst, dst_b, rows) in ((w_in, w_in_bf, w_in_bf_b, D), (w_q_up, w_q_bf, w_q_bf_b, QL),
                                            (w_kv_up, w_kv_bf, w_kv_bf_b, KVL), (w_out, w_out_bf, w_out_bf_b, DMIX),
                                            (ssm_w_glu, w_glu_bf, w_glu_bf_b, DSSM)):
                for r0 in range(0, rows, 512):
                    r1 = min(rows, r0 + 512)
                    S.dma("pool", lambda e, src=src, dst=dst, r0=r0, r1=r1: e.dma_start(out=dst[r0:r1, :], in_=src[r0:r1, :]),
                          writes=[dst_b])

        def phase_a(xsrc, ntile, col_lo, col_hi, dst, dst_b, fm=None):
          GT = min(4, ntile)
          GW = GT * 128
          with Phase(nc, S) as ph:
              cx = ph.cx
              ident, ident_b = ph.identity()
              g1, g1_b = ph.bcast_load(attn_norm_g[0:1, :], D, "g1")
              xt_p = RR([cx.sb([128, D], F32, "xt") for _ in range(2)])
              scr, scr_b = cx.sb([128, D], BF16, "scr")
              z_p = RR([cx.sb([128, D], BF16, "z") for _ in range(1)])
              ss_p = RR([cx.sb([128, 1], F32, "ss") for _ in range(2)])
              zT_p = RR([cx.sb([128, KC, GW], BF16, "zT") for _ in range(1)])
              wt_p = RR([cx.sb([128, KC, 512], BF16, "wt") for _ in range(2)])
              ot_p = RR([cx.sb([128, 512], F32, "ot") for _ in range(3)])
              ob_p = RR([cx.sb([128, 512], BF16, "ob") for _ in range(2)])
              pts = [cx.ps([128, 8, 128], BF16, "ptr") for _ in range(2)]
              pm_p = RR([cx.ps([128, 512], F32, "pm") for _ in range(3)])
              for gi in range(ntile // GT):
                  own_g = (gi * GW) // OWN
                  zT, zT_b = zT_p.get()
                  for ti in range(GT):
                      t0 = gi * GW + ti * 128
                      xt, xt_b = xt_p.get()
                      z, z_b = z_p.get()
                      ss, ss_b = ss_p.get()
                      S.dma("sp", lambda e, xt=xt, t0=t0: e.dma_start(out=xt[:], in_=xsrc[t0:t0 + 128, :]), writes=[xt_b])
                      ph.rmsnorm(xt[:], xt_b, D, g1[:], g1_b, z[:], z_b, scr[:], scr_b, ss[:], ss_b, EPS)
                      ph.transpose_chunks(z, z_b, KC, zT[:, :, ti * 128:(ti + 1) * 128], zT_b, ident, ident_b, pts)
                  for c0 in range(col_lo, col_hi, 512):
                      c1 = min(col_hi, c0 + 512)
                      wt, wt_b = wt_p.get()
                      S.dma("sp", lambda e, wt=wt, c0=c0, c1=c1: e.dma_start(
                          out=wt[:, :, 0:c1 - c0], in_=w_in_bf[:, c0:c1].rearrange("(c p) n -> p c n", p=128)),
                          reads=[w_in_bf_b], writes=[wt_b])
                      for ti in range(GT):
                          t0 = gi * GW + ti * 128
                          pm, pm_b = pm_p.get()
                          for c in range(KC):
                              S.op("pe", lambda e, pm=pm, zT=zT, wt=wt, c=c, ti=ti, n=c1 - c0: e.matmul(
                                  pm[:, 0:n], lhsT=zT[:, c, ti * 128:(ti + 1) * 128], rhs=wt[:, c, 0:n],
                                  start=(c == 0), stop=(c == KC - 1)), reads=[zT_b, wt_b], writes=[pm_b])
                          ot, ot_b = ot_p.get()
                          S.op("dve", lambda e, ot=ot, pm=pm, n=c1 - c0: e.tensor_copy(out=ot[:, 0:n], in_=pm[:, 0:n]),
                               reads=[pm_b], writes=[ot_b])
                          S.dma("sp", lambda e, ot=ot, t0=t0, c0=c0, c1=c1: e.dma_start(out=dst[t0:t0 + 128, c0 - col_lo:c1 - col_lo],
                                                                                    in_=ot[:, 0:c1 - c0]),
                                reads=[ot_b], writes=[dst_b])
                  if fm is not None:
                      g0 = gi * GW
                      for c0 in range(fm[0], fm[1], 512):
                          c1 = min(fm[1], c0 + 512)
                          wt, wt_b = wt_p.get()
                          S.dma("sp", lambda e, wt=wt, c0=c0, c1=c1: e.dma_start(
                              out=wt[:, :, 0:c1 - c0], in_=w_in_bf[:, c0:c1].rearrange("(c p) n -> p c n", p=128)),
                              reads=[w_in_bf_b], writes=[wt_b])
                          for k in range((c1 - c0) // 128):
                              r0 = c0 - fm[0] + k * 128
                              pm, pm_b = pm_p.get()
                              for c in range(KC):
                                  S.op("pe", lambda e, pm=pm, zT=zT, wt=wt, c=c, k=k: e.matmul(
                                      pm[:, 0:GW], lhsT=wt[:, c, k * 128:(k + 1) * 128], rhs=zT[:, c, :],
                                      start=(c == 0), stop=(c == KC - 1)), reads=[zT_b, wt_b], writes=[pm_b])
                              ot, ot_b = ot_p.get()
                              ob, ob_b = ob_p.get()
                              S.op("act", lambda e, ot=ot, pm=pm: e.copy(out=ot[:, 0:GW], in_=pm[:, 0:GW]), reads=[pm_b], writes=[ot_b])
                              S.op("dve", lambda e, ob=ob, ot=ot: e.tensor_copy(out=ob[:, 0:GW], in_=ot[:, 0:GW]), reads=[ot_b], writes=[ob_b])
                              S.dma("sp", lambda e, ot=ot, r0=r0, g0=g0: e.dma_start(out=uT32[r0:r0 + 128, g0:g0 + GW], in_=ot[:, 0:GW]),
                                    reads=[ot_b], writes=[uT32_b])
                              S.dma("sp", lambda e, ob=ob, r0=r0, g0=g0: e.dma_start(out=uTb[r0:r0 + 128, g0:g0 + GW], in_=ob[:, 0:GW]),
                                    reads=[ob_b], writes=[uTb_b])


        phase_a(x, NT, QL, QL + KVL + ROPE, proj, proj_b, fm=(QL + KVL + ROPE, DIN))
        phase_a(own_x, OWN // 128, 0, QL, projq, projq_b)

        with Phase(nc, S) as ph:
            cx = ph.cx
            ident, ident_b = ph.identity()
            gq, gq_b = ph.bcast_load(q_lora_norm_g[0:1, :], QL, "gq")
            gkv, gkv_b = ph.bcast_load(kv_lora_norm_g[0:1, :], KVL, "gkv")
            gqh, gqh_b = ph.bcast_load(q_head_norm_g[0:1, :], QKD, "gqh")
            gkh, gkh_b = ph.bcast_load(k_head_norm_g[0:1, :], QKD, "gkh")
            ivf, ivf_b = ph.bcast_load(invf[0:1, :], HALF, "ivf")
            wq, wq_b = cx.sb([128, QL // 128, H * QKD], BF16, "wq")
            wkv, wkv_b = cx.sb([128, KVL // 128, H * 256], BF16, "wkv")
            S.dma("sp", lambda e: e.dma_start(out=wq[:], in_=w_q_bf.rearrange("(c p) n -> p c n", p=128)),
                  reads=[w_q_bf_b], writes=[wq_b])
            S.dma("sp", lambda e: e.dma_start(out=wkv[:], in_=w_kv_bf.rearrange("(c p) n -> p c n", p=128)),
                  reads=[w_kv_bf_b], writes=[wkv_b])
            pts = [cx.ps([128, 8, 128], BF16, "ptr") for _ in range(2)]
            pm_p = RR([cx.ps([128, 512], F32, "pm") for _ in range(3)])
            NMAX = max(QL, KVL)
            scr, scr_b = cx.sb([128, NMAX], BF16, "scr")
            ss, ss_b = cx.sb([128, 1], F32, "ss")
            lat, lat_b = cx.sb([128, NMAX], F32, "lat")
            latn, latn_b = cx.sb([128, NMAX], BF16, "latn")
            latT, latT_b = cx.sb([128, NMAX // 128, 128], BF16, "latT")
            big, big_b = cx.sb([128, H * 256], F32, "big")
            sq, sq_b = cx.sb([128, H * QKD], F32, "sq")
            ssh, ssh_b = cx.sb([128, H], F32, "ssh")
            kpe, kpe_b = cx.sb([128, ROPE], F32, "kpe")
            sspe, sspe_b = cx.sb([128, 1], F32, "sspe")
            pe2, pe2_b = cx.sb([128, ROPE], F32, "pe2")
            nn, nn_b = cx.sb([128, H, 128], F32, "nn")
            nnb, nnb_b = cx.sb([128, H * 128], BF16, "nnb")
            rr, rr_b = cx.sb([128, H, ROPE], F32, "rr")
            ra, ra_b = cx.sb([128, H, HALF], F32, "ra")
            rb, rb_b = cx.sb([128, H, HALF], F32, "rb")
            rrb, rrb_b = cx.sb([128, H * ROPE], BF16, "rrb")
            nT, nT_b = cx.sb([128, H, 128], BF16, "nT")
            rT, rT_b = cx.sb([128, max(1, H * ROPE // 128), 128], BF16, "rT")
            vb, vb_b = cx.sb([128, H, DV], BF16, "vb")
            posi, posi_b = cx.sb([128, 1], I32, "posi")
            posf, posf_b = cx.sb([128, 1], F32, "posf")
            ang, ang_b = cx.sb([128, HALF], F32, "ang")
            cs, cs_b = cx.sb([128, 2, HALF], F32, "cs")

            def heads_finish(nope_view, rope_in, rope_bcast_h, gains, gains_b, dstn, dstn_b, dstr, dstr_b, col0):
                ph.rstd(ssh[:], ssh_b, QKD, EPS)
                S.op("dve", lambda e: e.tensor_tensor(out=nn[:], in0=nope_view, in1=ssh[:, :, None].to_broadcast([128, H, 128]),
                                                      op=ALU.mult), reads=[big_b, ssh_b], writes=[nn_b])
                S.op("dve", lambda e: e.tensor_tensor(out=nnb[:].rearrange("p (h d) -> p h d", h=H), in0=nn[:],
                                                      in1=gains[:, None, 0:128].to_broadcast([128, H, 128]), op=ALU.mult),
                     reads=[nn_b, gains_b], writes=[nnb_b])
                rin = rope_in if not rope_bcast_h else rope_in[:, None, :].to_broadcast([128, H, ROPE])
                S.op("dve", lambda e: e.tensor_tensor(out=rr[:], in0=rin, in1=ssh[:, :, None].to_broadcast([128, H, ROPE]),
                                                      op=ALU.mult), reads=[big_b, kpe_b, ssh_b], writes=[rr_b])
                S.op("dve", lambda e: e.tensor_tensor(out=rr[:], in0=rr[:], in1=gains[:, None, 128:QKD].to_broadcast([128, H, ROPE]),
                                                      op=ALU.mult), reads=[rr_b, gains_b], writes=[rr_b])
                sinb = cs[:, 0:1, :].to_broadcast([128, H, HALF])
                cosb = cs[:, 1:2, :].to_broadcast([128, H, HALF])
                x1 = rr[:, :, 0:HALF]
                x2 = rr[:, :, HALF:ROPE]
                ro = rrb[:].rearrange("p (h d) -> p h d", h=H)
                S.op("dve", lambda e: e.tensor_tensor(out=ra[:], in0=x1, in1=cosb, op=ALU.mult), reads=[rr_b, cs_b], writes=[ra_b])
                S.op("dve", lambda e: e.tensor_tensor(out=rb[:], in0=x2, in1=sinb, op=ALU.mult), reads=[rr_b, cs_b], writes=[rb_b])
                S.op("dve", lambda e: e.tensor_tensor(out=ro[:, :, 0:HALF], in0=ra[:], in1=rb[:], op=ALU.subtract),
                     reads=[ra_b, rb_b], writes=[rrb_b])
                S.op("dve", lambda e: e.tensor_tensor(out=ra[:], in0=x2, in1=cosb, op=ALU.mult), reads=[rr_b, cs_b, rrb_b], writes=[ra_b])
                S.op("dve", lambda e: e.tensor_tensor(out=rb[:], in0=x1, in1=sinb, op=ALU.mult), reads=[rr_b, cs_b, rrb_b], writes=[rb_b])
                S.op("dve", lambda e: e.tensor_tensor(out=ro[:, :, HALF:ROPE], in0=ra[:], in1=rb[:], op=ALU.add),
                     reads=[ra_b, rb_b], writes=[rrb_b])
                ph.transpose_chunks(nnb, nnb_b, H, nT[:], nT_b, ident, ident_b, pts)
                ph.transpose_chunks(rrb, rrb_b, H * ROPE // 128, rT[:], rT_b, ident, ident_b, pts)
                S.dma("sp", lambda e: e.dma_start(out=dstn[:, :, col0:col0 + 128].rearrange("h d t -> d h t"), in_=nT[:]),
                      reads=[nT_b], writes=[dstn_b])
                S.dma("sp", lambda e: e.dma_start(
                    out=dstr[:, :, col0:col0 + 128].rearrange("(j two) d t -> (two d) j t", two=2), in_=rT[:]),
                    reads=[rT_b], writes=[dstr_b])

            for ti in range(NT):
                t0 = ti * 128
                S.dma("sp", lambda e, t0=t0: e.dma_start(out=posi[:], in_=pos[t0:t0 + 128, :]), writes=[posi_b])
                S.op("dve", lambda e: e.tensor_copy(out=posf[:], in_=posi[:]), reads=[posi_b], writes=[posf_b])
                S.op("dve", lambda e: e.tensor_scalar(out=ang[:], in0=ivf[:], scalar1=posf[:, 0:1], scalar2=None, op0=ALU.mult),
                     reads=[ivf_b, posf_b], writes=[ang_b])
                ph.sincos(ang[:], ang_b, [128, HALF], cs[:, 0, :], cs[:, 1, :], cs_b)
                S.dma("sp", lambda e, t0=t0: e.dma_start(out=lat[:, 0:KVL], in_=proj[t0:t0 + 128, 0:KVL]),
                      reads=[proj_b], writes=[lat_b])
                S.dma("sp", lambda e, t0=t0: e.dma_start(out=kpe[:], in_=proj[t0:t0 + 128, KVL:KVL + ROPE]),
                      reads=[proj_b], writes=[kpe_b])
                ph.rmsnorm(lat[:, 0:KVL], lat_b, KVL, gkv[:], gkv_b, latn[:, 0:KVL], latn_b, scr[:, 0:KVL], scr_b, ss[:], ss_b, EPS)
                ph.transpose_chunks(latn, latn_b, KVL // 128, latT[:, 0:KVL // 128, :], latT_b, ident, ident_b, pts)
                for c0 in range(0, H * 256, 512):
                    n = min(512, H * 256 - c0)
                    pm, pm_b = pm_p.get()
                    for c in range(KVL // 128):
                        S.op("pe", lambda e, pm=pm, c=c, c0=c0, n=n: e.matmul(pm[:, 0:n], lhsT=latT[:, c, :], rhs=wkv[:, c, c0:c0 + n],
                                                                          start=(c == 0), stop=(c == KVL // 128 - 1)),
                             reads=[latT_b, wkv_b], writes=[pm_b])
                    S.op("act", lambda e, pm=pm, c0=c0, n=n: e.copy(out=big[:, c0:c0 + n], in_=pm[:, 0:n]), reads=[pm_b], writes=[big_b])
                kvv = big[:].rearrange("p (h d) -> p h d", h=H)
                S.op("dve", lambda e: e.tensor_copy(out=vb[:], in_=kvv[:, :, 128:256]), reads=[big_b], writes=[vb_b])
                S.dma("sp", lambda e, t0=t0: e.dma_start(out=vsc[t0:t0 + 128, :], in_=vb[:].rearrange("p h d -> p (h d)")),
                      reads=[vb_b], writes=[vsc_b])
                sqv = sq[:, 0:H * 128].rearrange("p (h d) -> p h d", h=H)
                S.op("dve", lambda e: e.tensor_tensor(out=sqv, in0=kvv[:, :, 0:128], in1=kvv[:, :, 0:128], op=ALU.mult),
                     reads=[big_b], writes=[sq_b])
                S.op("dve", lambda e: e.reduce_sum(out=ssh[:], in_=sqv, axis=AX.X), reads=[sq_b], writes=[ssh_b])
                S.op("dve", lambda e: e.tensor_tensor(out=pe2[:], in0=kpe[:], in1=kpe[:], op=ALU.mult), reads=[kpe_b], writes=[pe2_b])
                S.op("dve", lambda e: e.reduce_sum(out=sspe[:], in_=pe2[:], axis=AX.X), reads=[pe2_b], writes=[sspe_b])
                S.op("dve", lambda e: e.tensor_scalar(out=ssh[:], in0=ssh[:], scalar1=sspe[:, 0:1], scalar2=None, op0=ALU.add),
                     reads=[ssh_b, sspe_b], writes=[ssh_b])
                heads_finish(kvv[:, :, 0:128], kpe[:], True, gkh, gkh_b, kTn, kTn_b, kTr, kTr_b, t0)
                if t0 < OWN:
                    pass
            for ti in range(OWN // 128):
                t0 = ti * 128
                S.dma("sp", lambda e, t0=t0: e.dma_start(out=posi[:], in_=own_pos[t0:t0 + 128, :]), writes=[posi_b])
                S.op("dve", lambda e: e.tensor_copy(out=posf[:], in_=posi[:]), reads=[posi_b], writes=[posf_b])
                S.op("dve", lambda e: e.tensor_scalar(out=ang[:], in0=ivf[:], scalar1=posf[:, 0:1], scalar2=None, op0=ALU.mult),
                     reads=[ivf_b, posf_b], writes=[ang_b])
                ph.sincos(ang[:], ang_b, [128, HALF], cs[:, 0, :], cs[:, 1, :], cs_b)
                S.dma("sp", lambda e, t0=t0: e.dma_start(out=lat[:, 0:QL], in_=projq[t0:t0 + 128, 0:QL]),
                      reads=[projq_b], writes=[lat_b])
                ph.rmsnorm(lat[:, 0:QL], lat_b, QL, gq[:], gq_b, latn[:, 0:QL], latn_b, scr[:, 0:QL], scr_b, ss[:], ss_b, EPS)
                ph.transpose_chunks(latn, latn_b, QL // 128, latT[:, 0:QL // 128, :], latT_b, ident, ident_b, pts)
                for c0 in range(0, H * QKD, 512):
                    n = min(512, H * QKD - c0)
                    pm, pm_b = pm_p.get()
                    for c in range(QL // 128):
                        S.op("pe", lambda e, pm=pm, c=c, c0=c0, n=n: e.matmul(pm[:, 0:n], lhsT=latT[:, c, :], rhs=wq[:, c, c0:c0 + n],
                                                                          start=(c == 0), stop=(c == QL // 128 - 1)),
                             reads=[latT_b, wq_b], writes=[pm_b])
                    S.op("act", lambda e, pm=pm, c0=c0, n=n: e.copy(out=big[:, c0:c0 + n], in_=pm[:, 0:n]), reads=[pm_b], writes=[big_b])
                qv = big[:, 0:H * QKD].rearrange("p (h d) -> p h d", h=H)
                sqq = sq[:].rearrange("p (h d) -> p h d", h=H)
                S.op("dve", lambda e: e.tensor_tensor(out=sqq, in0=qv, in1=qv, op=ALU.mult), reads=[big_b], writes=[sq_b])
                S.op("dve", lambda e: e.reduce_sum(out=ssh[:], in_=sqq, axis=AX.X), reads=[sq_b], writes=[ssh_b])
                heads_finish(qv[:, :, 0:128], qv[:, :, 128:QKD], False, gqh, gqh_b, qTn, qTn_b, qTr, qTr_b, t0)

        QB = min(512, OWN)
        NKT = SEQ // 128
        att_scale = QKD ** -0.5
        att_shift = -12.0
        with Phase(nc, S) as ph:
            cx = ph.cx
            ones, ones_b = cx.sb([128, 128], BF16, "ones")
            S.op("pool", lambda e: e.memset(ones[:], 1.0), writes=[ones_b])
            shf, shf_b = cx.sb([128, 1], F32, "shf")
            S.op("pool", lambda e: e.memset(shf[:], att_shift), writes=[shf_b])
            kn_p = RR([cx.sb([128, SEQ], BF16, "kn") for _ in range(2)])
            kr_p = RR([cx.sb([64, SEQ], BF16, "kr") for _ in range(2)])
            v_p = RR([cx.sb([128, NKT, DV], BF16, "v") for _ in range(2)])
            qn_p = RR([cx.sb([128, QB], BF16, "qn") for _ in range(2)])
            qr_p = RR([cx.sb([64, QB], BF16, "qr") for _ in range(2)])
            pT_p = RR([cx.sb([128, QB], BF16, "pT") for _ in range(3)])
            psS_p = RR([cx.ps([128, QB], F32, "psS") for _ in range(3)])
            psO_p = RR([cx.ps([128, QB], F32, "psO") for _ in range(2)])
            psL_p = RR([cx.ps([128, QB], F32, "psL") for _ in range(2)])
            rl_p = RR([cx.sb([128, QB], F32, "rl") for _ in range(2)])
            o_p = RR([cx.sb([128, QB], F32, "o") for _ in range(2)])
            for h in range(H):
                kn, kn_b = kn_p.get()
                kr, kr_b = kr_p.get()
                v, v_b = v_p.get()
                S.dma("sp", lambda e, kn=kn, h=h: e.dma_start(out=kn[:], in_=kTn[h]), reads=[kTn_b], writes=[kn_b])
                S.dma("sp", lambda e, kr=kr, h=h: e.dma_start(out=kr[:], in_=kTr[h]), reads=[kTr_b], writes=[kr_b])
                S.dma("sp", lambda e, v=v, h=h: e.dma_start(
                    out=v[:], in_=vsc[:, h * DV:(h + 1) * DV].rearrange("(t p) d -> p t d", p=128)),
                    reads=[vsc_b], writes=[v_b])
                for qb in range(OWN // QB):
                    q0 = qb * QB
                    qn, qn_b = qn_p.get()
                    qr, qr_b = qr_p.get()
                    S.dma("sp", lambda e, qn=qn, h=h, q0=q0: e.dma_start(out=qn[:], in_=qTn[h, :, q0:q0 + QB]),
                          reads=[qTn_b], writes=[qn_b])
                    S.dma("sp", lambda e, qr=qr, h=h, q0=q0: e.dma_start(out=qr[:], in_=qTr[h, :, q0:q0 + QB]),
                          reads=[qTr_b], writes=[qr_b])
                    psO, psO_b = psO_p.get()
                    psL, psL_b = psL_p.get()
                    for kt in range(NKT):
                        psS, psS_b = psS_p.get()
                        pT, pT_b = pT_p.get()
                        S.op("pe", lambda e, psS=psS, kn=kn, qn=qn, kt=kt: e.matmul(
                            psS[:], lhsT=kn[:, kt * 128:(kt + 1) * 128], rhs=qn[:], start=True, stop=False),
                            reads=[kn_b, qn_b], writes=[psS_b])
                        S.op("pe", lambda e, psS=psS, kr=kr, qr=qr, kt=kt: e.matmul(
                            psS[:], lhsT=kr[:, kt * 128:(kt + 1) * 128], rhs=qr[:], start=False, stop=True),
                            reads=[kr_b, qr_b], writes=[psS_b])
                        S.op("act", lambda e, pT=pT, psS=psS: e.activation(out=pT[:], in_=psS[:], func=AF.Exp,
                                                                            bias=shf[:, 0:1], scale=att_scale),
                             reads=[psS_b, shf_b], writes=[pT_b])
                        S.op("pe", lambda e, psO=psO, v=v, pT=pT, kt=kt: e.matmul(
                            psO[:], lhsT=v[:, kt, :], rhs=pT[:], start=(kt == 0), stop=(kt == NKT - 1)),
                            reads=[v_b, pT_b], writes=[psO_b])
                        S.op("pe", lambda e, psL=psL, pT=pT, kt=kt: e.matmul(
                            psL[:], lhsT=ones[:], rhs=pT[:], start=(kt == 0), stop=(kt == NKT - 1)),
                            reads=[ones_b, pT_b], writes=[psL_b])
                    rl, rl_b = rl_p.get()
                    o, o_b = o_p.get()
                    S.op("dve", lambda e, rl=rl, psL=psL: e.reciprocal(out=rl[:], in_=psL[:]), reads=[psL_b], writes=[rl_b])
                    S.op("dve", lambda e, o=o, psO=psO, rl=rl: e.tensor_tensor(out=o[:], in0=psO[:], in1=rl[:], op=ALU.mult),
                         reads=[psO_b, rl_b], writes=[o_b])
                    S.dma("sp", lambda e, o=o, h=h, q0=q0: e.dma_start(out=mixT[h * DV:(h + 1) * DV, q0:q0 + QB], in_=o[:]),
                          reads=[o_b], writes=[mixT_b])
        if debug == "C":
            return nc

        T = SEQ
        SEG = min(cfg.get("SEG", 512), T)
        NSEG = T // SEG
        SC = DSSM // 128
        NCH = G // 8
        u0 = KVL + ROPE
        NG2 = 2 * G
        TWO_PI = 2.0 * math.pi
        if debug == "D0":
            return nc
        with Phase(nc, S) as ph:
            cx = ph.cx
            identb, identb_b = ph.identity()

            def small(name, dt=F32, w=NG2):
                return cx.sb([128, w], dt, name)
            AR, AR_b = small("AR")
            AI, AI_b = small("AI")
            for (t_, b_, src) in ((AR, AR_b, ssm_a_re), (AI, AI_b, ssm_a_im)):
                for r0 in (0, 64):
                    S.dma("sp", lambda e, t_=t_, src=src, r0=r0: e.dma_start(out=t_[r0:r0 + 64, :], in_=src.rearrange("d g p -> p (d g)"),
                                                                          allow_slow_non_contiguous=True), writes=[b_])
            LS, LS_b = ph.bcast_load(ssm_log_step[0:1, :], NG2, "LS")
            hf, hf_b = ph.bcast_load(halff[0:1, :], 1, "hf")
            dcol, dcol_b = cx.sb([128, SC], F32, "dcol")
            S.dma("sp", lambda e: e.dma_start(out=dcol[:], in_=ssm_d.rearrange("o (c p) -> p (o c)", p=128), allow_slow_non_contiguous=True),
                  writes=[dcol_b])
            SGN, SGN_b = cx.sb([128, 1], F32, "SGN")
            NSGN, NSGN_b = cx.sb([128, 1], F32, "NSGN")
            S.op("pool", lambda e: e.memset(SGN[0:64, :], -1.0), writes=[SGN_b])
            S.op("pool", lambda e: e.memset(SGN[64:128, :], 1.0), writes=[SGN_b])
            S.op("pool", lambda e: e.memset(NSGN[0:64, :], 1.0), writes=[NSGN_b])
            S.op("pool", lambda e: e.memset(NSGN[64:128, :], -1.0), writes=[NSGN_b])
            hpi, hpi_b = cx.sb([128, 1], F32, "hpi")
            S.op("pool", lambda e: e.memset(hpi[:], math.pi / 2), writes=[hpi_b])

            def tt(out, a, b, op, rb, wb, eng="dve"):
                S.op(eng, lambda e: e.tensor_tensor(out=out, in0=a, in1=b, op=op), reads=rb, writes=wb)

            DT, DT_b = small("DT")
            MAG, MAG_b = small("MAG")
            TH, TH_b = small("TH")
            SN, SN_b = small("SN")
            CS, CS_b = small("CS")
            S.op("act", lambda e: e.activation(out=DT[:], in_=LS[:], func=AF.Exp), reads=[LS_b], writes=[DT_b])
            tt(MAG[:], AR[:], DT[:], ALU.mult, [AR_b, DT_b], [MAG_b])
            S.op("act", lambda e: e.activation(out=MAG[:], in_=MAG[:], func=AF.Exp), reads=[MAG_b], writes=[MAG_b])
            tt(TH[:], AI[:], DT[:], ALU.mult, [AI_b, DT_b], [TH_b])
            ph.sincos(TH[:], TH_b, [128, NG2], SN[:], CS[:], SN_b)
            CS_b = SN_b
            ABR, ABR_b = small("ABR")
            ABI, ABI_b = small("ABI")
            tt(ABR[:], MAG[:], CS[:], ALU.mult, [MAG_b, CS_b], [ABR_b])
            tt(ABI[:], MAG[:], SN[:], ALU.mult, [MAG_b, SN_b], [ABI_b])
            DEN, DEN_b = small("DEN")
            TMP, TMP_b = small("TMP")
            tt(DEN[:], AR[:], AR[:], ALU.mult, [AR_b], [DEN_b])
            tt(TMP[:], AI[:], AI[:], ALU.mult, [AI_b], [TMP_b])
            tt(DEN[:], DEN[:], TMP[:], ALU.add, [DEN_b, TMP_b], [DEN_b])
            S.op("dve", lambda e: e.reciprocal(out=DEN[:], in_=DEN[:]), reads=[DEN_b], writes=[DEN_b])
            E1, E1_b = small("E1")
            S.op("dve", lambda e: e.tensor_scalar(out=E1[:], in0=ABR[:], scalar1=-1.0, scalar2=None, op0=ALU.add), reads=[ABR_b], writes=[E1_b])
            ZR, ZR_b = small("ZR")
            ZI, ZI_b = small("ZI")
            tt(ZR[:], E1[:], AR[:], ALU.mult, [E1_b, AR_b], [ZR_b])
            tt(TMP[:], ABI[:], AI[:], ALU.mult, [ABI_b, AI_b], [TMP_b])
            tt(ZR[:], ZR[:], TMP[:], ALU.add, [ZR_b, TMP_b], [ZR_b])
            tt(ZR[:], ZR[:], DEN[:], ALU.mult, [ZR_b, DEN_b], [ZR_b])
            tt(ZI[:], ABI[:], AR[:], ALU.mult, [ABI_b, AR_b], [ZI_b])
            tt(TMP[:], E1[:], AI[:], ALU.mult, [E1_b, AI_b, ZR_b], [TMP_b])
            tt(ZI[:], ZI[:], TMP[:], ALU.subtract, [ZI_b, TMP_b], [ZI_b])
            tt(ZI[:], ZI[:], DEN[:], ALU.mult, [ZI_b, DEN_b], [ZI_b])
            ZIs, ZIs_b = small("ZIs")
            ZRs, ZRs_b = small("ZRs")
            S.op("dve", lambda e: e.tensor_scalar(out=ZIs[:], in0=ZI[:], scalar1=SGN[:, 0:1], scalar2=None, op0=ALU.mult),
                 reads=[ZI_b, SGN_b], writes=[ZIs_b])
            S.op("dve", lambda e: e.tensor_scalar(out=ZRs[:], in0=ZR[:], scalar1=NSGN[:, 0:1], scalar2=None, op0=ALU.mult),
                 reads=[ZR_b, NSGN_b], writes=[ZRs_b])
            FQ, FQ_b = small("FQ")
            S.op("dve", lambda e: e.tensor_scalar(out=FQ[:, 0:G], in0=TH[:, 0:G], scalar1=1.0 / TWO_PI, scalar2=None, op0=ALU.mult),
                 reads=[TH_b], writes=[FQ_b])
            S.op("dve", lambda e: e.tensor_scalar(out=FQ[:, G:NG2], in0=TH[:, G:NG2], scalar1=-1.0 / TWO_PI, scalar2=None, op0=ALU.mult),
                 reads=[TH_b], writes=[FQ_b])
            rmi, rmi_b = cx.sb([128, 8], I32, "rmi")
            rmf, rmf_b = cx.sb([128, 8], F32, "rmf")
            rm2, rm2_b = cx.sb([128, 8], F32, "rm2")
            RM, RM_b = cx.sb([128, 8], F32, "RM")
            S.op("pool", lambda e: e.iota(out=rmi[:], pattern=[[-16, 8]], base=0, channel_multiplier=1), writes=[rmi_b])
            S.op("dve", lambda e: e.tensor_copy(out=rmf[:], in_=rmi[:]), reads=[rmi_b], writes=[rmf_b])
            S.op("dve", lambda e: e.tensor_scalar(out=rm2[:], in0=rmf[:], scalar1=0.0, scalar2=None, op0=ALU.is_ge), reads=[rmf_b], writes=[rm2_b])
            S.op("dve", lambda e: e.tensor_scalar(out=RM[:], in0=rmf[:], scalar1=16.0, scalar2=None, op0=ALU.is_lt), reads=[rmf_b], writes=[RM_b])
            tt(RM[:], RM[:], rm2[:], ALU.mult, [RM_b, rm2_b], [RM_b])
            cmi, cmi_b = cx.sb([128, 8, 128], I32, "cmi")
            cmf, cmf_b = cx.sb([128, 8, 128], F32, "cmf")
            cm2, cm2_b = cx.sb([128, 8, 128], F32, "cm2")
            CM, CM_b = cx.sb([128, 8, 128], F32, "CM")
            S.op("pool", lambda e: e.iota(out=cmi[:], pattern=[[-16, 8], [1, 128]], base=0, channel_multiplier=0), writes=[cmi_b])
            S.op("dve", lambda e: e.tensor_copy(out=cmf[:], in_=cmi[:]), reads=[cmi_b], writes=[cmf_b])
            S.op("dve", lambda e: e.tensor_scalar(out=cm2[:], in0=cmf[:], scalar1=0.0, scalar2=None, op0=ALU.is_ge), reads=[cmf_b], writes=[cm2_b])
            S.op("dve", lambda e: e.tensor_scalar(out=CM[:], in0=cmf[:], scalar1=16.0, scalar2=None, op0=ALU.is_lt), reads=[cmf_b], writes=[CM_b])
            tt(CM[:], CM[:], cm2[:], ALU.mult, [CM_b, cm2_b], [CM_b])
            tii, tii_b = cx.sb([128, T], I32, "tii")
            tio, tio_b = cx.sb([128, T], F32, "tio")
            S.op("pool", lambda e: e.iota(out=tii[:], pattern=[[1, T]], base=0, channel_multiplier=0), writes=[tii_b])
            S.op("dve", lambda e: e.tensor_copy(out=tio[:], in_=tii[:]), reads=[tii_b], writes=[tio_b])

            ucb, ucb_b = cx.sb([128, T], BF16, "ucb")
            uc32, uc32_b = cx.sb([128, T], F32, "uc32")
            ysum, ysum_b = tii[:].bitcast(F32), tii_b
            X1, X1_b = cx.sb([128, 8, C], F32, "X1")
            X2, X2_b = cx.sb([128, 8, C], F32, "X2")
            SB1, SB1_b = cx.sb([128, 8, C], F32, "SB1")
            SB2, SB2_b = cx.sb([128, 8, C], F32, "SB2")
            TM, TM_b = cx.sb([128, 8, C], F32, "TM")
            SBb, SBb_b = cx.sb([128, 2, 128], BF16, "SBb")
            CC, CC_b = cx.sb([128, 2, 128], F32, "CC")
            CCb, CCb_b = cx.sb([128, 2, 128], BF16, "CCb")
            L12, L12_b = cx.sb([128, 2, 128], BF16, "L12")
            LC, LC_b = cx.sb([128, 2, 128], F32, "LC")
            ptb, ptb_b = cx.ps([128, 4, 128], BF16, "ptb")
            PT4, PT4_b = cx.sb([128, 4, 128], BF16, "PT4")
            Lg_p = RR([cx.sb([128, 4, 128], BF16, "Lg") for _ in range(2)])
            ki_p = RR([cx.sb([128, SEG], I32, "ki") for _ in range(2)])
            r_p = RR([cx.sb([128, SEG], F32, "r") for _ in range(2)])
            ab_p = RR([cx.sb([128, SEG], F32, "ab") for _ in range(2)])
            sn_p = RR([cx.sb([128, SEG], F32, "sn") for _ in range(2)])
            cs_p = RR([cx.sb([128, SEG], F32, "cs") for _ in range(2)])
            IN_p = RR([cx.sb([128, SEG], F32, "IN") for _ in range(2)])
            W_p = RR([cx.sb([128, SEG], F32, "W") for _ in range(2)])
            A1_p = RR([cx.sb([128, SEG], BF16, "A1") for _ in range(2)])
            A2_p = RR([cx.sb([128, SEG], BF16, "A2") for _ in range(2)])
            t1_p = RR([cx.sb([128, 512], F32, "t1") for _ in range(2)])
            cw_p = RR([cx.sb([128, 1], F32, "cw") for _ in range(2)])
            pM_p = RR([cx.ps([128, 512], F32, "pM") for _ in range(4)])
            pY_p = RR([cx.ps([128, 512], F32, "pY") for _ in range(2)])
            yo, yo_b = cx.sb([128, OWN], F32, "yo")
            ga, ga_b = cx.sb([128, OWN], F32, "ga")
            gb, gb_b = cx.sb([128, OWN], F32, "gb")
            gbf, gbf_b = cx.sb([128, OWN], BF16, "gbf")
            BW = min(512, SEG)

            for j in range(NCH if debug != "D1a" else 0):
                S.dma("sp", lambda e, j=j: e.dma_start(out=ucb[:], in_=uTb[j * 128:(j + 1) * 128, :]), reads=[uTb_b], writes=[ucb_b])
                S.dma("sp", lambda e, j=j: e.dma_start(out=uc32[:], in_=uT32[j * 128:(j + 1) * 128, :]), reads=[uT32_b], writes=[uc32_b])
                S.op("act", lambda e, j=j: e.activation(out=ysum, in_=uc32[:], func=AF.Copy, scale=dcol[:, j:j + 1]),
                     reads=[uc32_b, dcol_b], writes=[ysum_b])
                for d in range(2):
                    col0 = d * G + 8 * j
                    gs = slice(8 * j, 8 * j + 8)
                    S.dma("sp", lambda e, d=d, gs=gs: e.dma_start(out=X1[0:64], in_=ssm_b_re[d, gs].rearrange("g p c -> p g c")), writes=[X1_b])
                    S.dma("sp", lambda e, d=d, gs=gs: e.dma_start(out=X1[64:128], in_=ssm_b_im[d, gs].rearrange("g p c -> p g c")), writes=[X1_b])
                    S.dma("sp", lambda e, d=d, gs=gs: e.dma_start(out=X2[0:64], in_=ssm_b_im[d, gs].rearrange("g p c -> p g c")), writes=[X2_b])
                    S.dma("sp", lambda e, d=d, gs=gs: e.dma_start(out=X2[64:128], in_=ssm_b_re[d, gs].rearrange("g p c -> p g c")), writes=[X2_b])
                    bc = lambda A, col0=col0: A[:, col0:col0 + 8, None].to_broadcast([128, 8, C])
                    tt(SB1[:], X1[:], bc(ZR), ALU.mult, [X1_b, ZR_b], [SB1_b])
                    tt(TM[:], X2[:], bc(ZIs), ALU.mult, [X2_b, ZIs_b], [TM_b])
                    S.op("dve", lambda e: e.tensor_tensor(out=SBb[:, 0, :].rearrange("p (g c) -> p g c", g=8), in0=SB1[:], in1=TM[:], op=ALU.add),
                         reads=[SB1_b, TM_b], writes=[SBb_b])
                    tt(SB2[:], X2[:], bc(ZRs), ALU.mult, [X2_b, ZRs_b], [SB2_b])
                    tt(TM[:], X1[:], bc(ZI), ALU.mult, [X1_b, ZI_b, SBb_b], [TM_b])
                    S.op("dve", lambda e: e.tensor_tensor(out=SBb[:, 1, :].rearrange("p (g c) -> p g c", g=8), in0=SB2[:], in1=TM[:], op=ALU.add),
                         reads=[SB2_b, TM_b], writes=[SBb_b])
                    S.dma("sp", lambda e, d=d, gs=gs: e.dma_start(out=CC[:, 0, 0:64], in_=ssm_c_re[d, gs].rearrange("g c p -> (g c) p")), writes=[CC_b])
                    S.dma("sp", lambda e, d=d, gs=gs: e.dma_start(out=CC[:, 0, 64:128], in_=ssm_c_im[d, gs].rearrange("g c p -> (g c) p")), writes=[CC_b])
                    S.dma("sp", lambda e, d=d, gs=gs: e.dma_start(out=CC[:, 1, 0:64], in_=ssm_c_im[d, gs].rearrange("g c p -> (g c) p")), writes=[CC_b])
                    S.dma("sp", lambda e, d=d, gs=gs: e.dma_start(out=CC[:, 1, 64:128], in_=ssm_c_re[d, gs].rearrange("g c p -> (g c) p")), writes=[CC_b])
                    S.op("dve", lambda e: e.tensor_copy(out=CCb[:], in_=CC[:]), reads=[CC_b], writes=[CCb_b])
                    for k_, (src, src_b) in enumerate(((SBb, SBb_b), (SBb, SBb_b), (CCb, CCb_b), (CCb, CCb_b))):
                        S.op("pe", lambda e, k_=k_, src=src: e.transpose(out=ptb[:, k_, :], in_=src[:, k_ % 2, :], identity=identb[:]),
                             reads=[src_b, identb_b], writes=[ptb_b])
                    S.op("act", lambda e: e.copy(out=PT4[:], in_=ptb[:]), reads=[ptb_b], writes=[PT4_b])
                    S.op("dve", lambda e: e.tensor_copy(out=L12[:], in_=PT4[:, 0:2, :]), reads=[PT4_b], writes=[L12_b])
                    S.op("dve", lambda e: e.tensor_scalar(out=LC[:, 0, :], in0=PT4[:, 2, :], scalar1=NSGN[:, 0:1], scalar2=None, op0=ALU.mult),
                         reads=[PT4_b, NSGN_b], writes=[LC_b])
                    S.op("dve", lambda e: e.tensor_scalar(out=LC[:, 1, :], in0=PT4[:, 3, :], scalar1=-1.0, scalar2=None, op0=ALU.mult),
                         reads=[PT4_b], writes=[LC_b])
                    for gl in range(8):
                        col = col0 + gl
                        Lg, Lg_b = Lg_p.get()
                        S.op("dve", lambda e, Lg=Lg, gl=gl: e.tensor_scalar(out=Lg[:, 0:2, :], in0=L12[:], scalar1=RM[:, gl:gl + 1], scalar2=None, op0=ALU.mult),
                             reads=[L12_b, RM_b], writes=[Lg_b])
                        S.op("dve", lambda e, Lg=Lg, gl=gl: e.tensor_tensor(out=Lg[:, 2:4, :], in0=LC[:], in1=CM[:, gl:gl + 1, :].to_broadcast([128, 2, 128]), op=ALU.mult),
                             reads=[LC_b, CM_b], writes=[Lg_b])
                        carry = None
                        segs = list(range(NSEG)) if d == 0 else list(range(NSEG - 1, -1, -1))
                        for si in segs:
                            s0 = si * SEG
                            ki, ki_b = ki_p.get()
                            r, r_b = r_p.get()
                            ab, ab_b = ab_p.get()
                            sn, sn_b = sn_p.get()
                            cs, cs_b = cs_p.get()
                            IN, IN_b = IN_p.get()
                            W, W_b = W_p.get()
                            A1, A1_b = A1_p.get()
                            A2, A2_b = A2_p.get()
                            fcol = FQ[:, col:col + 1]
                            S.op("act", lambda e, ki=ki, s0=s0, fcol=fcol: e.activation(out=ki[:], in_=tio[:, s0:s0 + SEG], func=AF.Copy, scale=fcol),
                                 reads=[tio_b, FQ_b], writes=[ki_b])
                            S.op("dve", lambda e, r=r, ki=ki, s0=s0, fcol=fcol: e.scalar_tensor_tensor(out=r[:], in0=tio[:, s0:s0 + SEG], scalar=fcol, in1=ki[:],
                                                                                              op0=ALU.mult, op1=ALU.subtract),
                                 reads=[tio_b, FQ_b, ki_b], writes=[r_b])
                            S.op("act", lambda e, ab=ab, r=r: e.activation(out=ab[:], in_=r[:], func=AF.Abs), reads=[r_b], writes=[ab_b])
                            S.op("act", lambda e, sn=sn, r=r: e.activation(out=sn[:], in_=r[:], func=AF.Sin, scale=TWO_PI), reads=[r_b], writes=[sn_b])
                            S.op("act", lambda e, cs=cs, ab=ab: e.activation(out=cs[:], in_=ab[:], func=AF.Sin, scale=-TWO_PI, bias=hpi[:, 0:1]),
                                 reads=[ab_b, hpi_b], writes=[cs_b])
                            for b0 in range(0, SEG, BW):
                                pM1, pM1_b = pM_p.get()
                                pM2, pM2_b = pM_p.get()
                                t1, t1_b = t1_p.get()
                                S.op("pe", lambda e, pM1=pM1, Lg=Lg, s0=s0, b0=b0: e.matmul(pM1[:, 0:BW], lhsT=Lg[:, 0, :], rhs=ucb[:, s0 + b0:s0 + b0 + BW],
                                                                                         start=True, stop=True), reads=[Lg_b, ucb_b], writes=[pM1_b])
                                S.op("pe", lambda e, pM2=pM2, Lg=Lg, s0=s0, b0=b0: e.matmul(pM2[:, 0:BW], lhsT=Lg[:, 1, :], rhs=ucb[:, s0 + b0:s0 + b0 + BW],
                                                                                         start=True, stop=True), reads=[Lg_b, ucb_b], writes=[pM2_b])
                                S.op("dve", lambda e, t1=t1, cs=cs, pM1=pM1, b0=b0: e.tensor_tensor(out=t1[:, 0:BW], in0=pM1[:, 0:BW], in1=cs[:, b0:b0 + BW], op=ALU.mult),
                                     reads=[pM1_b, cs_b], writes=[t1_b])
                                S.op("dve", lambda e, IN=IN, sn=sn, pM2=pM2, b0=b0: e.tensor_tensor(out=IN[:, b0:b0 + BW], in0=pM2[:, 0:BW], in1=sn[:, b0:b0 + BW], op=ALU.mult),
                                     reads=[pM2_b, sn_b], writes=[IN_b])
                                S.op("dve", lambda e, IN=IN, t1=t1, b0=b0: e.tensor_tensor(out=IN[:, b0:b0 + BW], in0=IN[:, b0:b0 + BW], in1=t1[:, 0:BW], op=ALU.add),
                                     reads=[IN_b, t1_b], writes=[IN_b])
                            rho = MAG[:, col:col + 1].to_broadcast([128, SEG])
                            init = 0.0 if carry is None else carry[0][:, 0:1]
                            rdeps = [MAG_b, IN_b] + ([carry[1]] if carry is not None else [])
                            if d == 0:
                                S.op("dve", lambda e, W=W, IN=IN, rho=rho, init=init: e.tensor_tensor_scan(out=W[:], data0=rho, data1=IN[:], initial=init,
                                                                                                   op0=ALU.mult, op1=ALU.add), reads=rdeps, writes=[W_b])
                                last = SEG - 1
                            else:
                                S.op("dve", lambda e, W=W, IN=IN, rho=rho, init=init: e.tensor_tensor_scan(out=W[:, ::-1], data0=rho, data1=IN[:, ::-1], initial=init,
                                                                                                   op0=ALU.mult, op1=ALU.add), reads=rdeps, writes=[W_b])
                                last = 0
                            if len(segs) > 1:
                                cw, cw_b = cw_p.get()
                                S.op("act", lambda e, cw=cw, W=W, last=last: e.copy(out=cw[:], in_=W[:, last:last + 1]), reads=[W_b], writes=[cw_b])
                                carry = (cw, cw_b)
                            S.op("pool", lambda e, A1=A1, cs=cs, W=W: e.tensor_tensor(out=A1[:], in0=cs[:], in1=W[:], op=ALU.mult), reads=[cs_b, W_b], writes=[A1_b])
                            S.op("pool", lambda e, A2=A2, sn=sn, W=W: e.tensor_tensor(out=A2[:], in0=sn[:], in1=W[:], op=ALU.mult), reads=[sn_b, W_b], writes=[A2_b])
                            for b0 in range(0, SEG, BW):
                                pY, pY_b = pY_p.get()
                                S.op("pe", lambda e, pY=pY, Lg=Lg, A1=A1, b0=b0: e.matmul(pY[:, 0:BW], lhsT=Lg[:, 2, :], rhs=A1[:, b0:b0 + BW], start=True, stop=False),
                                     reads=[Lg_b, A1_b], writes=[pY_b])
                                S.op("pe", lambda e, pY=pY, Lg=Lg, A2=A2, b0=b0: e.matmul(pY[:, 0:BW], lhsT=Lg[:, 3, :], rhs=A2[:, b0:b0 + BW], start=False, stop=True),
                                     reads=[Lg_b, A2_b], writes=[pY_b])
                                S.op("dve", lambda e, pY=pY, s0=s0, b0=b0: e.tensor_tensor(out=ysum[:, s0 + b0:s0 + b0 + BW], in0=pY[:, 0:BW],
                                                                                       in1=ysum[:, s0 + b0:s0 + b0 + BW], op=ALU.add),
                                     reads=[pY_b, ysum_b], writes=[ysum_b])
                tt(ga[:], ysum[:, OWN:T], ysum[:, 0:OWN], ALU.subtract, [ysum_b], [ga_b])
                S.op("dve", lambda e: e.scalar_tensor_tensor(out=yo[:], in0=ga[:], scalar=hf[:, 0:1], in1=ysum[:, 0:OWN], op0=ALU.mult, op1=ALU.add),
                     reads=[ga_b, hf_b, ysum_b], writes=[yo_b])
                tt(ga[:], yo[:], yo[:], ALU.mult, [yo_b], [ga_b])
                S.op("dve", lambda e: e.tensor_scalar(out=ga[:], in0=ga[:], scalar1=0.044715, scalar2=1.0, op0=ALU.mult, op1=ALU.add), reads=[ga_b], writes=[ga_b])
                tt(ga[:], ga[:], yo[:], ALU.mult, [ga_b, yo_b], [ga_b])
                S.op("act", lambda e: e.activation(out=gb[:], in_=ga[:], func=AF.Sigmoid, scale=2.0 * math.sqrt(2.0 / math.pi)), reads=[ga_b], writes=[gb_b])
                tt(gb[:], gb[:], yo[:], ALU.mult, [gb_b, yo_b], [gb_b])
                S.op("act", lambda e: e.copy(out=gbf[:], in_=gb[:]), reads=[gb_b], writes=[gbf_b])
                S.dma("sp", lambda e, j=j: e.dma_start(out=gT32[j * 128:(j + 1) * 128, :], in_=gb[:]), reads=[gb_b], writes=[gT32_b])
                S.dma("sp", lambda e, j=j: e.dma_start(out=gTb[j * 128:(j + 1) * 128, :], in_=gbf[:]), reads=[gbf_b], writes=[gTb_b])

        if debug == "D1a":
            return nc
        with Phase(nc, S) as ph:
            cx = ph.cx
            wg, wg_b = cx.sb([128, SC, DSSM], BF16, "wg")
            S.dma("sp", lambda e: e.dma_start(out=wg[:], in_=w_glu_bf.rearrange("(c p) n -> p c n", p=128)), reads=[w_glu_bf_b], writes=[wg_b])
            bcol, bcol_b = cx.sb([128, SC], F32, "bcol")
            S.dma("sp", lambda e: e.dma_start(out=bcol[:], in_=ssm_b_glu.rearrange("o (c p) -> p (o c)", p=128), allow_slow_non_contiguous=True),
                  writes=[bcol_b])
            GQ = min(512, OWN)
            gbl_p = RR([cx.sb([128, SC, GQ], BF16, "gbl") for _ in range(2)])
            g32_p = RR([cx.sb([128, SC, GQ], F32, "g32") for _ in range(1)])
            pg_p = RR([cx.ps([128, GQ], F32, "pg") for _ in range(3)])
            sg_p = RR([cx.sb([128, GQ], F32, "sg") for _ in range(3)])
            for q0 in range(0, OWN, GQ):
                gbl, gbl_b = gbl_p.get()
                g32, g32_b = g32_p.get()
                S.dma("sp", lambda e, gbl=gbl, q0=q0: e.dma_start(out=gbl[:], in_=gTb[:, q0:q0 + GQ].rearrange("(c p) t -> p c t", p=128)),
                      reads=[gTb_b], writes=[gbl_b])
                S.dma("sp", lambda e, g32=g32, q0=q0: e.dma_start(out=g32[:], in_=gT32[:, q0:q0 + GQ].rearrange("(c p) t -> p c t", p=128)),
                      reads=[gT32_b], writes=[g32_b])
                for m in range(SC):
                    pg, pg_b = pg_p.get()
                    sg, sg_b = sg_p.get()
                    for c in range(SC):
                        S.op("pe", lambda e, pg=pg, gbl=gbl, c=c, m=m: e.matmul(pg[:], lhsT=wg[:, c, m * 128:(m + 1) * 128], rhs=gbl[:, c, :],
                                                                              start=(c == 0), stop=(c == SC - 1)), reads=[wg_b, gbl_b], writes=[pg_b])
                    S.op("act", lambda e, sg=sg, pg=pg, m=m: e.activation(out=sg[:], in_=pg[:], func=AF.Sigmoid, bias=bcol[:, m:m + 1]),
                         reads=[pg_b, bcol_b], writes=[sg_b])
                    S.op("dve", lambda e, sg=sg, g32=g32, m=m: e.tensor_tensor(out=sg[:], in0=sg[:], in1=g32[:, m, :], op=ALU.mult),
                         reads=[sg_b, g32_b], writes=[sg_b])
                    S.dma("sp", lambda e, sg=sg, m=m, q0=q0: e.dma_start(out=mixT[DATT + m * 128:DATT + (m + 1) * 128, q0:q0 + GQ], in_=sg[:]),
                          reads=[sg_b], writes=[mixT_b])
        if debug == "D":
            return nc

        MC = DMIX // 128
        AC = DATT // 128
        QB = min(256, OWN)
        with Phase(nc, S) as ph:
            cx = ph.cx
            ones, ones_b = cx.sb([128, 128], BF16, "ones")
            S.op("pool", lambda e: e.memset(ones[:], 1.0), writes=[ones_b])
            gcol, gcol_b = cx.sb([128, MC], F32, "gcol")
            S.dma("sp", lambda e: e.dma_start(out=gcol[:, 0:AC], in_=attn_out_norm_g.rearrange("o (c p) -> p (o c)", p=128),
                                              allow_slow_non_contiguous=True), writes=[gcol_b])
            S.dma("sp", lambda e: e.dma_start(out=gcol[:, AC:MC], in_=ssm_out_norm_g.rearrange("o (c p) -> p (o c)", p=128),
                                              allow_slow_non_contiguous=True), writes=[gcol_b])
            mx, mx_b = cx.sb([128, MC, QB], F32, "mx")
            sqb, sqb_b = cx.sb([128, MC, QB], BF16, "sqb")
            mn, mn_b = cx.sb([128, MC, QB], BF16, "mn")
            rs_a, rs_a_b = cx.sb([128, QB], F32, "rs_a")
            rs_s, rs_s_b = cx.sb([128, QB], F32, "rs_s")
            psn_p = RR([cx.ps([128, QB], F32, "psn") for _ in range(2)])
            pm_p = RR([cx.ps([128, 512], F32, "pm") for _ in range(3)])
            wo_p = RR([cx.sb([128, MC, 512], BF16, "wo") for _ in range(2)])
            xr_p = RR([cx.sb([128, 512], F32, "xr") for _ in range(3)])
            ho_p = RR([cx.sb([128, 512], F32, "ho") for _ in range(3)])
            for qb in range(OWN // QB):
                q0 = qb * QB
                S.dma("sp", lambda e, q0=q0: e.dma_start(out=mx[:], in_=mixT[:, q0:q0 + QB].rearrange("(c p) t -> p c t", p=128)),
                      reads=[mixT_b], writes=[mx_b])
                S.op("act", lambda e: e.activation(out=sqb[:], in_=mx[:], func=AF.Square), reads=[mx_b], writes=[sqb_b])
                for (lo, hi, rs, rs_b, n) in ((0, AC, rs_a, rs_a_b, DATT), (AC, MC, rs_s, rs_s_b, DSSM)):
                    psn, psn_b = psn_p.get()
                    for c in range(lo, hi):
                        S.op("pe", lambda e, psn=psn, c=c, lo=lo, hi=hi: e.matmul(psn[:], lhsT=ones[:], rhs=sqb[:, c, :],
                                                                                start=(c == lo), stop=(c == hi - 1)),
                             reads=[ones_b, sqb_b], writes=[psn_b])
                    S.op("dve", lambda e, psn=psn, rs=rs: e.tensor_copy(out=rs[:], in_=psn[:]), reads=[psn_b], writes=[rs_b])
                    ph.rstd(rs[:], rs_b, n, EPS)
                    for c in range(lo, hi):
                        S.op("dve", lambda e, c=c, rs=rs: e.scalar_tensor_tensor(out=mn[:, c, :], in0=mx[:, c, :], scalar=gcol[:, c:c + 1],
                                                                               in1=rs[:], op0=ALU.mult, op1=ALU.mult),
                             reads=[mx_b, gcol_b, rs_b], writes=[mn_b])
                for c0 in range(0, D, 512):
                    wo, wo_b = wo_p.get()
                    S.dma("sp", lambda e, wo=wo, c0=c0: e.dma_start(out=wo[:], in_=w_out_bf[:, c0:c0 + 512].rearrange("(c p) n -> p c n", p=128)),
                          reads=[w_out_bf_b], writes=[wo_b])
                    for ti in range(QB // 128):
                        t0 = q0 + ti * 128
                        pm, pm_b = pm_p.get()
                        for c in range(MC):
                            S.op("pe", lambda e, pm=pm, wo=wo, c=c, ti=ti: e.matmul(pm[:], lhsT=mn[:, c, ti * 128:(ti + 1) * 128], rhs=wo[:, c, :],
                                                                                  start=(c == 0), stop=(c == MC - 1)),
                                 reads=[mn_b, wo_b], writes=[pm_b])
                        xr, xr_b = xr_p.get()
                        ho, ho_b = ho_p.get()
                        S.dma("sp", lambda e, xr=xr, t0=t0, c0=c0: e.dma_start(out=xr[:], in_=own_x[t0:t0 + 128, c0:c0 + 512]), writes=[xr_b])
                        S.op("dve", lambda e, ho=ho, pm=pm, xr=xr: e.tensor_tensor(out=ho[:], in0=pm[:], in1=xr[:], op=ALU.add),
                             reads=[pm_b, xr_b], writes=[ho_b])
                        S.dma("sp", lambda e, ho=ho, t0=t0, c0=c0: e.dma_start(out=h1[t0:t0 + 128, c0:c0 + 512], in_=ho[:]),
                              reads=[ho_b], writes=[h1_b])
        if debug == "E":
            return nc

        BIG = float(1 << 22)
        _bc = {}

        def bchk(e):
            if "r" not in _bc:
                _bc["r"] = e.to_reg(NSLOT - 1)
            return _bc["r"]
        with Phase(nc, S) as ph:
            cx = ph.cx
            identb, identb_b = ph.identity()
            idf, idf_b = ph.identity(F32)
            g2, g2_b = ph.bcast_load(ffn_norm_g[0:1, :], D, "g2")
            br, br_b = ph.bcast_load(b_router[0:1, :], E, "br")
            wr, wr_b = cx.sb([128, KC, E], F32, "wr")
            S.dma("sp", lambda e: e.dma_start(out=wr[:], in_=w_router.rearrange("(c p) n -> p c n", p=128)), writes=[wr_b])
            ones, ones_b = cx.sb([128, 128], BF16, "ones")
            S.op("pool", lambda e: e.memset(ones[:], 1.0), writes=[ones_b])
            uf, uf_b = cx.sb([128, 128], F32, "uf")
            U, U_b = cx.sb([128, 128], BF16, "U")
            S.op("pool", lambda e: e.memset(uf[:], 1.0), writes=[uf_b])
            S.op("pool", lambda e: e.affine_select(out=uf[:], in_=uf[:], pattern=[[1, 128]], compare_op=ALU.is_gt, fill=0.0,
                                                   base=0, channel_multiplier=-1), reads=[uf_b], writes=[uf_b])
            S.op("dve", lambda e: e.tensor_copy(out=U[:], in_=uf[:]), reads=[uf_b], writes=[U_b])
            ebase_i, ebase_i_b = cx.sb([128, E], I32, "ebase_i")
            ebase, ebase_b = cx.sb([128, E], F32, "ebase")
            S.op("pool", lambda e: e.iota(out=ebase_i[:], pattern=[[CAP, E]], base=0, channel_multiplier=0), writes=[ebase_i_b])
            S.op("dve", lambda e: e.tensor_copy(out=ebase[:], in_=ebase_i[:]), reads=[ebase_i_b], writes=[ebase_b])
            cnt, cnt_b = cx.sb([128, E], F32, "cnt")
            S.op("pool", lambda e: e.memset(cnt[:], 0.0), writes=[cnt_b])
            ht_p = RR([cx.sb([128, D], F32, "ht") for _ in range(2)])
            scr, scr_b = cx.sb([128, D], BF16, "scr")
            ss, ss_b = cx.sb([128, 1], F32, "ss")
            hn32, hn32_b = cx.sb([128, D], F32, "hn32")
            hnb_p = RR([cx.sb([128, D], BF16, "hnb") for _ in range(2)])
            hT, hT_b = cx.sb([128, KC, 128], F32, "hT")
            ptf_p = RR([cx.ps([128, 4, 128], F32, "ptf") for _ in range(2)])
            plg, plg_b = cx.ps([128, E], F32, "plg")
            ppos, ppos_b = cx.ps([128, E], F32, "ppos")
            pcs, pcs_b = cx.ps([128, E], F32, "pcs")
            lg, lg_b = cx.sb([128, E], F32, "lg")
            mx8, mx8_b = cx.sb([128, 8], F32, "mx8")
            nmx, nmx_b = cx.sb([128, 1], F32, "nmx")
            mask, mask_b = cx.sb([128, E], F32, "mask")
            maskb, maskb_b = cx.sb([128, E], BF16, "maskb")
            ex, ex_b = cx.sb([128, E], F32, "ex")
            den, den_b = cx.sb([128, 1], F32, "den")
            gate, gate_b = cx.sb([128, E], F32, "gate")
            pos, pos_b = cx.sb([128, E], F32, "pos")
            ovf, ovf_b = cx.sb([128, E], F32, "ovf")
            sidx, sidx_b = cx.sb([128, E], F32, "sidx")
            oh, oh_b = cx.sb([128, E], F32, "oh")
            tmpe, tmpe_b = cx.sb([128, E], F32, "tmpe")
            idxf, idxf_b = cx.sb([128, 4], F32, "idxf")
            idxi_p = RR([cx.sb([128, 4], I32, "idxi") for _ in range(2)])
            gk_p = RR([cx.sb([128, 4], F32, "gk") for _ in range(2)])
            xdw = Buf("xdw")

            def tt(out, a, b, op, rb, wb, eng="dve"):
                S.op(eng, lambda e: e.tensor_tensor(out=out, in0=a, in1=b, op=op), reads=rb, writes=wb)

            for ti in range(OWN // 128):
                t0 = ti * 128
                ht, ht_b = ht_p.get()
                hnb, hnb_b = hnb_p.get()
                idxi, idxi_b = idxi_p.get()
                gk, gk_b = gk_p.get()
                S.dma("sp", lambda e, ht=ht, t0=t0: e.dma_start(out=ht[:], in_=h1[t0:t0 + 128, :]), reads=[h1_b], writes=[ht_b])
                ph.rmsnorm(ht[:], ht_b, D, g2[:], g2_b, hn32[:], hn32_b, scr[:], scr_b, ss[:], ss_b, EPS)
                S.op("act", lambda e, hnb=hnb: e.copy(out=hnb[:], in_=hn32[:]), reads=[hn32_b], writes=[hnb_b])
                for c0 in range(0, KC, 4):
                    ptf, ptf_b = ptf_p.get()
                    for c in range(4):
                        S.op("pe", lambda e, ptf=ptf, c=c, c0=c0: e.transpose(out=ptf[:, c, :], in_=hn32[:, (c0 + c) * 128:(c0 + c + 1) * 128],
                                                                          identity=idf[:]), reads=[hn32_b, idf_b], writes=[ptf_b])
                    S.op("act", lambda e, ptf=ptf, c0=c0: e.copy(out=hT[:, c0:c0 + 4, :], in_=ptf[:]), reads=[ptf_b], writes=[hT_b])
                for c in range(KC):
                    S.op("pe", lambda e, c=c: e.matmul(plg[:], lhsT=hT[:, c, :], rhs=wr[:, c, :], start=(c == 0), stop=(c == KC - 1)),
                         reads=[hT_b, wr_b], writes=[plg_b])
                tt(lg[:], plg[:], br[:], ALU.add, [plg_b, br_b], [lg_b])
                S.op("dve", lambda e: e.max(out=mx8[:], in_=lg[:]), reads=[lg_b], writes=[mx8_b])
                S.op("dve", lambda e: e.tensor_scalar(out=mask[:], in0=lg[:], scalar1=mx8[:, 3:4], scalar2=None, op0=ALU.is_ge),
                     reads=[lg_b, mx8_b], writes=[mask_b])
                S.op("dve", lambda e: e.tensor_copy(out=maskb[:], in_=mask[:]), reads=[mask_b], writes=[maskb_b])
                S.op("dve", lambda e: e.tensor_scalar(out=nmx[:], in0=mx8[:, 0:1], scalar1=-1.0, scalar2=None, op0=ALU.mult),
                     reads=[mx8_b], writes=[nmx_b])
                S.op("act", lambda e: e.activation(out=ex[:], in_=lg[:], func=AF.Exp, bias=nmx[:, 0:1]), reads=[lg_b, nmx_b], writes=[ex_b])
                tt(ex[:], ex[:], mask[:], ALU.mult, [ex_b, mask_b], [ex_b])
                S.op("dve", lambda e: e.reduce_sum(out=den[:], in_=ex[:], axis=AX.X), reads=[ex_b], writes=[den_b])
                S.op("dve", lambda e: e.reciprocal(out=den[:], in_=den[:]), reads=[den_b], writes=[den_b])
                S.op("dve", lambda e: e.tensor_scalar(out=gate[:], in0=ex[:], scalar1=den[:, 0:1], scalar2=None, op0=ALU.mult),
                     reads=[ex_b, den_b], writes=[gate_b])
                S.op("pe", lambda e: e.matmul(ppos[:], lhsT=U[:], rhs=maskb[:], start=True, stop=True), reads=[U_b, maskb_b], writes=[ppos_b])
                S.op("pe", lambda e: e.matmul(pcs[:], lhsT=ones[:], rhs=maskb[:], start=True, stop=True), reads=[ones_b, maskb_b], writes=[pcs_b])
                tt(pos[:], ppos[:], cnt[:], ALU.add, [ppos_b, cnt_b], [pos_b])
                tt(cnt[:], pcs[:], cnt[:], ALU.add, [pcs_b, cnt_b, pos_b], [cnt_b])
                S.op("dve", lambda e: e.tensor_scalar(out=ovf[:], in0=pos[:], scalar1=float(CAP), scalar2=BIG, op0=ALU.is_ge, op1=ALU.mult),
                     reads=[pos_b], writes=[ovf_b])
                tt(sidx[:], pos[:], ebase[:], ALU.add, [pos_b, ebase_b], [sidx_b])
                tt(sidx[:], sidx[:], ovf[:], ALU.add, [sidx_b, ovf_b], [sidx_b])
                S.op("dve", lambda e: e.tensor_scalar(out=ovf[:], in0=ovf[:], scalar1=0.0, scalar2=None, op0=ALU.is_equal), reads=[ovf_b], writes=[ovf_b])
                tt(gate[:], gate[:], ovf[:], ALU.mult, [gate_b, ovf_b], [gate_b])
                for k in range(4):
                    S.op("dve", lambda e, k=k: e.tensor_scalar(out=oh[:], in0=lg[:], scalar1=mx8[:, k:k + 1], scalar2=None, op0=ALU.is_equal),
                         reads=[lg_b, mx8_b], writes=[oh_b])
                    tt(tmpe[:], oh[:], sidx[:], ALU.mult, [oh_b, sidx_b], [tmpe_b])
                    S.op("dve", lambda e, k=k: e.reduce_sum(out=idxf[:, k:k + 1], in_=tmpe[:], axis=AX.X), reads=[tmpe_b], writes=[idxf_b])
                    tt(tmpe[:], oh[:], gate[:], ALU.mult, [oh_b, gate_b, idxf_b], [tmpe_b])
                    S.op("dve", lambda e, k=k, gk=gk: e.reduce_sum(out=gk[:, k:k + 1], in_=tmpe[:], axis=AX.X), reads=[tmpe_b], writes=[gk_b])
                S.op("dve", lambda e, idxi=idxi: e.tensor_copy(out=idxi[:], in_=idxf[:]), reads=[idxf_b], writes=[idxi_b])
                S.dma("sp", lambda e, idxi=idxi, t0=t0: e.dma_start(out=idxd[t0:t0 + 128, :], in_=idxi[:]), reads=[idxi_b], writes=[idxd_b])
                S.dma("sp", lambda e, gk=gk, t0=t0: e.dma_start(out=gated[t0:t0 + 128, :], in_=gk[:]), reads=[gk_b], writes=[gated_b])
                for k in range(4):
                    S.dma("pool", lambda e, hnb=hnb, idxi=idxi, k=k: e.indirect_dma_start(
                        out=Xd, out_offset=bass.IndirectOffsetOnAxis(ap=idxi[:, k:k + 1], axis=0), in_=hnb[:], in_offset=None,
                        bounds_check=bchk(e), oob_is_err=False), reads=[hnb_b, idxi_b], writes=[Xd_b])
                if ti == OWN // 128 - 1:
                    S.dma("sp", lambda e: e.dma_start(out=cnt_out, in_=cnt[:]), reads=[cnt_b], writes=[Buf()])
        if debug == "F1":
            return nc

        NST = CAP // 128
        FC = DE // 128
        WB = 128
        DW = min(256, D)
        with Phase(nc, S) as ph:
            cx = ph.cx
            identb, identb_b = ph.identity()
            xe, xe_b = cx.sb([128, D], BF16, "xe")
            xT, xT_b = cx.sb([128, KC, CAP], BF16, "xT")
            HW_ = 384 if CAP % 384 == 0 else CAP
            NH = CAP // HW_
            pts = [cx.ps([128, 8, 128], BF16, "ptr") for _ in range(2)]
            stg_g, stg_g_b = cx.sb([128, KC, WB], F32, "stg_g")
            stg_u, stg_u_b = cx.sb([128, KC, WB], F32, "stg_u")
            stg_d, stg_d_b = cx.sb([128, FC, DW], F32, "stg_d")
            wg_p = RR([cx.sb([128, KC, WB], BF16, "wg") for _ in range(2)])
            wu_p = RR([cx.sb([128, KC, WB], BF16, "wu") for _ in range(2)])
            wd_p = RR([cx.sb([128, FC, DW], BF16, "wd") for _ in range(2)])
            bgc, bgc_b = cx.sb([128, FC], F32, "bgc")
            buc, buc_b = cx.sb([128, FC], F32, "buc")
            bdr_p = RR([cx.sb([128, DW], F32, "bdr") for _ in range(2)])
            actT, actT_b = cx.sb([128, FC, CAP], BF16, "actT")
            pg_p = RR([cx.ps([128, HW_], F32, "pg") for _ in range(2)])
            pu_p = RR([cx.ps([128, HW_], F32, "pu") for _ in range(2)])
            py_p = RR([cx.ps([128, DW], F32, "py") for _ in range(2)])
            ac_p = RR([cx.sb([128, HW_], F32, "ac") for _ in range(2)])
            sg_p = RR([cx.sb([128, HW_], F32, "sg") for _ in range(2)])
            lc_p = RR([cx.sb([128, HW_], F32, "lc") for _ in range(2)])
            yo_p = RR([cx.sb([128, DW], BF16, "yo") for _ in range(3)])
            flip = [0]
            for ex_ in range(E):
                S.dma("sp", lambda e, ex_=ex_: e.dma_start(out=bgc[:], in_=b_gate[ex_:ex_ + 1, :].rearrange("o (c p) -> p (o c)", p=128),
                                                          allow_slow_non_contiguous=True), writes=[bgc_b])
                S.dma("sp", lambda e, ex_=ex_: e.dma_start(out=buc[:], in_=b_up[ex_:ex_ + 1, :].rearrange("o (c p) -> p (o c)", p=128),
                                                          allow_slow_non_contiguous=True), writes=[buc_b])
                for st in range(NST):
                    S.dma("sp", lambda e, ex_=ex_, st=st: e.dma_start(out=xe[:], in_=Xd[ex_ * CAP + st * 128:ex_ * CAP + (st + 1) * 128, :]),
                          reads=[Xd_b], writes=[xe_b])
                    ph.transpose_chunks(xe, xe_b, KC, xT[:, :, st * 128:(st + 1) * 128], xT_b, identb, identb_b, pts)
                for f0 in range(0, DE, WB):
                    wg, wg_b = wg_p.get()
                    wu, wu_b = wu_p.get()
                    S.dma("sp", lambda e, ex_=ex_, f0=f0: e.dma_start(
                        out=stg_g[:], in_=w_gate[ex_ * D:(ex_ + 1) * D, f0:f0 + WB].rearrange("(c p) n -> p c n", p=128)), writes=[stg_g_b])
                    S.op("act", lambda e, wg=wg: e.copy(out=wg[:], in_=stg_g[:]), reads=[stg_g_b], writes=[wg_b])
                    S.dma("sp", lambda e, ex_=ex_, f0=f0: e.dma_start(
                        out=stg_u[:], in_=w_up[ex_ * D:(ex_ + 1) * D, f0:f0 + WB].rearrange("(c p) n -> p c n", p=128)), writes=[stg_u_b])
                    S.op("pool", lambda e, wu=wu: e.tensor_copy(out=wu[:], in_=stg_u[:]), reads=[stg_u_b], writes=[wu_b])
                    for mm in range(WB // 128):
                      m = f0 // 128 + mm
                      for hh in range(NH):
                        h0 = hh * HW_
                        pg, pg_b = pg_p.get()
                        pu, pu_b = pu_p.get()
                        for c in range(KC):
                            S.op("pe", lambda e, pg=pg, wg=wg, c=c, mm=mm, h0=h0: e.matmul(pg[:], lhsT=wg[:, c, mm * 128:(mm + 1) * 128], rhs=xT[:, c, h0:h0 + HW_],
                                                                                 start=(c == 0), stop=(c == KC - 1)), reads=[wg_b, xT_b], writes=[pg_b])
                        for c in range(KC):
                            S.op("pe", lambda e, pu=pu, wu=wu, c=c, mm=mm, h0=h0: e.matmul(pu[:], lhsT=wu[:, c, mm * 128:(mm + 1) * 128], rhs=xT[:, c, h0:h0 + HW_],
                                                                                 start=(c == 0), stop=(c == KC - 1)), reads=[wu_b, xT_b], writes=[pu_b])
                        ac, ac_b = ac_p.get()
                        sg, sg_b = sg_p.get()
                        lc, lc_b = lc_p.get()
                        S.op("dve", lambda e, ac=ac, pg=pg, m=m: e.tensor_scalar(out=ac[:], in0=pg[:], scalar1=bgc[:, m:m + 1], scalar2=cfg["LIMIT"],
                                                                                op0=ALU.add, op1=ALU.min), reads=[pg_b, bgc_b], writes=[ac_b])
                        S.op("act", lambda e, sg=sg, ac=ac: e.activation(out=sg[:], in_=ac[:], func=AF.Sigmoid, scale=cfg["ALPHA"]), reads=[ac_b], writes=[sg_b])
                        S.op("dve", lambda e, lc=lc, pu=pu, m=m: e.tensor_scalar(out=lc[:], in0=pu[:], scalar1=buc[:, m:m + 1], scalar2=cfg["LIMIT"],
                                                                                op0=ALU.add, op1=ALU.min), reads=[pu_b, buc_b], writes=[lc_b])
                        S.op("dve", lambda e, lc=lc: e.tensor_scalar(out=lc[:], in0=lc[:], scalar1=-cfg["LIMIT"], scalar2=1.0, op0=ALU.max, op1=ALU.add),
                             reads=[lc_b], writes=[lc_b])
                        S.op("dve", lambda e, sg=sg, ac=ac: e.tensor_tensor(out=sg[:], in0=sg[:], in1=ac[:], op=ALU.mult), reads=[sg_b, ac_b], writes=[sg_b])
                        S.op("dve", lambda e, sg=sg, lc=lc, m=m, h0=h0: e.tensor_tensor(out=actT[:, m, h0:h0 + HW_], in0=sg[:], in1=lc[:], op=ALU.mult),
                             reads=[sg_b, lc_b], writes=[actT_b])
                for d0 in range(0, D, DW):
                    wd, wd_b = wd_p.get()
                    S.dma("sp", lambda e, ex_=ex_, d0=d0: e.dma_start(
                        out=stg_d[:], in_=w_down[ex_ * DE:(ex_ + 1) * DE, d0:d0 + DW].rearrange("(c p) n -> p c n", p=128)), writes=[stg_d_b])
                    flip[0] ^= 1
                    if flip[0]:
                        S.op("act", lambda e, wd=wd: e.copy(out=wd[:], in_=stg_d[:]), reads=[stg_d_b], writes=[wd_b])
                    else:
                        S.op("pool", lambda e, wd=wd: e.tensor_copy(out=wd[:], in_=stg_d[:]), reads=[stg_d_b], writes=[wd_b])
                    bdr, bdr_b = bdr_p.get()
                    S.dma("sp", lambda e, bdr=bdr, ex_=ex_, d0=d0: e.dma_start(out=bdr[:], in_=b_down[ex_:ex_ + 1, d0:d0 + DW].to_broadcast([128, DW])),
                          writes=[bdr_b])
                    for st in range(NST):
                        py, py_b = py_p.get()
                        for fc in range(FC):
                            S.op("pe", lambda e, py=py, wd=wd, fc=fc, st=st: e.matmul(py[:], lhsT=actT[:, fc, st * 128:(st + 1) * 128], rhs=wd[:, fc, :],
                                                                                    start=(fc == 0), stop=(fc == FC - 1)), reads=[actT_b, wd_b], writes=[py_b])
                        yo, yo_b = yo_p.get()
                        S.op("dve", lambda e, yo=yo, py=py, bdr=bdr: e.tensor_tensor(out=yo[:], in0=py[:], in1=bdr[:], op=ALU.add),
                             reads=[py_b, bdr_b], writes=[yo_b])
                        S.dma("sp", lambda e, yo=yo, ex_=ex_, st=st, d0=d0: e.dma_start(
                            out=Yd[ex_ * CAP + st * 128:ex_ * CAP + (st + 1) * 128, d0:d0 + DW], in_=yo[:]), reads=[yo_b], writes=[Yd_b])

        with Phase(nc, S) as ph:
            cx = ph.cx
            acc_p = RR([cx.sb([128, D], F32, "acc") for _ in range(2)])
            yk_p = RR([cx.sb([128, D], BF16, "yk") for _ in range(3)])
            ii_p = RR([cx.sb([128, 4], I32, "ii") for _ in range(2)])
            gg_p = RR([cx.sb([128, 4], F32, "gg") for _ in range(2)])
            outb = Buf("out")
            for ti in range(OWN // 128):
                t0 = ti * 128
                acc, acc_b = acc_p.get()
                ii, ii_b = ii_p.get()
                gg, gg_b = gg_p.get()
                S.dma("sp", lambda e, acc=acc, t0=t0: e.dma_start(out=acc[:], in_=h1[t0:t0 + 128, :]), reads=[h1_b], writes=[acc_b])
                S.dma("sp", lambda e, ii=ii, t0=t0: e.dma_start(out=ii[:], in_=idxd[t0:t0 + 128, :]), reads=[idxd_b], writes=[ii_b])
                S.dma("sp", lambda e, gg=gg, t0=t0: e.dma_start(out=gg[:], in_=gated[t0:t0 + 128, :]), reads=[gated_b], writes=[gg_b])
                for k in range(4):
                    yk, yk_b = yk_p.get()
                    S.op("pool", lambda e, yk=yk: e.memset(yk[:], 0.0), writes=[yk_b])
                    S.dma("pool", lambda e, yk=yk, ii=ii, k=k: e.indirect_dma_start(
                        out=yk[:], out_offset=None, in_=Yd, in_offset=bass.IndirectOffsetOnAxis(ap=ii[:, k:k + 1], axis=0),
                        bounds_check=bchk(e), oob_is_err=False), reads=[Yd_b, ii_b], writes=[yk_b])
                    S.op("dve", lambda e, acc=acc, yk=yk, gg=gg, k=k: e.scalar_tensor_tensor(out=acc[:], in0=yk[:], scalar=gg[:, k:k + 1], in1=acc[:],
                                                                                         op0=ALU.mult, op1=ALU.add), reads=[yk_b, gg_b, acc_b], writes=[acc_b])
                S.dma("sp", lambda e, acc=acc, t0=t0: e.dma_start(out=out[t0:t0 + 128, :], in_=acc[:]), reads=[acc_b], writes=[outb])
    return nc


def make_in_maps(cfg, inp):
    SEQ, D = cfg["SEQ"], cfg["D"]
    OWN = SEQ // 2
    HALF = cfg["ROPE"] // 2
    invf = (cfg["THETA"] ** (-np.arange(HALF, dtype=np.float32) / HALF)).astype(np.float32).reshape(1, HALF)
    maps = []
    f = lambda a: np.ascontiguousarray(a)
    for core in range(N_CORES):
        b, half = core // 2, core % 2
        sl = slice(half * OWN, (half + 1) * OWN)
        m = {
            "x": f(inp["x"][b]),
            "pos": f(inp["positions"][b].reshape(SEQ, 1).astype(np.int32)),
            "half_sel": np.full((1, 1), half, np.int32),
            "invf": invf,
            "own_x": f(inp["x"][b, sl]),
            "own_pos": f(inp["positions"][b, sl].reshape(OWN, 1).astype(np.int32)),
        }
        m["halff"] = np.full((1, 1), float(half), np.float32)
        E, EL = cfg["E"], cfg["E"] // N_CORES
        m["w_gate"] = inp["w_gate"][0].reshape(E * D, cfg["DE"])
        m["w_up"] = inp["w_up"][0].reshape(E * D, cfg["DE"])
        m["w_down"] = inp["w_down"][0].reshape(E * cfg["DE"], D)
        m["b_gate"] = f(inp["b_gate"][0])
        m["b_up"] = f(inp["b_up"][0])
        m["b_down"] = f(inp["b_down"][0])
        m["ffn_norm_g"] = f(inp["ffn_norm_g"][0].reshape(1, -1))
        m["w_router"] = f(inp["w_router"][0])
        m["b_router"] = f(inp["b_router"][0].reshape(1, -1))
        for k in ("ssm_a_re", "ssm_a_im", "ssm_b_re", "ssm_b_im", "ssm_c_re", "ssm_c_im", "ssm_w_glu"):
            m[k] = f(inp[k][0])
        m["ssm_log_step"] = f(inp["ssm_log_step"][0].reshape(1, -1))
        m["ssm_d"] = f(inp["ssm_d"][0].reshape(1, -1))
        m["ssm_b_glu"] = f(inp["ssm_b_glu"][0].reshape(1, -1))
        for k in ("attn_norm_g", "w_in", "q_lora_norm_g", "w_q_up", "kv_lora_norm_g", "w_kv_up", "q_head_norm_g",
                  "k_head_norm_g", "attn_out_norm_g", "ssm_out_norm_g", "w_out"):
            a = inp[k][0]
            m[k] = f(a.reshape(1, -1) if a.ndim == 1 else a)
        maps.append(m)
    return maps


_NC_CACHE = {}


def kernel(**inputs):
    cfg = FULL_CFG
    inp = {k: np.asarray(v) for k, v in inputs.items()}
    if "nc" not in _NC_CACHE:
        _NC_CACHE["nc"] = build(cfg)
    nc = _NC_CACHE["nc"]
    maps = make_in_maps(cfg, inp)
    res = run_bass_kernel_spmd(nc, maps, core_ids=list(range(N_CORES)))
    B, SEQ, D = cfg["B"], cfg["SEQ"], cfg["D"]
    OWN = SEQ // 2
    try:
        print("MOE max routed tokens per (core, expert):", [int(r["cnt_out"][0].max()) for r in res.results], "capacity", cfg["CAP"], flush=True)
    except Exception:
        pass
    outp = np.empty((B, SEQ, D), np.float32)
    for core in range(N_CORES):
        b, half = core // 2, core % 2
        outp[b, half * OWN:(half + 1) * OWN] = res.results[core]["out"]
    return outp
```

```python
import math
import os
from contextlib import ExitStack

import numpy as np
import concourse.bass as bass
import concourse.mybir as mybir
from concourse.bass_utils import run_bass_kernel_spmd

F32 = mybir.dt.float32
BF16 = mybir.dt.bfloat16
I32 = mybir.dt.int32
U32 = mybir.dt.uint32
ALU = mybir.AluOpType
AF = mybir.ActivationFunctionType
AX = mybir.AxisListType

N_CORES = 8

FULL_CFG = dict(D=4096, B=4, SEQ=4096, H=16, NOPE=128, ROPE=64, DV=128, QL=1024, KVL=512,
                G=128, P=64, C=16, E=32, DE=2048, TOPK=4, CAP=768, THETA=10000.0,
                ALPHA=1.702, LIMIT=7.0, EPS=1e-6)


class Buf:
    __slots__ = ("name", "w", "r")

    def __init__(self, name=""):
        self.name = name
        self.w = None
        self.r = []


class Sched:
    ENGS = ("pe", "act", "dve", "pool", "sp")
    NDMA = 48
    NPOOL = 8

    def __init__(self, nc, es):
        self.nc = nc
        self.q = {e: [] for e in self.ENGS}
        self.cnt = {e: 0 for e in self.ENGS}
        self.seen = {e: {} for e in self.ENGS}
        self.sem = {e: es.enter_context(nc.semaphore("s_" + e)) for e in ("pe", "act", "dve", "pool")}
        self.dsem = [es.enter_context(nc.semaphore("d%d" % i)) for i in range(self.NDMA)]
        self.dtot = [0] * self.NDMA
        self.drr = 0
        self.prr = 0
        self.nops = 0

    def _semobj(self, key):
        return self.sem[key] if isinstance(key, str) else self.dsem[key]

    def _waits(self, eng, deps):
        need = {}
        for d in deps:
            if d is None:
                continue
            k, v = d
            if eng == "pe" and k == "pe":
                continue
            if self.seen[eng].get(k, 0) >= v:
                continue
            if need.get(k, 0) < v:
                need[k] = v
        for k, v in need.items():
            self.seen[eng][k] = v
            so = self._semobj(k)
            self.q[eng].append(lambda e, so=so, v=v: e.wait_ge(so, v))

    @staticmethod
    def _deps(reads, writes):
        deps = []
        for b in reads:
            deps.append(b.w)
        for b in writes:
            deps.append(b.w)
            deps.extend(b.r)
        return deps

    @staticmethod
    def _mark(tok, reads, writes):
        for b in writes:
            b.w = tok
            b.r = []
        for b in reads:
            if b.w is not tok:
                b.r.append(tok)

    _cap = None

    def capture(self):
        self._cap = []

    def end_capture(self):
        c, self._cap = self._cap, None
        return c

    def replay_interleaved(self, lists, gran=3):
        idx = [0] * len(lists)
        while any(idx[i] < len(lists[i]) for i in range(len(lists))):
            for i, l in enumerate(lists):
                for _ in range(gran):
                    if idx[i] < len(l):
                        kind, a = l[idx[i]]
                        idx[i] += 1
                        (self.op if kind == "op" else self.dma)(*a)

    def op(self, eng, fn, reads=(), writes=()):
        if self._cap is not None:
            self._cap.append(("op", (eng, fn, tuple(reads), tuple(writes))))
            return
        self._waits(eng, self._deps(reads, writes))
        self.cnt[eng] += 1
        so = self.sem[eng]
        self.q[eng].append(lambda e, fn=fn, so=so: fn(e).then_inc(so, 1))
        self._mark((eng, self.cnt[eng]), reads, writes)
        self.nops += 1

    def dma(self, eng, fn, reads=(), writes=(), inc=16):
        if self._cap is not None:
            self._cap.append(("dma", (eng, fn, tuple(reads), tuple(writes), inc)))
            return
        if eng == "pool":
            k = self.prr
            self.prr = (self.prr + 1) % self.NPOOL
        else:
            k = self.NPOOL + self.drr
            self.drr = (self.drr + 1) % (self.NDMA - self.NPOOL)
        deps = self._deps(reads, writes)
        if self.dtot[k]:
            deps.append((k, self.dtot[k]))
        self._waits(eng, deps)
        self.dtot[k] += inc
        so = self.dsem[k]
        self.q[eng].append(lambda e, fn=fn, so=so, inc=inc: fn(e).then_inc(so, inc))
        self._mark((k, self.dtot[k]), reads, writes)
        self.nops += 1

    def flush(self):
        allk = [(k, self.dtot[k]) for k in range(self.NDMA) if self.dtot[k]]
        allk += [(e, self.cnt[e]) for e in ("pe", "act", "dve", "pool") if self.cnt[e]]
        for eng in self.ENGS:
            need = [d for d in allk if self.seen[eng].get(d[0], 0) < d[1]]
            for k, v in need:
                self.seen[eng][k] = v
                so = self._semobj(k)
                self.q[eng].append(lambda e, so=so, v=v: e.wait_ge(so, v))
        nc = self.nc
        q = self.q
        self.q = {e: [] for e in self.ENGS}
        with nc.Block() as block:
            @block.sync
            def _(e):
                for f in q["sp"]:
                    f(e)

            @block.tensor
            def _(e):
                for f in q["pe"]:
                    f(e)

            @block.scalar
            def _(e):
                for f in q["act"]:
                    f(e)

            @block.vector
            def _(e):
                for f in q["dve"]:
                    f(e)

            @block.gpsimd
            def _(e):
                for f in q["pool"]:
                    f(e)

    def finish(self, final_bufs):
        self.flush()


class Ctx:
    _n = [0]

    def __init__(self, nc, es, S):
        self.nc, self.es, self.S = nc, es, S

    def sb(self, shape, dt, name=None):
        Ctx._n[0] += 1
        t = self.es.enter_context(self.nc.sbuf_tensor("%s_%d" % (name or "t", Ctx._n[0]), list(shape), dt))
        return t, Buf(name or "t")

    def ps(self, shape, dt, name=None):
        Ctx._n[0] += 1
        t = self.es.enter_context(self.nc.psum_tensor("%s_%d" % (name or "p", Ctx._n[0]), list(shape), dt))
        return t, Buf(name or "p")

    def dram(self, shape, dt, name):
        t = self.nc.dram_tensor(name, list(shape), dt, kind="Internal")
        return t.ap(), Buf(name)


class Phase:
    def __init__(self, nc, S):
        self.nc = nc
        self.S = S
        self.es = ExitStack()

    def __enter__(self):
        self.es.__enter__()
        self.cx = Ctx(self.nc, self.es, self.S)
        return self

    def __exit__(self, *a):
        if a[0] is None:
            self.S.flush()
        return self.es.__exit__(*a)

    def identity(self, dt=BF16):
        S, cx = self.S, self.cx
        idf, idf_b = cx.sb([128, 128], F32, "idf")
        S.op("pool", lambda e: e.memset(idf[:], 0.0), writes=[idf_b])
        S.op("pool", lambda e: e.affine_select(out=idf[:], in_=idf[:], pattern=[[-1, 128]],
                                               compare_op=ALU.not_equal, fill=1.0, base=0,
                                               channel_multiplier=1), reads=[idf_b], writes=[idf_b])
        if dt == F32:
            return idf, idf_b
        idb, idb_b = cx.sb([128, 128], BF16, "idb")
        S.op("dve", lambda e: e.tensor_copy(out=idb[:], in_=idf[:]), reads=[idf_b], writes=[idb_b])
        return idb, idb_b

    def bcast_load(self, src_row_ap, n, name="g"):
        t, b = self.cx.sb([128, n], F32, name)
        self.S.dma("sp", lambda e: e.dma_start(out=t[:], in_=src_row_ap.to_broadcast([128, n])), writes=[b])
        return t, b

    def rstd(self, ss, ss_b, n, eps, np_=128):
        S = self.S
        S.op("dve", lambda e: e.tensor_scalar(out=ss, in0=ss, scalar1=1.0 / n, scalar2=eps,
                                              op0=ALU.mult, op1=ALU.add), reads=[ss_b], writes=[ss_b])
        S.op("act", lambda e: e.activation(out=ss, in_=ss, func=AF.Sqrt), reads=[ss_b], writes=[ss_b])
        S.op("dve", lambda e: e.reciprocal(out=ss, in_=ss), reads=[ss_b], writes=[ss_b])

    def rmsnorm(self, x, x_b, n, g, g_b, out, out_b, scr, scr_b, ss, ss_b, eps):
        S = self.S
        S.op("dve", lambda e: e.memset(ss, 0.0), writes=[ss_b])
        S.op("act", lambda e: e.activation(out=scr, in_=x, func=AF.Square, accum_out=ss),
             reads=[x_b, ss_b], writes=[scr_b, ss_b])
        self.rstd(ss, ss_b, n, eps)
        S.op("dve", lambda e: e.scalar_tensor_tensor(out=out, in0=x, scalar=ss, in1=g, op0=ALU.mult,
                                                     op1=ALU.mult), reads=[x_b, ss_b, g_b], writes=[out_b])

    def transpose_chunks(self, src, src_b, nch, dst, dst_b, ident, ident_b, pts, rows=128, evac="act"):
        S = self.S
        for c0 in range(0, nch, 8):
            n = min(8, nch - c0)
            pt, pt_b = pts[self._ptr % len(pts)]
            self._ptr += 1
            for c in range(n):
                S.op("pe", lambda e, pt=pt, c=c, c0=c0: e.transpose(out=pt[:, c, 0:rows],
                                                                  in_=src[:, (c0 + c) * 128:(c0 + c + 1) * 128],
                                                                  identity=ident[0:rows, 0:rows]),
                     reads=[src_b, ident_b], writes=[pt_b])
            if evac == "act":
                S.op("act", lambda e, pt=pt, c0=c0, n=n: e.copy(out=dst[:, c0:c0 + n, :], in_=pt[:, 0:n, 0:rows]),
                     reads=[pt_b], writes=[dst_b])
            else:
                S.op("dve", lambda e, pt=pt, c0=c0, n=n: e.tensor_copy(out=dst[:, c0:c0 + n, :], in_=pt[:, 0:n, 0:rows]),
                     reads=[pt_b], writes=[dst_b])
    _ptr = 0

    def sincos(self, ang, ang_b, shape, sin_out, cos_out, out_b):
        S, cx = self.S, self.cx
        ki, ki_b = cx.sb(shape, I32, "ki")
        kf, kf_b = cx.sb(shape, F32, "kf")
        r, r_b = cx.sb(shape, F32, "r")
        for which, outt in ((0, sin_out), (1, cos_out)):
            sh = 0.0 if which == 0 else 0.25
            S.op("dve", lambda e, sh=sh: e.tensor_scalar(out=r[:], in0=ang, scalar1=1.0 / (2 * math.pi), scalar2=sh,
                                                        op0=ALU.mult, op1=ALU.add), reads=[ang_b], writes=[r_b])
            S.op("dve", lambda e: e.tensor_copy(out=ki[:], in_=r[:]), reads=[r_b], writes=[ki_b])
            S.op("dve", lambda e: e.tensor_copy(out=kf[:], in_=ki[:]), reads=[ki_b], writes=[kf_b])
            S.op("dve", lambda e: e.tensor_tensor(out=r[:], in0=r[:], in1=kf[:], op=ALU.subtract),
                 reads=[r_b, kf_b], writes=[r_b])
            S.op("dve", lambda e: e.tensor_scalar(out=kf[:], in0=r[:], scalar1=0.5, scalar2=None, op0=ALU.is_gt),
                 reads=[r_b], writes=[kf_b])
            S.op("dve", lambda e: e.tensor_tensor(out=r[:], in0=r[:], in1=kf[:], op=ALU.subtract),
                 reads=[r_b, kf_b], writes=[r_b])
            S.op("dve", lambda e: e.tensor_scalar(out=kf[:], in0=r[:], scalar1=-0.5, scalar2=None, op0=ALU.is_lt),
                 reads=[r_b], writes=[kf_b])
            S.op("dve", lambda e: e.tensor_tensor(out=r[:], in0=r[:], in1=kf[:], op=ALU.add),
                 reads=[r_b, kf_b], writes=[r_b])
            S.op("act", lambda e, outt=outt: e.activation(out=outt, in_=r[:], func=AF.Sin, scale=2 * math.pi),
                 reads=[r_b], writes=[out_b])


class RR:
    def __init__(self, items):
        self.items = items
        self.i = 0

    def get(self):
        it = self.items[self.i % len(self.items)]
        self.i += 1
        return it


def build(cfg, debug=False):
    D, SEQ, H, NOPE, ROPE, DV = cfg["D"], cfg["SEQ"], cfg["H"], cfg["NOPE"], cfg["ROPE"], cfg["DV"]
    QL, KVL, G, P, C, E, DE = cfg["QL"], cfg["KVL"], cfg["G"], cfg["P"], cfg["C"], cfg["E"], cfg["DE"]
    EPS = cfg["EPS"]
    QKD = NOPE + ROPE
    DATT = H * DV
    DSSM = G * C
    DMIX = DATT + DSSM
    DIN = QL + KVL + ROPE + DSSM
    OWN = SEQ // 2
    KC = D // 128
    NT = SEQ // 128
    EL = E // N_CORES
    HALF = ROPE // 2
    assert NOPE == 128 and DV == 128 and ROPE == 64 and DMIX == D

    nc = bass.Bass("TRN2", target_bir_lowering=False)

    def din(name, shape, dt=F32):
        return nc.dram_tensor(name, list(shape), dt, kind="ExternalInput").ap()

    x = din("x", [SEQ, D])
    pos = din("pos", [SEQ, 1], I32)
    half_sel = din("half_sel", [1, 1], I32)
    invf = din("invf", [1, HALF])
    attn_norm_g = din("attn_norm_g", [1, D])
    w_in = din("w_in", [D, DIN])
    q_lora_norm_g = din("q_lora_norm_g", [1, QL])
    w_q_up = din("w_q_up", [QL, H * QKD])
    kv_lora_norm_g = din("kv_lora_norm_g", [1, KVL])
    w_kv_up = din("w_kv_up", [KVL, H * (NOPE + DV)])
    q_head_norm_g = din("q_head_norm_g", [1, QKD])
    k_head_norm_g = din("k_head_norm_g", [1, QKD])
    attn_out_norm_g = din("attn_out_norm_g", [1, DATT])
    ssm_out_norm_g = din("ssm_out_norm_g", [1, DSSM])
    w_out = din("w_out", [DMIX, D])
    ssm_a_re = din("ssm_a_re", [2, G, P])
    ssm_a_im = din("ssm_a_im", [2, G, P])
    ssm_log_step = din("ssm_log_step", [1, 2 * G])
    ssm_b_re = din("ssm_b_re", [2, G, P, C])
    ssm_b_im = din("ssm_b_im", [2, G, P, C])
    ssm_c_re = din("ssm_c_re", [2, G, C, P])
    ssm_c_im = din("ssm_c_im", [2, G, C, P])
    ssm_d = din("ssm_d", [1, DSSM])
    ssm_w_glu = din("ssm_w_glu", [DSSM, DSSM])
    ssm_b_glu = din("ssm_b_glu", [1, DSSM])
    halff = din("halff", [1, 1])
    ffn_norm_g = din("ffn_norm_g", [1, D])
    w_router = din("w_router", [D, E])
    b_router = din("b_router", [1, E])
    _early = debug in ("C", "D", "D0", "D1a", "E", "F1")
    w_gate = din("w_gate", [128 if _early else E * D, DE])
    w_up = din("w_up", [128 if _early else E * D, DE])
    w_down = din("w_down", [128 if _early else E * DE, D])
    b_gate = din("b_gate", [E, DE])
    b_up = din("b_up", [E, DE])
    b_down = din("b_down", [E, D])
    CAP = cfg["CAP"]
    NSLOT = E * CAP
    own_x = din("own_x", [OWN, D])
    own_pos = din("own_pos", [OWN, 1], I32)
    out = nc.dram_tensor("out", [OWN, D], F32, kind="ExternalOutput").ap()
    cnt_out = nc.dram_tensor("cnt_out", [128, E], F32, kind="ExternalOutput").ap()
    dbg = {}

    def dout(name, shape, dt=F32):
        return nc.dram_tensor(name, list(shape), dt, kind="ExternalOutput").ap()

    ges = ExitStack()
    with ges:
        S = Sched(nc, ges)
        gcx = Ctx(nc, ges, S)
        w_in_bf, w_in_bf_b = gcx.dram([D, DIN], BF16, "w_in_bf")
        w_q_bf, w_q_bf_b = gcx.dram([QL, H * QKD], BF16, "w_q_bf")
        w_kv_bf, w_kv_bf_b = gcx.dram([KVL, H * 256], BF16, "w_kv_bf")
        w_out_bf, w_out_bf_b = gcx.dram([DMIX, D], BF16, "w_out_bf")
        proj, proj_b = gcx.dram([SEQ, KVL + ROPE], F32, "proj")
        projq, projq_b = gcx.dram([OWN, QL], F32, "projq")
        kTn, kTn_b = gcx.dram([H, 128, SEQ], BF16, "kTn")
        kTr, kTr_b = gcx.dram([H, 64, SEQ], BF16, "kTr")
        qTn, qTn_b = gcx.dram([H, 128, OWN], BF16, "qTn")
        qTr, qTr_b = gcx.dram([H, 64, OWN], BF16, "qTr")
        vsc, vsc_b = gcx.dram([SEQ, H * DV], BF16, "vsc")
        if debug:
            mixT = dout("mixT", [DMIX, OWN])
            mixT_b = Buf("mixT")
        else:
            mixT, mixT_b = gcx.dram([DMIX, OWN], F32, "mixT")
        h1, h1_b = gcx.dram([OWN, D], F32, "h1")
        w_glu_bf, w_glu_bf_b = gcx.dram([DSSM, DSSM], BF16, "w_glu_bf")
        Xd, Xd_b = gcx.dram([NSLOT, D], BF16, "Xd")
        Yd, Yd_b = gcx.dram([NSLOT, D], BF16, "Yd")
        idxd, idxd_b = gcx.dram([OWN, 4], I32, "idxd")
        gated, gated_b = gcx.dram([OWN, 4], F32, "gated")
        uT32, uT32_b = gcx.dram([DSSM, SEQ], F32, "uT32")
        uTb, uTb_b = gcx.dram([DSSM, SEQ], BF16, "uTb")
        gT32, gT32_b = gcx.dram([DSSM, OWN], F32, "gT32")
        gTb, gTb_b = gcx.dram([DSSM, OWN], BF16, "gTb")

        with Phase(nc, S) as ph:
            for (src, dst, dst_b, rows) in ((w_in, w_in_bf, w_in_bf_b, D), (w_q_up, w_q_bf, w_q_bf_b, QL),
                                            (w_kv_up, w_kv_bf, w_kv_bf_b, KVL), (w_out, w_out_bf, w_out_bf_b, DMIX),
                                            (ssm_w_glu, w_glu_bf, w_glu_bf_b, DSSM)):
                for r0 in range(0, rows, 512):
                    r1 = min(rows, r0 + 512)
                    S.dma("pool", lambda e, src=src, dst=dst, r0=r0, r1=r1: e.dma_start(out=dst[r0:r1, :], in_=src[r0:r1, :]),
                          writes=[dst_b])

        def phase_a(xsrc, ntile, col_lo, col_hi, dst, dst_b, fm=None):
          GT = min(4, ntile)
          GW = GT * 128
          with Phase(nc, S) as ph:
              cx = ph.cx
              ident, ident_b = ph.identity()
              g1, g1_b = ph.bcast_load(attn_norm_g[0:1, :], D, "g1")
              xt_p = RR([cx.sb([128, D], F32, "xt") for _ in range(2)])
              scr, scr_b = cx.sb([128, D], BF16, "scr")
              z_p = RR([cx.sb([128, D], BF16, "z") for _ in range(1)])
              ss_p = RR([cx.sb([128, 1], F32, "ss") for _ in range(2)])
              zT_p = RR([cx.sb([128, KC, GW], BF16, "zT") for _ in range(1)])
              wt_p = RR([cx.sb([128, KC, 512], BF16, "wt") for _ in range(2)])
              ot_p = RR([cx.sb([128, 512], F32, "ot") for _ in range(3)])
              ob_p = RR([cx.sb([128, 512], BF16, "ob") for _ in range(2)])
              pts = [cx.ps([128, 8, 128], BF16, "ptr") for _ in range(2)]
              pm_p = RR([cx.ps([128, 512], F32, "pm") for _ in range(3)])
              for gi in range(ntile // GT):
                  own_g = (gi * GW) // OWN
                  zT, zT_b = zT_p.get()
                  for ti in range(GT):
                      t0 = gi * GW + ti * 128
                      xt, xt_b = xt_p.get()
                      z, z_b = z_p.get()
                      ss, ss_b = ss_p.get()
                      S.dma("sp", lambda e, xt=xt, t0=t0: e.dma_start(out=xt[:], in_=xsrc[t0:t0 + 128, :]), writes=[xt_b])
                      ph.rmsnorm(xt[:], xt_b, D, g1[:], g1_b, z[:], z_b, scr[:], scr_b, ss[:], ss_b, EPS)
                      ph.transpose_chunks(z, z_b, KC, zT[:, :, ti * 128:(ti + 1) * 128], zT_b, ident, ident_b, pts)
                  for c0 in range(col_lo, col_hi, 512):
                      c1 = min(col_hi, c0 + 512)
                      wt, wt_b = wt_p.get()
                      S.dma("sp", lambda e, wt=wt, c0=c0, c1=c1: e.dma_start(
                          out=wt[:, :, 0:c1 - c0], in_=w_in_bf[:, c0:c1].rearrange("(c p) n -> p c n", p=128)),
                          reads=[w_in_bf_b], writes=[wt_b])
                      for ti in range(GT):
                          t0 = gi * GW + ti * 128
                          pm, pm_b = pm_p.get()
                          for c in range(KC):
                              S.op("pe", lambda e, pm=pm, zT=zT, wt=wt, c=c, ti=ti, n=c1 - c0: e.matmul(
                                  pm[:, 0:n], lhsT=zT[:, c, ti * 128:(ti + 1) * 128], rhs=wt[:, c, 0:n],
                                  start=(c == 0), stop=(c == KC - 1)), reads=[zT_b, wt_b], writes=[pm_b])
                          ot, ot_b = ot_p.get()
                          S.op("dve", lambda e, ot=ot, pm=pm, n=c1 - c0: e.tensor_copy(out=ot[:, 0:n], in_=pm[:, 0:n]),
                               reads=[pm_b], writes=[ot_b])
                          S.dma("sp", lambda e, ot=ot, t0=t0, c0=c0, c1=c1: e.dma_start(out=dst[t0:t0 + 128, c0 - col_lo:c1 - col_lo],
                                                                                    in_=ot[:, 0:c1 - c0]),
                                reads=[ot_b], writes=[dst_b])
                  if fm is not None:
                      g0 = gi * GW
                      for c0 in range(fm[0], fm[1], 512):
                          c1 = min(fm[1], c0 + 512)
                          wt, wt_b = wt_p.get()
                          S.dma("sp", lambda e, wt=wt, c0=c0, c1=c1: e.dma_start(
                              out=wt[:, :, 0:c1 - c0], in_=w_in_bf[:, c0:c1].rearrange("(c p) n -> p c n", p=128)),
                              reads=[w_in_bf_b], writes=[wt_b])
                          for k in range((c1 - c0) // 128):
                              r0 = c0 - fm[0] + k * 128
                              pm, pm_b = pm_p.get()
                              for c in range(KC):
                                  S.op("pe", lambda e, pm=pm, zT=zT, wt=wt, c=c, k=k: e.matmul(
                                      pm[:, 0:GW], lhsT=wt[:, c, k * 128:(k + 1) * 128], rhs=zT[:, c, :],
                                      start=(c == 0), stop=(c == KC - 1)), reads=[zT_b, wt_b], writes=[pm_b])
                              ot, ot_b = ot_p.get()
                              ob, ob_b = ob_p.get()
                              S.op("act", lambda e, ot=ot, pm=pm: e.copy(out=ot[:, 0:GW], in_=pm[:, 0:GW]), reads=[pm_b], writes=[ot_b])
                              S.op("dve", lambda e, ob=ob, ot=ot: e.tensor_copy(out=ob[:, 0:GW], in_=ot[:, 0:GW]), reads=[ot_b], writes=[ob_b])
                              S.dma("sp", lambda e, ot=ot, r0=r0, g0=g0: e.dma_start(out=uT32[r0:r0 + 128, g0:g0 + GW], in_=ot[:, 0:GW]),
                                    reads=[ot_b], writes=[uT32_b])
                              S.dma("sp", lambda e, ob=ob, r0=r0, g0=g0: e.dma_start(out=uTb[r0:r0 + 128, g0:g0 + GW], in_=ob[:, 0:GW]),
                                    reads=[ob_b], writes=[uTb_b])


        phase_a(x, NT, QL, QL + KVL + ROPE, proj, proj_b, fm=(QL + KVL + ROPE, DIN))
        phase_a(own_x, OWN // 128, 0, QL, projq, projq_b)

        with Phase(nc, S) as ph:
            cx = ph.cx
            ident, ident_b = ph.identity()
            gq, gq_b = ph.bcast_load(q_lora_norm_g[0:1, :], QL, "gq")
            gkv, gkv_b = ph.bcast_load(kv_lora_norm_g[0:1, :], KVL, "gkv")
            gqh, gqh_b = ph.bcast_load(q_head_norm_g[0:1, :], QKD, "gqh")
            gkh, gkh_b = ph.bcast_load(k_head_norm_g[0:1, :], QKD, "gkh")
            ivf, ivf_b = ph.bcast_load(invf[0:1, :], HALF, "ivf")
            wq, wq_b = cx.sb([128, QL // 128, H * QKD], BF16, "wq")
            wkv, wkv_b = cx.sb([128, KVL // 128, H * 256], BF16, "wkv")
            S.dma("sp", lambda e: e.dma_start(out=wq[:], in_=w_q_bf.rearrange("(c p) n -> p c n", p=128)),
                  reads=[w_q_bf_b], writes=[wq_b])
            S.dma("sp", lambda e: e.dma_start(out=wkv[:], in_=w_kv_bf.rearrange("(c p) n -> p c n", p=128)),
                  reads=[w_kv_bf_b], writes=[wkv_b])
            pts = [cx.ps([128, 8, 128], BF16, "ptr") for _ in range(2)]
            pm_p = RR([cx.ps([128, 512], F32, "pm") for _ in range(3)])
            NMAX = max(QL, KVL)
            scr, scr_b = cx.sb([128, NMAX], BF16, "scr")
            ss, ss_b = cx.sb([128, 1], F32, "ss")
            lat, lat_b = cx.sb([128, NMAX], F32, "lat")
            latn, latn_b = cx.sb([128, NMAX], BF16, "latn")
            latT, latT_b = cx.sb([128, NMAX // 128, 128], BF16, "latT")
            big, big_b = cx.sb([128, H * 256], F32, "big")
            sq, sq_b = cx.sb([128, H * QKD], F32, "sq")
            ssh, ssh_b = cx.sb([128, H], F32, "ssh")
            kpe, kpe_b = cx.sb([128, ROPE], F32, "kpe")
            sspe, sspe_b = cx.sb([128, 1], F32, "sspe")
            pe2, pe2_b = cx.sb([128, ROPE], F32, "pe2")
            nn, nn_b = cx.sb([128, H, 128], F32, "nn")
            nnb, nnb_b = cx.sb([128, H * 128], BF16, "nnb")
            rr, rr_b = cx.sb([128, H, ROPE], F32, "rr")
            ra, ra_b = cx.sb([128, H, HALF], F32, "ra")
            rb, rb_b = cx.sb([128, H, HALF], F32, "rb")
            rrb, rrb_b = cx.sb([128, H * ROPE], BF16, "rrb")
            nT, nT_b = cx.sb([128, H, 128], BF16, "nT")
            rT, rT_b = cx.sb([128, max(1, H * ROPE // 128), 128], BF16, "rT")
            vb, vb_b = cx.sb([128, H, DV], BF16, "vb")
            posi, posi_b = cx.sb([128, 1], I32, "posi")
            posf, posf_b = cx.sb([128, 1], F32, "posf")
            ang, ang_b = cx.sb([128, HALF], F32, "ang")
            cs, cs_b = cx.sb([128, 2, HALF], F32, "cs")

            def heads_finish(nope_view, rope_in, rope_bcast_h, gains, gains_b, dstn, dstn_b, dstr, dstr_b, col0):
                ph.rstd(ssh[:], ssh_b, QKD, EPS)
                S.op("dve", lambda e: e.tensor_tensor(out=nn[:], in0=nope_view, in1=ssh[:, :, None].to_broadcast([128, H, 128]),
                                                      op=ALU.mult), reads=[big_b, ssh_b], writes=[nn_b])
                S.op("dve", lambda e: e.tensor_tensor(out=nnb[:].rearrange("p (h d) -> p h d", h=H), in0=nn[:],
                                                      in1=gains[:, None, 0:128].to_broadcast([128, H, 128]), op=ALU.mult),
                     reads=[nn_b, gains_b], writes=[nnb_b])
                rin = rope_in if not rope_bcast_h else rope_in[:, None, :].to_broadcast([128, H, ROPE])
                S.op("dve", lambda e: e.tensor_tensor(out=rr[:], in0=rin, in1=ssh[:, :, None].to_broadcast([128, H, ROPE]),
                                                      op=ALU.mult), reads=[big_b, kpe_b, ssh_b], writes=[rr_b])
                S.op("dve", lambda e: e.tensor_tensor(out=rr[:], in0=rr[:], in1=gains[:, None, 128:QKD].to_broadcast([128, H, ROPE]),
                                                      op=ALU.mult), reads=[rr_b, gains_b], writes=[rr_b])
                sinb = cs[:, 0:1, :].to_broadcast([128, H, HALF])
                cosb = cs[:, 1:2, :].to_broadcast([128, H, HALF])
                x1 = rr[:, :, 0:HALF]
                x2 = rr[:, :, HALF:ROPE]
                ro = rrb[:].rearrange("p (h d) -> p h d", h=H)
                S.op("dve", lambda e: e.tensor_tensor(out=ra[:], in0=x1, in1=cosb, op=ALU.mult), reads=[rr_b, cs_b], writes=[ra_b])
                S.op("dve", lambda e: e.tensor_tensor(out=rb[:], in0=x2, in1=sinb, op=ALU.mult), reads=[rr_b, cs_b], writes=[rb_b])
                S.op("dve", lambda e: e.tensor_tensor(out=ro[:, :, 0:HALF], in0=ra[:], in1=rb[:], op=ALU.subtract),
                     reads=[ra_b, rb_b], writes=[rrb_b])
                S.op("dve", lambda e: e.tensor_tensor(out=ra[:], in0=x2, in1=cosb, op=ALU.mult), reads=[rr_b, cs_b, rrb_b], writes=[ra_b])
                S.op("dve", lambda e: e.tensor_tensor(out=rb[:], in0=x1, in1=sinb, op=ALU.mult), reads=[rr_b, cs_b, rrb_b], writes=[rb_b])
                S.op("dve", lambda e: e.tensor_tensor(out=ro[:, :, HALF:ROPE], in0=ra[:], in1=rb[:], op=ALU.add),
                     reads=[ra_b, rb_b], writes=[rrb_b])
                ph.transpose_chunks(nnb, nnb_b, H, nT[:], nT_b, ident, ident_b, pts)
                ph.transpose_chunks(rrb, rrb_b, H * ROPE // 128, rT[:], rT_b, ident, ident_b, pts)
                S.dma("sp", lambda e: e.dma_start(out=dstn[:, :, col0:col0 + 128].rearrange("h d t -> d h t"), in_=nT[:]),
                      reads=[nT_b], writes=[dstn_b])
                S.dma("sp", lambda e: e.dma_start(
                    out=dstr[:, :, col0:col0 + 128].rearrange("(j two) d t -> (two d) j t", two=2), in_=rT[:]),
                    reads=[rT_b], writes=[dstr_b])

            for ti in range(NT):
                t0 = ti * 128
                S.dma("sp", lambda e, t0=t0: e.dma_start(out=posi[:], in_=pos[t0:t0 + 128, :]), writes=[posi_b])
                S.op("dve", lambda e: e.tensor_copy(out=posf[:], in_=posi[:]), reads=[posi_b], writes=[posf_b])
                S.op("dve", lambda e: e.tensor_scalar(out=ang[:], in0=ivf[:], scalar1=posf[:, 0:1], scalar2=None, op0=ALU.mult),
                     reads=[ivf_b, posf_b], writes=[ang_b])
                ph.sincos(ang[:], ang_b, [128, HALF], cs[:, 0, :], cs[:, 1, :], cs_b)
                S.dma("sp", lambda e, t0=t0: e.dma_start(out=lat[:, 0:KVL], in_=proj[t0:t0 + 128, 0:KVL]),
                      reads=[proj_b], writes=[lat_b])
                S.dma("sp", lambda e, t0=t0: e.dma_start(out=kpe[:], in_=proj[t0:t0 + 128, KVL:KVL + ROPE]),
                      reads=[proj_b], writes=[kpe_b])
                ph.rmsnorm(lat[:, 0:KVL], lat_b, KVL, gkv[:], gkv_b, latn[:, 0:KVL], latn_b, scr[:, 0:KVL], scr_b, ss[:], ss_b, EPS)
                ph.transpose_chunks(latn, latn_b, KVL // 128, latT[:, 0:KVL // 128, :], latT_b, ident, ident_b, pts)
                for c0 in range(0, H * 256, 512):
                    n = min(512, H * 256 - c0)
                    pm, pm_b = pm_p.get()
                    for c in range(KVL // 128):
                        S.op("pe", lambda e, pm=pm, c=c, c0=c0, n=n: e.matmul(pm[:, 0:n], lhsT=latT[:, c, :], rhs=wkv[:, c, c0:c0 + n],
                                                                          start=(c == 0), stop=(c == KVL // 128 - 1)),
                             reads=[latT_b, wkv_b], writes=[pm_b])
                    S.op("act", lambda e, pm=pm, c0=c0, n=n: e.copy(out=big[:, c0:c0 + n], in_=pm[:, 0:n]), reads=[pm_b], writes=[big_b])
                kvv = big[:].rearrange("p (h d) -> p h d", h=H)
                S.op("dve", lambda e: e.tensor_copy(out=vb[:], in_=kvv[:, :, 128:256]), reads=[big_b], writes=[vb_b])
                S.dma("sp", lambda e, t0=t0: e.dma_start(out=vsc[t0:t0 + 128, :], in_=vb[:].rearrange("p h d -> p (h d)")),
                      reads=[vb_b], writes=[vsc_b])
                sqv = sq[:, 0:H * 128].rearrange("p (h d) -> p h d", h=H)
                S.op("dve", lambda e: e.tensor_tensor(out=sqv, in0=kvv[:, :, 0:128], in1=kvv[:, :, 0:128], op=ALU.mult),
                     reads=[big_b], writes=[sq_b])
                S.op("dve", lambda e: e.reduce_sum(out=ssh[:], in_=sqv, axis=AX.X), reads=[sq_b], writes=[ssh_b])
                S.op("dve", lambda e: e.tensor_tensor(out=pe2[:], in0=kpe[:], in1=kpe[:], op=ALU.mult), reads=[kpe_b], writes=[pe2_b])
                S.op("dve", lambda e: e.reduce_sum(out=sspe[:], in_=pe2[:], axis=AX.X), reads=[pe2_b], writes=[sspe_b])
                S.op("dve", lambda e: e.tensor_scalar(out=ssh[:], in0=ssh[:], scalar1=sspe[:, 0:1], scalar2=None, op0=ALU.add),
                     reads=[ssh_b, sspe_b], writes=[ssh_b])
                heads_finish(kvv[:, :, 0:128], kpe[:], True, gkh, gkh_b, kTn, kTn_b, kTr, kTr_b, t0)
                if t0 < OWN:
                    pass
            for ti in range(OWN // 128):
                t0 = ti * 128
                S.dma("sp", lambda e, t0=t0: e.dma_start(out=posi[:], in_=own_pos[t0:t0 + 128, :]), writes=[posi_b])
                S.op("dve", lambda e: e.tensor_copy(out=posf[:], in_=posi[:]), reads=[posi_b], writes=[posf_b])
                S.op("dve", lambda e: e.tensor_scalar(out=ang[:], in0=ivf[:], scalar1=posf[:, 0:1], scalar2=None, op0=ALU.mult),
                     reads=[ivf_b, posf_b], writes=[ang_b])
                ph.sincos(ang[:], ang_b, [128, HALF], cs[:, 0, :], cs[:, 1, :], cs_b)
                S.dma("sp", lambda e, t0=t0: e.dma_start(out=lat[:, 0:QL], in_=projq[t0:t0 + 128, 0:QL]),
                      reads=[projq_b], writes=[lat_b])
                ph.rmsnorm(lat[:, 0:QL], lat_b, QL, gq[:], gq_b, latn[:, 0:QL], latn_b, scr[:, 0:QL], scr_b, ss[:], ss_b, EPS)
                ph.transpose_chunks(latn, latn_b, QL // 128, latT[:, 0:QL // 128, :], latT_b, ident, ident_b, pts)
                for c0 in range(0, H * QKD, 512):
                    n = min(512, H * QKD - c0)
                    pm, pm_b = pm_p.get()
                    for c in range(QL // 128):
                        S.op("pe", lambda e, pm=pm, c=c, c0=c0, n=n: e.matmul(pm[:, 0:n], lhsT=latT[:, c, :], rhs=wq[:, c, c0:c0 + n],
                                                                          start=(c == 0), stop=(c == QL // 128 - 1)),
                             reads=[latT_b, wq_b], writes=[pm_b])
                    S.op("act", lambda e, pm=pm, c0=c0, n=n: e.copy(out=big[:, c0:c0 + n], in_=pm[:, 0:n]), reads=[pm_b], writes=[big_b])
                qv = big[:, 0:H * QKD].rearrange("p (h d) -> p h d", h=H)
                sqq = sq[:].rearrange("p (h d) -> p h d", h=H)
                S.op("dve", lambda e: e.tensor_tensor(out=sqq, in0=qv, in1=qv, op=ALU.mult), reads=[big_b], writes=[sq_b])
                S.op("dve", lambda e: e.reduce_sum(out=ssh[:], in_=sqq, axis=AX.X), reads=[sq_b], writes=[ssh_b])
                heads_finish(qv[:, :, 0:128], qv[:, :, 128:QKD], False, gqh, gqh_b, qTn, qTn_b, qTr, qTr_b, t0)

        QB = min(512, OWN)
        NKT = SEQ // 128
        att_scale = QKD ** -0.5
        att_shift = -12.0
        with Phase(nc, S) as ph:
            cx = ph.cx
            ones, ones_b = cx.sb([128, 128], BF16, "ones")
            S.op("pool", lambda e: e.memset(ones[:], 1.0), writes=[ones_b])
            shf, shf_b = cx.sb([128, 1], F32, "shf")
            S.op("pool", lambda e: e.memset(shf[:], att_shift), writes=[shf_b])
            kn_p = RR([cx.sb([128, SEQ], BF16, "kn") for _ in range(2)])
            kr_p = RR([cx.sb([64, SEQ], BF16, "kr") for _ in range(2)])
            v_p = RR([cx.sb([128, NKT, DV], BF16, "v") for _ in range(2)])
            qn_p = RR([cx.sb([128, QB], BF16, "qn") for _ in range(2)])
            qr_p = RR([cx.sb([64, QB], BF16, "qr") for _ in range(2)])
            pT_p = RR([cx.sb([128, QB], BF16, "pT") for _ in range(3)])
            psS_p = RR([cx.ps([128, QB], F32, "psS") for _ in range(3)])
            psO_p = RR([cx.ps([128, QB], F32, "psO") for _ in range(2)])
            psL_p = RR([cx.ps([128, QB], F32, "psL") for _ in range(2)])
            rl_p = RR([cx.sb([128, QB], F32, "rl") for _ in range(2)])
            o_p = RR([cx.sb([128, QB], F32, "o") for _ in range(2)])
            for h in range(H):
                kn, kn_b = kn_p.get()
                kr, kr_b = kr_p.get()
                v, v_b = v_p.get()
                S.dma("sp", lambda e, kn=kn, h=h: e.dma_start(out=kn[:], in_=kTn[h]), reads=[kTn_b], writes=[kn_b])
                S.dma("sp", lambda e, kr=kr, h=h: e.dma_start(out=kr[:], in_=kTr[h]), reads=[kTr_b], writes=[kr_b])
                S.dma("sp", lambda e, v=v, h=h: e.dma_start(
                    out=v[:], in_=vsc[:, h * DV:(h + 1) * DV].rearrange("(t p) d -> p t d", p=128)),
                    reads=[vsc_b], writes=[v_b])
                for qb in range(OWN // QB):
                    q0 = qb * QB
                    qn, qn_b = qn_p.get()
                    qr, qr_b = qr_p.get()
                    S.dma("sp", lambda e, qn=qn, h=h, q0=q0: e.dma_start(out=qn[:], in_=qTn[h, :, q0:q0 + QB]),
                          reads=[qTn_b], writes=[qn_b])
                    S.dma("sp", lambda e, qr=qr, h=h, q0=q0: e.dma_start(out=qr[:], in_=qTr[h, :, q0:q0 + QB]),
                          reads=[qTr_b], writes=[qr_b])
                    psO, psO_b = psO_p.get()
                    psL, psL_b = psL_p.get()
                    for kt in range(NKT):
                        psS, psS_b = psS_p.get()
                        pT, pT_b = pT_p.get()
                        S.op("pe", lambda e, psS=psS, kn=kn, qn=qn, kt=kt: e.matmul(
                            psS[:], lhsT=kn[:, kt * 128:(kt + 1) * 128], rhs=qn[:], start=True, stop=False),
                            reads=[kn_b, qn_b], writes=[psS_b])
                        S.op("pe", lambda e, psS=psS, kr=kr, qr=qr, kt=kt: e.matmul(
                            psS[:], lhsT=kr[:, kt * 128:(kt + 1) * 128], rhs=qr[:], start=False, stop=True),
                            reads=[kr_b, qr_b], writes=[psS_b])
                        S.op("act", lambda e, pT=pT, psS=psS: e.activation(out=pT[:], in_=psS[:], func=AF.Exp,
                                                                            bias=shf[:, 0:1], scale=att_scale),
                             reads=[psS_b, shf_b], writes=[pT_b])
                        S.op("pe", lambda e, psO=psO, v=v, pT=pT, kt=kt: e.matmul(
                            psO[:], lhsT=v[:, kt, :], rhs=pT[:], start=(kt == 0), stop=(kt == NKT - 1)),
                            reads=[v_b, pT_b], writes=[psO_b])
                        S.op("pe", lambda e, psL=psL, pT=pT, kt=kt: e.matmul(
                            psL[:], lhsT=ones[:], rhs=pT[:], start=(kt == 0), stop=(kt == NKT - 1)),
                            reads=[ones_b, pT_b], writes=[psL_b])
                    rl, rl_b = rl_p.get()
                    o, o_b = o_p.get()
                    S.op("dve", lambda e, rl=rl, psL=psL: e.reciprocal(out=rl[:], in_=psL[:]), reads=[psL_b], writes=[rl_b])
                    S.op("dve", lambda e, o=o, psO=psO, rl=rl: e.tensor_tensor(out=o[:], in0=psO[:], in1=rl[:], op=ALU.mult),
                         reads=[psO_b, rl_b], writes=[o_b])
                    S.dma("sp", lambda e, o=o, h=h, q0=q0: e.dma_start(out=mixT[h * DV:(h + 1) * DV, q0:q0 + QB], in_=o[:]),
                          reads=[o_b], writes=[mixT_b])
        if debug == "C":
            return nc

        T = SEQ
        SEG = min(cfg.get("SEG", 512), T)
        NSEG = T // SEG
        SC = DSSM // 128
        NCH = G // 8
        u0 = KVL + ROPE
        NG2 = 2 * G
        TWO_PI = 2.0 * math.pi
        if debug == "D0":
            return nc
        with Phase(nc, S) as ph:
            cx = ph.cx
            identb, identb_b = ph.identity()

            def small(name, dt=F32, w=NG2):
                return cx.sb([128, w], dt, name)
            AR, AR_b = small("AR")
            AI, AI_b = small("AI")
            for (t_, b_, src) in ((AR, AR_b, ssm_a_re), (AI, AI_b, ssm_a_im)):
                for r0 in (0, 64):
                    S.dma("sp", lambda e, t_=t_, src=src, r0=r0: e.dma_start(out=t_[r0:r0 + 64, :], in_=src.rearrange("d g p -> p (d g)"),
                                                                          allow_slow_non_contiguous=True), writes=[b_])
            LS, LS_b = ph.bcast_load(ssm_log_step[0:1, :], NG2, "LS")
            hf, hf_b = ph.bcast_load(halff[0:1, :], 1, "hf")
            dcol, dcol_b = cx.sb([128, SC], F32, "dcol")
            S.dma("sp", lambda e: e.dma_start(out=dcol[:], in_=ssm_d.rearrange("o (c p) -> p (o c)", p=128), allow_slow_non_contiguous=True),
                  writes=[dcol_b])
            SGN, SGN_b = cx.sb([128, 1], F32, "SGN")
            NSGN, NSGN_b = cx.sb([128, 1], F32, "NSGN")
            S.op("pool", lambda e: e.memset(SGN[0:64, :], -1.0), writes=[SGN_b])
            S.op("pool", lambda e: e.memset(SGN[64:128, :], 1.0), writes=[SGN_b])
            S.op("pool", lambda e: e.memset(NSGN[0:64, :], 1.0), writes=[NSGN_b])
            S.op("pool", lambda e: e.memset(NSGN[64:128, :], -1.0), writes=[NSGN_b])
            hpi, hpi_b = cx.sb([128, 1], F32, "hpi")
            S.op("pool", lambda e: e.memset(hpi[:], math.pi / 2), writes=[hpi_b])

            def tt(out, a, b, op, rb, wb, eng="dve"):
                S.op(eng, lambda e: e.tensor_tensor(out=out, in0=a, in1=b, op=op), reads=rb, writes=wb)

            DT, DT_b = small("DT")
            MAG, MAG_b = small("MAG")
            TH, TH_b = small("TH")
            SN, SN_b = small("SN")
            CS, CS_b = small("CS")
            S.op("act", lambda e: e.activation(out=DT[:], in_=LS[:], func=AF.Exp), reads=[LS_b], writes=[DT_b])
            tt(MAG[:], AR[:], DT[:], ALU.mult, [AR_b, DT_b], [MAG_b])
            S.op("act", lambda e: e.activation(out=MAG[:], in_=MAG[:], func=AF.Exp), reads=[MAG_b], writes=[MAG_b])
            tt(TH[:], AI[:], DT[:], ALU.mult, [AI_b, DT_b], [TH_b])
            ph.sincos(TH[:], TH_b, [128, NG2], SN[:], CS[:], SN_b)
            CS_b = SN_b
            ABR, ABR_b = small("ABR")
            ABI, ABI_b = small("ABI")
            tt(ABR[:], MAG[:], CS[:], ALU.mult, [MAG_b, CS_b], [ABR_b])
            tt(ABI[:], MAG[:], SN[:], ALU.mult, [MAG_b, SN_b], [ABI_b])
            DEN, DEN_b = small("DEN")
            TMP, TMP_b = small("TMP")
            tt(DEN[:], AR[:], AR[:], ALU.mult, [AR_b], [DEN_b])
            tt(TMP[:], AI[:], AI[:], ALU.mult, [AI_b], [TMP_b])
            tt(DEN[:], DEN[:], TMP[:], ALU.add, [DEN_b, TMP_b], [DEN_b])
            S.op("dve", lambda e: e.reciprocal(out=DEN[:], in_=DEN[:]), reads=[DEN_b], writes=[DEN_b])
            E1, E1_b = small("E1")
            S.op("dve", lambda e: e.tensor_scalar(out=E1[:], in0=ABR[:], scalar1=-1.0, scalar2=None, op0=ALU.add), reads=[ABR_b], writes=[E1_b])
            ZR, ZR_b = small("ZR")
            ZI, ZI_b = small("ZI")
            tt(ZR[:], E1[:], AR[:], ALU.mult, [E1_b, AR_b], [ZR_b])
            tt(TMP[:], ABI[:], AI[:], ALU.mult, [ABI_b, AI_b], [TMP_b])
            tt(ZR[:], ZR[:], TMP[:], ALU.add, [ZR_b, TMP_b], [ZR_b])
            tt(ZR[:], ZR[:], DEN[:], ALU.mult, [ZR_b, DEN_b], [ZR_b])
            tt(ZI[:], ABI[:], AR[:], ALU.mult, [ABI_b, AR_b], [ZI_b])
            tt(TMP[:], E1[:], AI[:], ALU.mult, [E1_b, AI_b, ZR_b], [TMP_b])
            tt(ZI[:], ZI[:], TMP[:], ALU.subtract, [ZI_b, TMP_b], [ZI_b])
            tt(ZI[:], ZI[:], DEN[:], ALU.mult, [ZI_b, DEN_b], [ZI_b])
            ZIs, ZIs_b = small("ZIs")
            ZRs, ZRs_b = small("ZRs")
            S.op("dve", lambda e: e.tensor_scalar(out=ZIs[:], in0=ZI[:], scalar1=SGN[:, 0:1], scalar2=None, op0=ALU.mult),
                 reads=[ZI_b, SGN_b], writes=[ZIs_b])
            S.op("dve", lambda e: e.tensor_scalar(out=ZRs[:], in0=ZR[:], scalar1=NSGN[:, 0:1], scalar2=None, op0=ALU.mult),
                 reads=[ZR_b, NSGN_b], writes=[ZRs_b])
            FQ, FQ_b = small("FQ")
            S.op("dve", lambda e: e.tensor_scalar(out=FQ[:, 0:G], in0=TH[:, 0:G], scalar1=1.0 / TWO_PI, scalar2=None, op0=ALU.mult),
                 reads=[TH_b], writes=[FQ_b])
            S.op("dve", lambda e: e.tensor_scalar(out=FQ[:, G:NG2], in0=TH[:, G:NG2], scalar1=-1.0 / TWO_PI, scalar2=None, op0=ALU.mult),
                 reads=[TH_b], writes=[FQ_b])
            rmi, rmi_b = cx.sb([128, 8], I32, "rmi")
            rmf, rmf_b = cx.sb([128, 8], F32, "rmf")
            rm2, rm2_b = cx.sb([128, 8], F32, "rm2")
            RM, RM_b = cx.sb([128, 8], F32, "RM")
            S.op("pool", lambda e: e.iota(out=rmi[:], pattern=[[-16, 8]], base=0, channel_multiplier=1), writes=[rmi_b])
            S.op("dve", lambda e: e.tensor_copy(out=rmf[:], in_=rmi[:]), reads=[rmi_b], writes=[rmf_b])
            S.op("dve", lambda e: e.tensor_scalar(out=rm2[:], in0=rmf[:], scalar1=0.0, scalar2=None, op0=ALU.is_ge), reads=[rmf_b], writes=[rm2_b])
            S.op("dve", lambda e: e.tensor_scalar(out=RM[:], in0=rmf[:], scalar1=16.0, scalar2=None, op0=ALU.is_lt), reads=[rmf_b], writes=[RM_b])
            tt(RM[:], RM[:], rm2[:], ALU.mult, [RM_b, rm2_b], [RM_b])
            cmi, cmi_b = cx.sb([128, 8, 128], I32, "cmi")
            cmf, cmf_b = cx.sb([128, 8, 128], F32, "cmf")
            cm2, cm2_b = cx.sb([128, 8, 128], F32, "cm2")
            CM, CM_b = cx.sb([128, 8, 128], F32, "CM")
            S.op("pool", lambda e: e.iota(out=cmi[:], pattern=[[-16, 8], [1, 128]], base=0, channel_multiplier=0), writes=[cmi_b])
            S.op("dve", lambda e: e.tensor_copy(out=cmf[:], in_=cmi[:]), reads=[cmi_b], writes=[cmf_b])
            S.op("dve", lambda e: e.tensor_scalar(out=cm2[:], in0=cmf[:], scalar1=0.0, scalar2=None, op0=ALU.is_ge), reads=[cmf_b], writes=[cm2_b])
            S.op("dve", lambda e: e.tensor_scalar(out=CM[:], in0=cmf[:], scalar1=16.0, scalar2=None, op0=ALU.is_lt), reads=[cmf_b], writes=[CM_b])
            tt(CM[:], CM[:], cm2[:], ALU.mult, [CM_b, cm2_b], [CM_b])
            tii, tii_b = cx.sb([128, T], I32, "tii")
            tio, tio_b = cx.sb([128, T], F32, "tio")
            S.op("pool", lambda e: e.iota(out=tii[:], pattern=[[1, T]], base=0, channel_multiplier=0), writes=[tii_b])
            S.op("dve", lambda e: e.tensor_copy(out=tio[:], in_=tii[:]), reads=[tii_b], writes=[tio_b])

            ucb, ucb_b = cx.sb([128, T], BF16, "ucb")
            uc32, uc32_b = cx.sb([128, T], F32, "uc32")
            ysum, ysum_b = tii[:].bitcast(F32), tii_b
            X1, X1_b = cx.sb([128, 8, C], F32, "X1")
            X2, X2_b = cx.sb([128, 8, C], F32, "X2")
            SB1, SB1_b = cx.sb([128, 8, C], F32, "SB1")
            SB2, SB2_b = cx.sb([128, 8, C], F32, "SB2")
            TM, TM_b = cx.sb([128, 8, C], F32, "TM")
            SBb, SBb_b = cx.sb([128, 2, 128], BF16, "SBb")
            CC, CC_b = cx.sb([128, 2, 128], F32, "CC")
            CCb, CCb_b = cx.sb([128, 2, 128], BF16, "CCb")
            L12d = [cx.sb([128, 2, 128], BF16, "L12") for _ in range(2)]
            LCd = [cx.sb([128, 2, 128], F32, "LC") for _ in range(2)]
            ptb, ptb_b = cx.ps([128, 4, 128], BF16, "ptb")
            PT4, PT4_b = cx.sb([128, 4, 128], BF16, "PT4")
            Lg_pd = [RR([cx.sb([128, 4, 128], BF16, "Lg") for _ in range(2)]) for _ in range(2)]
            ki_pd = [RR([cx.sb([128, SEG], I32, "ki") for _ in range(2)]) for _ in range(2)]
            r_pd = [RR([cx.sb([128, SEG], F32, "r") for _ in range(2)]) for _ in range(2)]
            ab_pd = [RR([cx.sb([128, SEG], F32, "ab") for _ in range(2)]) for _ in range(2)]
            sn_pd = [RR([cx.sb([128, SEG], F32, "sn") for _ in range(2)]) for _ in range(2)]
            cs_pd = [RR([cx.sb([128, SEG], F32, "cs") for _ in range(2)]) for _ in range(2)]
            IN_pd = [RR([cx.sb([128, SEG], F32, "IN") for _ in range(2)]) for _ in range(2)]
            W_pd = [RR([cx.sb([128, SEG], F32, "W") for _ in range(2)]) for _ in range(2)]
            A1_pd = [RR([cx.sb([128, SEG], BF16, "A1") for _ in range(2)]) for _ in range(2)]
            A2_pd = [RR([cx.sb([128, SEG], BF16, "A2") for _ in range(2)]) for _ in range(2)]
            t1_pd = [RR([cx.sb([128, 512], F32, "t1") for _ in range(2)]) for _ in range(2)]
            cw_pd = [RR([cx.sb([128, 1], F32, "cw") for _ in range(2)]) for _ in range(2)]
            pM_pd = [RR([cx.ps([128, 512], F32, "pM") for _ in range(2)]) for _ in range(2)]
            pY_pd = [RR([cx.ps([128, 512], F32, "pY") for _ in range(1)]) for _ in range(2)]
            yo, yo_b = cx.sb([128, OWN], F32, "yo")
            ga, ga_b = cx.sb([128, OWN], F32, "ga")
            gb, gb_b = cx.sb([128, OWN], F32, "gb")
            gbf, gbf_b = cx.sb([128, OWN], BF16, "gbf")
            BW = min(512, SEG)

            for j in range(NCH if debug != "D1a" else 0):
                S.dma("sp", lambda e, j=j: e.dma_start(out=ucb[:], in_=uTb[j * 128:(j + 1) * 128, :]), reads=[uTb_b], writes=[ucb_b])
                S.dma("sp", lambda e, j=j: e.dma_start(out=uc32[:], in_=uT32[j * 128:(j + 1) * 128, :]), reads=[uT32_b], writes=[uc32_b])
                S.op("act", lambda e, j=j: e.activation(out=ysum, in_=uc32[:], func=AF.Copy, scale=dcol[:, j:j + 1]),
                     reads=[uc32_b, dcol_b], writes=[ysum_b])
                for d in range(2):
                    col0 = d * G + 8 * j
                    gs = slice(8 * j, 8 * j + 8)
                    S.dma("sp", lambda e, d=d, gs=gs: e.dma_start(out=X1[0:64], in_=ssm_b_re[d, gs].rearrange("g p c -> p g c")), writes=[X1_b])
                    S.dma("sp", lambda e, d=d, gs=gs: e.dma_start(out=X1[64:128], in_=ssm_b_im[d, gs].rearrange("g p c -> p g c")), writes=[X1_b])
                    S.dma("sp", lambda e, d=d, gs=gs: e.dma_start(out=X2[0:64], in_=ssm_b_im[d, gs].rearrange("g p c -> p g c")), writes=[X2_b])
                    S.dma("sp", lambda e, d=d, gs=gs: e.dma_start(out=X2[64:128], in_=ssm_b_re[d, gs].rearrange("g p c -> p g c")), writes=[X2_b])
                    bc = lambda A, col0=col0: A[:, col0:col0 + 8, None].to_broadcast([128, 8, C])
                    tt(SB1[:], X1[:], bc(ZR), ALU.mult, [X1_b, ZR_b], [SB1_b])
                    tt(TM[:], X2[:], bc(ZIs), ALU.mult, [X2_b, ZIs_b], [TM_b])
                    S.op("dve", lambda e: e.tensor_tensor(out=SBb[:, 0, :].rearrange("p (g c) -> p g c", g=8), in0=SB1[:], in1=TM[:], op=ALU.add),
                         reads=[SB1_b, TM_b], writes=[SBb_b])
                    tt(SB2[:], X2[:], bc(ZRs), ALU.mult, [X2_b, ZRs_b], [SB2_b])
                    tt(TM[:], X1[:], bc(ZI), ALU.mult, [X1_b, ZI_b, SBb_b], [TM_b])
                    S.op("dve", lambda e: e.tensor_tensor(out=SBb[:, 1, :].rearrange("p (g c) -> p g c", g=8), in0=SB2[:], in1=TM[:], op=ALU.add),
                         reads=[SB2_b, TM_b], writes=[SBb_b])
                    S.dma("sp", lambda e, d=d, gs=gs: e.dma_start(out=CC[:, 0, 0:64], in_=ssm_c_re[d, gs].rearrange("g c p -> (g c) p")), writes=[CC_b])
                    S.dma("sp", lambda e, d=d, gs=gs: e.dma_start(out=CC[:, 0, 64:128], in_=ssm_c_im[d, gs].rearrange("g c p -> (g c) p")), writes=[CC_b])
                    S.dma("sp", lambda e, d=d, gs=gs: e.dma_start(out=CC[:, 1, 0:64], in_=ssm_c_im[d, gs].rearrange("g c p -> (g c) p")), writes=[CC_b])
                    S.dma("sp", lambda e, d=d, gs=gs: e.dma_start(out=CC[:, 1, 64:128], in_=ssm_c_re[d, gs].rearrange("g c p -> (g c) p")), writes=[CC_b])
                    S.op("dve", lambda e: e.tensor_copy(out=CCb[:], in_=CC[:]), reads=[CC_b], writes=[CCb_b])
                    for k_, (src, src_b) in enumerate(((SBb, SBb_b), (SBb, SBb_b), (CCb, CCb_b), (CCb, CCb_b))):
                        S.op("pe", lambda e, k_=k_, src=src: e.transpose(out=ptb[:, k_, :], in_=src[:, k_ % 2, :], identity=identb[:]),
                             reads=[src_b, identb_b], writes=[ptb_b])
                    S.op("act", lambda e: e.copy(out=PT4[:], in_=ptb[:]), reads=[ptb_b], writes=[PT4_b])
                    S.op("dve", lambda e, d=d: e.tensor_copy(out=L12d[d][0][:], in_=PT4[:, 0:2, :]), reads=[PT4_b], writes=[L12d[d][1]])
                    S.op("dve", lambda e, d=d: e.tensor_scalar(out=LCd[d][0][:, 0, :], in0=PT4[:, 2, :], scalar1=NSGN[:, 0:1], scalar2=None, op0=ALU.mult),
                         reads=[PT4_b, NSGN_b], writes=[LCd[d][1]])
                    S.op("dve", lambda e, d=d: e.tensor_scalar(out=LCd[d][0][:, 1, :], in0=PT4[:, 3, :], scalar1=-1.0, scalar2=None, op0=ALU.mult),
                         reads=[PT4_b], writes=[LCd[d][1]])
                streams = []
                for d in range(2):
                    col0 = d * G + 8 * j
                    S.capture()
                    for gl in range(8):
                        col = col0 + gl
                        Lg, Lg_b = Lg_pd[d].get()
                        S.op("dve", lambda e, d=d, Lg=Lg, gl=gl: e.tensor_scalar(out=Lg[:, 0:2, :], in0=L12d[d][0][:], scalar1=RM[:, gl:gl + 1], scalar2=None, op0=ALU.mult),
                             reads=[L12d[d][1], RM_b], writes=[Lg_b])
                        S.op("dve", lambda e, d=d, Lg=Lg, gl=gl: e.tensor_tensor(out=Lg[:, 2:4, :], in0=LCd[d][0][:], in1=CM[:, gl:gl + 1, :].to_broadcast([128, 2, 128]), op=ALU.mult),
                             reads=[LCd[d][1], CM_b], writes=[Lg_b])
                        carry = None
                        segs = list(range(NSEG)) if d == 0 else list(range(NSEG - 1, -1, -1))
                        for si in segs:
                            s0 = si * SEG
                            ki, ki_b = ki_pd[d].get()
                            r, r_b = r_pd[d].get()
                            ab, ab_b = ab_pd[d].get()
                            sn, sn_b = sn_pd[d].get()
                            cs, cs_b = cs_pd[d].get()
                            IN, IN_b = IN_pd[d].get()
                            W, W_b = W_pd[d].get()
                            A1, A1_b = A1_pd[d].get()
                            A2, A2_b = A2_pd[d].get()
                            fcol = FQ[:, col:col + 1]
                            S.op("act", lambda e, ki=ki, s0=s0, fcol=fcol: e.activation(out=ki[:], in_=tio[:, s0:s0 + SEG], func=AF.Copy, scale=fcol),
                                 reads=[tio_b, FQ_b], writes=[ki_b])
                            S.op("dve", lambda e, r=r, ki=ki, s0=s0, fcol=fcol: e.scalar_tensor_tensor(out=r[:], in0=tio[:, s0:s0 + SEG], scalar=fcol, in1=ki[:],
                                                                                              op0=ALU.mult, op1=ALU.subtract),
                                 reads=[tio_b, FQ_b, ki_b], writes=[r_b])
                            S.op("act", lambda e, ab=ab, r=r: e.activation(out=ab[:], in_=r[:], func=AF.Abs), reads=[r_b], writes=[ab_b])
                            S.op("act", lambda e, sn=sn, r=r: e.activation(out=sn[:], in_=r[:], func=AF.Sin, scale=TWO_PI), reads=[r_b], writes=[sn_b])
                            S.op("act", lambda e, cs=cs, ab=ab: e.activation(out=cs[:], in_=ab[:], func=AF.Sin, scale=-TWO_PI, bias=hpi[:, 0:1]),
                                 reads=[ab_b, hpi_b], writes=[cs_b])
                            for b0 in range(0, SEG, BW):
                                pM1, pM1_b = pM_pd[d].get()
                                pM2, pM2_b = pM_pd[d].get()
                                t1, t1_b = t1_pd[d].get()
                                S.op("pe", lambda e, pM1=pM1, Lg=Lg, s0=s0, b0=b0: e.matmul(pM1[:, 0:BW], lhsT=Lg[:, 0, :], rhs=ucb[:, s0 + b0:s0 + b0 + BW],
                                                                                         start=True, stop=True), reads=[Lg_b, ucb_b], writes=[pM1_b])
                                S.op("pe", lambda e, pM2=pM2, Lg=Lg, s0=s0, b0=b0: e.matmul(pM2[:, 0:BW], lhsT=Lg[:, 1, :], rhs=ucb[:, s0 + b0:s0 + b0 + BW],
                                                                                         start=True, stop=True), reads=[Lg_b, ucb_b], writes=[pM2_b])
                                S.op("dve", lambda e, t1=t1, cs=cs, pM1=pM1, b0=b0: e.tensor_tensor(out=t1[:, 0:BW], in0=pM1[:, 0:BW], in1=cs[:, b0:b0 + BW], op=ALU.mult),
                                     reads=[pM1_b, cs_b], writes=[t1_b])
                                S.op("dve", lambda e, IN=IN, sn=sn, pM2=pM2, b0=b0: e.tensor_tensor(out=IN[:, b0:b0 + BW], in0=pM2[:, 0:BW], in1=sn[:, b0:b0 + BW], op=ALU.mult),
                                     reads=[pM2_b, sn_b], writes=[IN_b])
                                S.op("dve", lambda e, IN=IN, t1=t1, b0=b0: e.tensor_tensor(out=IN[:, b0:b0 + BW], in0=IN[:, b0:b0 + BW], in1=t1[:, 0:BW], op=ALU.add),
                                     reads=[IN_b, t1_b], writes=[IN_b])
                            rho = MAG[:, col:col + 1].to_broadcast([128, SEG])
                            init = 0.0 if carry is None else carry[0][:, 0:1]
                            rdeps = [MAG_b, IN_b] + ([carry[1]] if carry is not None else [])
                            if d == 0:
                                S.op("dve", lambda e, W=W, IN=IN, rho=rho, init=init: e.tensor_tensor_scan(out=W[:], data0=rho, data1=IN[:], initial=init,
                                                                                                   op0=ALU.mult, op1=ALU.add), reads=rdeps, writes=[W_b])
                                last = SEG - 1
                            else:
                                S.op("dve", lambda e, W=W, IN=IN, rho=rho, init=init: e.tensor_tensor_scan(out=W[:, ::-1], data0=rho, data1=IN[:, ::-1], initial=init,
                                                                                                   op0=ALU.mult, op1=ALU.add), reads=rdeps, writes=[W_b])
                                last = 0
                            if len(segs) > 1:
                                cw, cw_b = cw_pd[d].get()
                                S.op("act", lambda e, cw=cw, W=W, last=last: e.copy(out=cw[:], in_=W[:, last:last + 1]), reads=[W_b], writes=[cw_b])
                                carry = (cw, cw_b)
                            S.op("pool", lambda e, A1=A1, cs=cs, W=W: e.tensor_tensor(out=A1[:], in0=cs[:], in1=W[:], op=ALU.mult), reads=[cs_b, W_b], writes=[A1_b])
                            S.op("pool", lambda e, A2=A2, sn=sn, W=W: e.tensor_tensor(out=A2[:], in0=sn[:], in1=W[:], op=ALU.mult), reads=[sn_b, W_b], writes=[A2_b])
                            for b0 in range(0, SEG, BW):
                                pY, pY_b = pY_pd[d].get()
                                S.op("pe", lambda e, pY=pY, Lg=Lg, A1=A1, b0=b0: e.matmul(pY[:, 0:BW], lhsT=Lg[:, 2, :], rhs=A1[:, b0:b0 + BW], start=True, stop=False),
                                     reads=[Lg_b, A1_b], writes=[pY_b])
                                S.op("pe", lambda e, pY=pY, Lg=Lg, A2=A2, b0=b0: e.matmul(pY[:, 0:BW], lhsT=Lg[:, 3, :], rhs=A2[:, b0:b0 + BW], start=False, stop=True),
                                     reads=[Lg_b, A2_b], writes=[pY_b])
                                S.op("dve", lambda e, pY=pY, s0=s0, b0=b0: e.tensor_tensor(out=ysum[:, s0 + b0:s0 + b0 + BW], in0=pY[:, 0:BW],
                                                                                       in1=ysum[:, s0 + b0:s0 + b0 + BW], op=ALU.add),
                                     reads=[pY_b, ysum_b], writes=[ysum_b])
                    streams.append(S.end_capture())
                S.replay_interleaved(streams, gran=int(os.environ.get("GRAN", "3")))
                tt(ga[:], ysum[:, OWN:T], ysum[:, 0:OWN], ALU.subtract, [ysum_b], [ga_b])
                S.op("dve", lambda e: e.scalar_tensor_tensor(out=yo[:], in0=ga[:], scalar=hf[:, 0:1], in1=ysum[:, 0:OWN], op0=ALU.mult, op1=ALU.add),
                     reads=[ga_b, hf_b, ysum_b], writes=[yo_b])
                tt(ga[:], yo[:], yo[:], ALU.mult, [yo_b], [ga_b])
                S.op("dve", lambda e: e.tensor_scalar(out=ga[:], in0=ga[:], scalar1=0.044715, scalar2=1.0, op0=ALU.mult, op1=ALU.add), reads=[ga_b], writes=[ga_b])
                tt(ga[:], ga[:], yo[:], ALU.mult, [ga_b, yo_b], [ga_b])
                S.op("act", lambda e: e.activation(out=gb[:], in_=ga[:], func=AF.Sigmoid, scale=2.0 * math.sqrt(2.0 / math.pi)), reads=[ga_b], writes=[gb_b])
                tt(gb[:], gb[:], yo[:], ALU.mult, [gb_b, yo_b], [gb_b])
                S.op("act", lambda e: e.copy(out=gbf[:], in_=gb[:]), reads=[gb_b], writes=[gbf_b])
                S.dma("sp", lambda e, j=j: e.dma_start(out=gT32[j * 128:(j + 1) * 128, :], in_=gb[:]), reads=[gb_b], writes=[gT32_b])
                S.dma("sp", lambda e, j=j: e.dma_start(out=gTb[j * 128:(j + 1) * 128, :], in_=gbf[:]), reads=[gbf_b], writes=[gTb_b])

        if debug == "D1a":
            return nc
        with Phase(nc, S) as ph:
            cx = ph.cx
            wg, wg_b = cx.sb([128, SC, DSSM], BF16, "wg")
            S.dma("sp", lambda e: e.dma_start(out=wg[:], in_=w_glu_bf.rearrange("(c p) n -> p c n", p=128)), reads=[w_glu_bf_b], writes=[wg_b])
            bcol, bcol_b = cx.sb([128, SC], F32, "bcol")
            S.dma("sp", lambda e: e.dma_start(out=bcol[:], in_=ssm_b_glu.rearrange("o (c p) -> p (o c)", p=128), allow_slow_non_contiguous=True),
                  writes=[bcol_b])
            GQ = min(512, OWN)
            gbl_p = RR([cx.sb([128, SC, GQ], BF16, "gbl") for _ in range(2)])
            g32_p = RR([cx.sb([128, SC, GQ], F32, "g32") for _ in range(1)])
            pg_p = RR([cx.ps([128, GQ], F32, "pg") for _ in range(3)])
            sg_p = RR([cx.sb([128, GQ], F32, "sg") for _ in range(3)])
            for q0 in range(0, OWN, GQ):
                gbl, gbl_b = gbl_p.get()
                g32, g32_b = g32_p.get()
                S.dma("sp", lambda e, gbl=gbl, q0=q0: e.dma_start(out=gbl[:], in_=gTb[:, q0:q0 + GQ].rearrange("(c p) t -> p c t", p=128)),
                      reads=[gTb_b], writes=[gbl_b])
                S.dma("sp", lambda e, g32=g32, q0=q0: e.dma_start(out=g32[:], in_=gT32[:, q0:q0 + GQ].rearrange("(c p) t -> p c t", p=128)),
                      reads=[gT32_b], writes=[g32_b])
                for m in range(SC):
                    pg, pg_b = pg_p.get()
                    sg, sg_b = sg_p.get()
                    for c in range(SC):
                        S.op("pe", lambda e, pg=pg, gbl=gbl, c=c, m=m: e.matmul(pg[:], lhsT=wg[:, c, m * 128:(m + 1) * 128], rhs=gbl[:, c, :],
                                                                              start=(c == 0), stop=(c == SC - 1)), reads=[wg_b, gbl_b], writes=[pg_b])
                    S.op("act", lambda e, sg=sg, pg=pg, m=m: e.activation(out=sg[:], in_=pg[:], func=AF.Sigmoid, bias=bcol[:, m:m + 1]),
                         reads=[pg_b, bcol_b], writes=[sg_b])
                    S.op("dve", lambda e, sg=sg, g32=g32, m=m: e.tensor_tensor(out=sg[:], in0=sg[:], in1=g32[:, m, :], op=ALU.mult),
                         reads=[sg_b, g32_b], writes=[sg_b])
                    S.dma("sp", lambda e, sg=sg, m=m, q0=q0: e.dma_start(out=mixT[DATT + m * 128:DATT + (m + 1) * 128, q0:q0 + GQ], in_=sg[:]),
                          reads=[sg_b], writes=[mixT_b])
        if debug == "D":
            return nc

        MC = DMIX // 128
        AC = DATT // 128
        QB = min(256, OWN)
        with Phase(nc, S) as ph:
            cx = ph.cx
            ones, ones_b = cx.sb([128, 128], BF16, "ones")
            S.op("pool", lambda e: e.memset(ones[:], 1.0), writes=[ones_b])
            gcol, gcol_b = cx.sb([128, MC], F32, "gcol")
            S.dma("sp", lambda e: e.dma_start(out=gcol[:, 0:AC], in_=attn_out_norm_g.rearrange("o (c p) -> p (o c)", p=128),
                                              allow_slow_non_contiguous=True), writes=[gcol_b])
            S.dma("sp", lambda e: e.dma_start(out=gcol[:, AC:MC], in_=ssm_out_norm_g.rearrange("o (c p) -> p (o c)", p=128),
                                              allow_slow_non_contiguous=True), writes=[gcol_b])
            mx, mx_b = cx.sb([128, MC, QB], F32, "mx")
            sqb, sqb_b = cx.sb([128, MC, QB], BF16, "sqb")
            mn, mn_b = cx.sb([128, MC, QB], BF16, "mn")
            rs_a, rs_a_b = cx.sb([128, QB], F32, "rs_a")
            rs_s, rs_s_b = cx.sb([128, QB], F32, "rs_s")
            psn_p = RR([cx.ps([128, QB], F32, "psn") for _ in range(2)])
            pm_p = RR([cx.ps([128, 512], F32, "pm") for _ in range(3)])
            wo_p = RR([cx.sb([128, MC, 512], BF16, "wo") for _ in range(2)])
            xr_p = RR([cx.sb([128, 512], F32, "xr") for _ in range(3)])
            ho_p = RR([cx.sb([128, 512], F32, "ho") for _ in range(3)])
            for qb in range(OWN // QB):
                q0 = qb * QB
                S.dma("sp", lambda e, q0=q0: e.dma_start(out=mx[:], in_=mixT[:, q0:q0 + QB].rearrange("(c p) t -> p c t", p=128)),
                      reads=[mixT_b], writes=[mx_b])
                S.op("act", lambda e: e.activation(out=sqb[:], in_=mx[:], func=AF.Square), reads=[mx_b], writes=[sqb_b])
                for (lo, hi, rs, rs_b, n) in ((0, AC, rs_a, rs_a_b, DATT), (AC, MC, rs_s, rs_s_b, DSSM)):
                    psn, psn_b = psn_p.get()
                    for c in range(lo, hi):
                        S.op("pe", lambda e, psn=psn, c=c, lo=lo, hi=hi: e.matmul(psn[:], lhsT=ones[:], rhs=sqb[:, c, :],
                                                                                start=(c == lo), stop=(c == hi - 1)),
                             reads=[ones_b, sqb_b], writes=[psn_b])
                    S.op("dve", lambda e, psn=psn, rs=rs: e.tensor_copy(out=rs[:], in_=psn[:]), reads=[psn_b], writes=[rs_b])
                    ph.rstd(rs[:], rs_b, n, EPS)
                    for c in range(lo, hi):
                        S.op("dve", lambda e, c=c, rs=rs: e.scalar_tensor_tensor(out=mn[:, c, :], in0=mx[:, c, :], scalar=gcol[:, c:c + 1],
                                                                               in1=rs[:], op0=ALU.mult, op1=ALU.mult),
                             reads=[mx_b, gcol_b, rs_b], writes=[mn_b])
                for c0 in range(0, D, 512):
                    wo, wo_b = wo_p.get()
                    S.dma("sp", lambda e, wo=wo, c0=c0: e.dma_start(out=wo[:], in_=w_out_bf[:, c0:c0 + 512].rearrange("(c p) n -> p c n", p=128)),
                          reads=[w_out_bf_b], writes=[wo_b])
                    for ti in range(QB // 128):
                        t0 = q0 + ti * 128
                        pm, pm_b = pm_p.get()
                        for c in range(MC):
                            S.op("pe", lambda e, pm=pm, wo=wo, c=c, ti=ti: e.matmul(pm[:], lhsT=mn[:, c, ti * 128:(ti + 1) * 128], rhs=wo[:, c, :],
                                                                                  start=(c == 0), stop=(c == MC - 1)),
                                 reads=[mn_b, wo_b], writes=[pm_b])
                        xr, xr_b = xr_p.get()
                        ho, ho_b = ho_p.get()
                        S.dma("sp", lambda e, xr=xr, t0=t0, c0=c0: e.dma_start(out=xr[:], in_=own_x[t0:t0 + 128, c0:c0 + 512]), writes=[xr_b])
                        S.op("dve", lambda e, ho=ho, pm=pm, xr=xr: e.tensor_tensor(out=ho[:], in0=pm[:], in1=xr[:], op=ALU.add),
                             reads=[pm_b, xr_b], writes=[ho_b])
                        S.dma("sp", lambda e, ho=ho, t0=t0, c0=c0: e.dma_start(out=h1[t0:t0 + 128, c0:c0 + 512], in_=ho[:]),
                              reads=[ho_b], writes=[h1_b])
        if debug == "E":
            return nc

        BIG = float(1 << 22)
        _bc = {}

        def bchk(e):
            if "r" not in _bc:
                _bc["r"] = e.to_reg(NSLOT - 1)
            return _bc["r"]
        with Phase(nc, S) as ph:
            cx = ph.cx
            identb, identb_b = ph.identity()
            idf, idf_b = ph.identity(F32)
            g2, g2_b = ph.bcast_load(ffn_norm_g[0:1, :], D, "g2")
            br, br_b = ph.bcast_load(b_router[0:1, :], E, "br")
            wr, wr_b = cx.sb([128, KC, E], F32, "wr")
            S.dma("sp", lambda e: e.dma_start(out=wr[:], in_=w_router.rearrange("(c p) n -> p c n", p=128)), writes=[wr_b])
            ones, ones_b = cx.sb([128, 128], BF16, "ones")
            S.op("pool", lambda e: e.memset(ones[:], 1.0), writes=[ones_b])
            uf, uf_b = cx.sb([128, 128], F32, "uf")
            U, U_b = cx.sb([128, 128], BF16, "U")
            S.op("pool", lambda e: e.memset(uf[:], 1.0), writes=[uf_b])
            S.op("pool", lambda e: e.affine_select(out=uf[:], in_=uf[:], pattern=[[1, 128]], compare_op=ALU.is_gt, fill=0.0,
                                                   base=0, channel_multiplier=-1), reads=[uf_b], writes=[uf_b])
            S.op("dve", lambda e: e.tensor_copy(out=U[:], in_=uf[:]), reads=[uf_b], writes=[U_b])
            ebase_i, ebase_i_b = cx.sb([128, E], I32, "ebase_i")
            ebase, ebase_b = cx.sb([128, E], F32, "ebase")
            S.op("pool", lambda e: e.iota(out=ebase_i[:], pattern=[[CAP, E]], base=0, channel_multiplier=0), writes=[ebase_i_b])
            S.op("dve", lambda e: e.tensor_copy(out=ebase[:], in_=ebase_i[:]), reads=[ebase_i_b], writes=[ebase_b])
            cnt, cnt_b = cx.sb([128, E], F32, "cnt")
            S.op("pool", lambda e: e.memset(cnt[:], 0.0), writes=[cnt_b])
            ht_p = RR([cx.sb([128, D], F32, "ht") for _ in range(2)])
            scr, scr_b = cx.sb([128, D], BF16, "scr")
            ss, ss_b = cx.sb([128, 1], F32, "ss")
            hn32, hn32_b = cx.sb([128, D], F32, "hn32")
            hnb_p = RR([cx.sb([128, D], BF16, "hnb") for _ in range(2)])
            hT, hT_b = cx.sb([128, KC, 128], F32, "hT")
            ptf_p = RR([cx.ps([128, 4, 128], F32, "ptf") for _ in range(2)])
            plg, plg_b = cx.ps([128, E], F32, "plg")
            ppos, ppos_b = cx.ps([128, E], F32, "ppos")
            pcs, pcs_b = cx.ps([128, E], F32, "pcs")
            lg, lg_b = cx.sb([128, E], F32, "lg")
            mx8, mx8_b = cx.sb([128, 8], F32, "mx8")
            nmx, nmx_b = cx.sb([128, 1], F32, "nmx")
            mask, mask_b = cx.sb([128, E], F32, "mask")
            maskb, maskb_b = cx.sb([128, E], BF16, "maskb")
            ex, ex_b = cx.sb([128, E], F32, "ex")
            den, den_b = cx.sb([128, 1], F32, "den")
            gate, gate_b = cx.sb([128, E], F32, "gate")
            pos, pos_b = cx.sb([128, E], F32, "pos")
            ovf, ovf_b = cx.sb([128, E], F32, "ovf")
            sidx, sidx_b = cx.sb([128, E], F32, "sidx")
            oh, oh_b = cx.sb([128, E], F32, "oh")
            tmpe, tmpe_b = cx.sb([128, E], F32, "tmpe")
            idxf, idxf_b = cx.sb([128, 4], F32, "idxf")
            idxi_p = RR([cx.sb([128, 4], I32, "idxi") for _ in range(2)])
            gk_p = RR([cx.sb([128, 4], F32, "gk") for _ in range(2)])
            xdw = Buf("xdw")

            def tt(out, a, b, op, rb, wb, eng="dve"):
                S.op(eng, lambda e: e.tensor_tensor(out=out, in0=a, in1=b, op=op), reads=rb, writes=wb)

            for ti in range(OWN // 128):
                t0 = ti * 128
                ht, ht_b = ht_p.get()
                hnb, hnb_b = hnb_p.get()
                idxi, idxi_b = idxi_p.get()
                gk, gk_b = gk_p.get()
                S.dma("sp", lambda e, ht=ht, t0=t0: e.dma_start(out=ht[:], in_=h1[t0:t0 + 128, :]), reads=[h1_b], writes=[ht_b])
                ph.rmsnorm(ht[:], ht_b, D, g2[:], g2_b, hn32[:], hn32_b, scr[:], scr_b, ss[:], ss_b, EPS)
                S.op("act", lambda e, hnb=hnb: e.copy(out=hnb[:], in_=hn32[:]), reads=[hn32_b], writes=[hnb_b])
                for c0 in range(0, KC, 4):
                    ptf, ptf_b = ptf_p.get()
                    for c in range(4):
                        S.op("pe", lambda e, ptf=ptf, c=c, c0=c0: e.transpose(out=ptf[:, c, :], in_=hn32[:, (c0 + c) * 128:(c0 + c + 1) * 128],
                                                                          identity=idf[:]), reads=[hn32_b, idf_b], writes=[ptf_b])
                    S.op("act", lambda e, ptf=ptf, c0=c0: e.copy(out=hT[:, c0:c0 + 4, :], in_=ptf[:]), reads=[ptf_b], writes=[hT_b])
                for c in range(KC):
                    S.op("pe", lambda e, c=c: e.matmul(plg[:], lhsT=hT[:, c, :], rhs=wr[:, c, :], start=(c == 0), stop=(c == KC - 1)),
                         reads=[hT_b, wr_b], writes=[plg_b])
                tt(lg[:], plg[:], br[:], ALU.add, [plg_b, br_b], [lg_b])
                S.op("dve", lambda e: e.max(out=mx8[:], in_=lg[:]), reads=[lg_b], writes=[mx8_b])
                S.op("dve", lambda e: e.tensor_scalar(out=mask[:], in0=lg[:], scalar1=mx8[:, 3:4], scalar2=None, op0=ALU.is_ge),
                     reads=[lg_b, mx8_b], writes=[mask_b])
                S.op("dve", lambda e: e.tensor_copy(out=maskb[:], in_=mask[:]), reads=[mask_b], writes=[maskb_b])
                S.op("dve", lambda e: e.tensor_scalar(out=nmx[:], in0=mx8[:, 0:1], scalar1=-1.0, scalar2=None, op0=ALU.mult),
                     reads=[mx8_b], writes=[nmx_b])
                S.op("act", lambda e: e.activation(out=ex[:], in_=lg[:], func=AF.Exp, bias=nmx[:, 0:1]), reads=[lg_b, nmx_b], writes=[ex_b])
                tt(ex[:], ex[:], mask[:], ALU.mult, [ex_b, mask_b], [ex_b])
                S.op("dve", lambda e: e.reduce_sum(out=den[:], in_=ex[:], axis=AX.X), reads=[ex_b], writes=[den_b])
                S.op("dve", lambda e: e.reciprocal(out=den[:], in_=den[:]), reads=[den_b], writes=[den_b])
                S.op("dve", lambda e: e.tensor_scalar(out=gate[:], in0=ex[:], scalar1=den[:, 0:1], scalar2=None, op0=ALU.mult),
                     reads=[ex_b, den_b], writes=[gate_b])
                S.op("pe", lambda e: e.matmul(ppos[:], lhsT=U[:], rhs=maskb[:], start=True, stop=True), reads=[U_b, maskb_b], writes=[ppos_b])
                S.op("pe", lambda e: e.matmul(pcs[:], lhsT=ones[:], rhs=maskb[:], start=True, stop=True), reads=[ones_b, maskb_b], writes=[pcs_b])
                tt(pos[:], ppos[:], cnt[:], ALU.add, [ppos_b, cnt_b], [pos_b])
                tt(cnt[:], pcs[:], cnt[:], ALU.add, [pcs_b, cnt_b, pos_b], [cnt_b])
                S.op("dve", lambda e: e.tensor_scalar(out=ovf[:], in0=pos[:], scalar1=float(CAP), scalar2=BIG, op0=ALU.is_ge, op1=ALU.mult),
                     reads=[pos_b], writes=[ovf_b])
                tt(sidx[:], pos[:], ebase[:], ALU.add, [pos_b, ebase_b], [sidx_b])
                tt(sidx[:], sidx[:], ovf[:], ALU.add, [sidx_b, ovf_b], [sidx_b])
                S.op("dve", lambda e: e.tensor_scalar(out=ovf[:], in0=ovf[:], scalar1=0.0, scalar2=None, op0=ALU.is_equal), reads=[ovf_b], writes=[ovf_b])
                tt(gate[:], gate[:], ovf[:], ALU.mult, [gate_b, ovf_b], [gate_b])
                for k in range(4):
                    S.op("dve", lambda e, k=k: e.tensor_scalar(out=oh[:], in0=lg[:], scalar1=mx8[:, k:k + 1], scalar2=None, op0=ALU.is_equal),
                         reads=[lg_b, mx8_b], writes=[oh_b])
                    tt(tmpe[:], oh[:], sidx[:], ALU.mult, [oh_b, sidx_b], [tmpe_b])
                    S.op("dve", lambda e, k=k: e.reduce_sum(out=idxf[:, k:k + 1], in_=tmpe[:], axis=AX.X), reads=[tmpe_b], writes=[idxf_b])
                    tt(tmpe[:], oh[:], gate[:], ALU.mult, [oh_b, gate_b, idxf_b], [tmpe_b])
                    S.op("dve", lambda e, k=k, gk=gk: e.reduce_sum(out=gk[:, k:k + 1], in_=tmpe[:], axis=AX.X), reads=[tmpe_b], writes=[gk_b])
                S.op("dve", lambda e, idxi=idxi: e.tensor_copy(out=idxi[:], in_=idxf[:]), reads=[idxf_b], writes=[idxi_b])
                S.dma("sp", lambda e, idxi=idxi, t0=t0: e.dma_start(out=idxd[t0:t0 + 128, :], in_=idxi[:]), reads=[idxi_b], writes=[idxd_b])
                S.dma("sp", lambda e, gk=gk, t0=t0: e.dma_start(out=gated[t0:t0 + 128, :], in_=gk[:]), reads=[gk_b], writes=[gated_b])
                for k in range(4):
                    S.dma("pool", lambda e, hnb=hnb, idxi=idxi, k=k: e.indirect_dma_start(
                        out=Xd, out_offset=bass.IndirectOffsetOnAxis(ap=idxi[:, k:k + 1], axis=0), in_=hnb[:], in_offset=None,
                        bounds_check=bchk(e), oob_is_err=False), reads=[hnb_b, idxi_b], writes=[Xd_b])
                if ti == OWN // 128 - 1:
                    S.dma("sp", lambda e: e.dma_start(out=cnt_out, in_=cnt[:]), reads=[cnt_b], writes=[Buf()])
        if debug == "F1":
            return nc

        NST = CAP // 128
        FC = DE // 128
        WB = 128
        DW = min(256, D)
        with Phase(nc, S) as ph:
            cx = ph.cx
            identb, identb_b = ph.identity()
            xe, xe_b = cx.sb([128, D], BF16, "xe")
            xT, xT_b = cx.sb([128, KC, CAP], BF16, "xT")
            HW_ = 384 if CAP % 384 == 0 else CAP
            NH = CAP // HW_
            pts = [cx.ps([128, 8, 128], BF16, "ptr") for _ in range(2)]
            stg_g, stg_g_b = cx.sb([128, KC, WB], F32, "stg_g")
            stg_u, stg_u_b = cx.sb([128, KC, WB], F32, "stg_u")
            stg_d, stg_d_b = cx.sb([128, FC, DW], F32, "stg_d")
            wg_p = RR([cx.sb([128, KC, WB], BF16, "wg") for _ in range(2)])
            wu_p = RR([cx.sb([128, KC, WB], BF16, "wu") for _ in range(2)])
            wd_p = RR([cx.sb([128, FC, DW], BF16, "wd") for _ in range(2)])
            bgc, bgc_b = cx.sb([128, FC], F32, "bgc")
            buc, buc_b = cx.sb([128, FC], F32, "buc")
            bdr_p = RR([cx.sb([128, DW], F32, "bdr") for _ in range(2)])
            actT, actT_b = cx.sb([128, FC, CAP], BF16, "actT")
            pg_p = RR([cx.ps([128, HW_], F32, "pg") for _ in range(2)])
            pu_p = RR([cx.ps([128, HW_], F32, "pu") for _ in range(2)])
            py_p = RR([cx.ps([128, DW], F32, "py") for _ in range(2)])
            ac_p = RR([cx.sb([128, HW_], F32, "ac") for _ in range(2)])
            sg_p = RR([cx.sb([128, HW_], F32, "sg") for _ in range(2)])
            lc_p = RR([cx.sb([128, HW_], F32, "lc") for _ in range(2)])
            yo_p = RR([cx.sb([128, DW], BF16, "yo") for _ in range(3)])
            flip = [0]
            for ex_ in range(E):
                S.dma("sp", lambda e, ex_=ex_: e.dma_start(out=bgc[:], in_=b_gate[ex_:ex_ + 1, :].rearrange("o (c p) -> p (o c)", p=128),
                                                          allow_slow_non_contiguous=True), writes=[bgc_b])
                S.dma("sp", lambda e, ex_=ex_: e.dma_start(out=buc[:], in_=b_up[ex_:ex_ + 1, :].rearrange("o (c p) -> p (o c)", p=128),
                                                          allow_slow_non_contiguous=True), writes=[buc_b])
                for st in range(NST):
                    S.dma("sp", lambda e, ex_=ex_, st=st: e.dma_start(out=xe[:], in_=Xd[ex_ * CAP + st * 128:ex_ * CAP + (st + 1) * 128, :]),
                          reads=[Xd_b], writes=[xe_b])
                    ph.transpose_chunks(xe, xe_b, KC, xT[:, :, st * 128:(st + 1) * 128], xT_b, identb, identb_b, pts)
                for f0 in range(0, DE, WB):
                    wg, wg_b = wg_p.get()
                    wu, wu_b = wu_p.get()
                    S.dma("sp", lambda e, ex_=ex_, f0=f0: e.dma_start(
                        out=stg_g[:], in_=w_gate[ex_ * D:(ex_ + 1) * D, f0:f0 + WB].rearrange("(c p) n -> p c n", p=128)), writes=[stg_g_b])
                    S.op("act", lambda e, wg=wg: e.copy(out=wg[:], in_=stg_g[:]), reads=[stg_g_b], writes=[wg_b])
                    S.dma("sp", lambda e, ex_=ex_, f0=f0: e.dma_start(
                        out=stg_u[:], in_=w_up[ex_ * D:(ex_ + 1) * D, f0:f0 + WB].rearrange("(c p) n -> p c n", p=128)), writes=[stg_u_b])
                    S.op("pool", lambda e, wu=wu: e.tensor_copy(out=wu[:], in_=stg_u[:]), reads=[stg_u_b], writes=[wu_b])
                    for mm in range(WB // 128):
                      m = f0 // 128 + mm
                      for hh in range(NH):
                        h0 = hh * HW_
                        pg, pg_b = pg_p.get()
                        pu, pu_b = pu_p.get()
                        for c in range(KC):
                            S.op("pe", lambda e, pg=pg, wg=wg, c=c, mm=mm, h0=h0: e.matmul(pg[:], lhsT=wg[:, c, mm * 128:(mm + 1) * 128], rhs=xT[:, c, h0:h0 + HW_],
                                                                                 start=(c == 0), stop=(c == KC - 1)), reads=[wg_b, xT_b], writes=[pg_b])
                        for c in range(KC):
                            S.op("pe", lambda e, pu=pu, wu=wu, c=c, mm=mm, h0=h0: e.matmul(pu[:], lhsT=wu[:, c, mm * 128:(mm + 1) * 128], rhs=xT[:, c, h0:h0 + HW_],
                                                                                 start=(c == 0), stop=(c == KC - 1)), reads=[wu_b, xT_b], writes=[pu_b])
                        ac, ac_b = ac_p.get()
                        sg, sg_b = sg_p.get()
                        lc, lc_b = lc_p.get()
                        S.op("dve", lambda e, ac=ac, pg=pg, m=m: e.tensor_scalar(out=ac[:], in0=pg[:], scalar1=bgc[:, m:m + 1], scalar2=cfg["LIMIT"],
                                                                                op0=ALU.add, op1=ALU.min), reads=[pg_b, bgc_b], writes=[ac_b])
                        S.op("act", lambda e, sg=sg, ac=ac: e.activation(out=sg[:], in_=ac[:], func=AF.Sigmoid, scale=cfg["ALPHA"]), reads=[ac_b], writes=[sg_b])
                        S.op("dve", lambda e, lc=lc, pu=pu, m=m: e.tensor_scalar(out=lc[:], in0=pu[:], scalar1=buc[:, m:m + 1], scalar2=cfg["LIMIT"],
                                                                                op0=ALU.add, op1=ALU.min), reads=[pu_b, buc_b], writes=[lc_b])
                        S.op("dve", lambda e, lc=lc: e.tensor_scalar(out=lc[:], in0=lc[:], scalar1=-cfg["LIMIT"], scalar2=1.0, op0=ALU.max, op1=ALU.add),
                             reads=[lc_b], writes=[lc_b])
                        S.op("dve", lambda e, sg=sg, ac=ac: e.tensor_tensor(out=sg[:], in0=sg[:], in1=ac[:], op=ALU.mult), reads=[sg_b, ac_b], writes=[sg_b])
                        S.op("dve", lambda e, sg=sg, lc=lc, m=m, h0=h0: e.tensor_tensor(out=actT[:, m, h0:h0 + HW_], in0=sg[:], in1=lc[:], op=ALU.mult),
                             reads=[sg_b, lc_b], writes=[actT_b])
                for d0 in range(0, D, DW):
                    wd, wd_b = wd_p.get()
                    S.dma("sp", lambda e, ex_=ex_, d0=d0: e.dma_start(
                        out=stg_d[:], in_=w_down[ex_ * DE:(ex_ + 1) * DE, d0:d0 + DW].rearrange("(c p) n -> p c n", p=128)), writes=[stg_d_b])
                    flip[0] ^= 1
                    if flip[0]:
                        S.op("act", lambda e, wd=wd: e.copy(out=wd[:], in_=stg_d[:]), reads=[stg_d_b], writes=[wd_b])
                    else:
                        S.op("pool", lambda e, wd=wd: e.tensor_copy(out=wd[:], in_=stg_d[:]), reads=[stg_d_b], writes=[wd_b])
                    bdr, bdr_b = bdr_p.get()
                    S.dma("sp", lambda e, bdr=bdr, ex_=ex_, d0=d0: e.dma_start(out=bdr[:], in_=b_down[ex_:ex_ + 1, d0:d0 + DW].to_broadcast([128, DW])),
                          writes=[bdr_b])
                    for st in range(NST):
                        py, py_b = py_p.get()
                        for fc in range(FC):
                            S.op("pe", lambda e, py=py, wd=wd, fc=fc, st=st: e.matmul(py[:], lhsT=actT[:, fc, st * 128:(st + 1) * 128], rhs=wd[:, fc, :],
                                                                                    start=(fc == 0), stop=(fc == FC - 1)), reads=[actT_b, wd_b], writes=[py_b])
                        yo, yo_b = yo_p.get()
                        S.op("dve", lambda e, yo=yo, py=py, bdr=bdr: e.tensor_tensor(out=yo[:], in0=py[:], in1=bdr[:], op=ALU.add),
                             reads=[py_b, bdr_b], writes=[yo_b])
                        S.dma("sp", lambda e, yo=yo, ex_=ex_, st=st, d0=d0: e.dma_start(
                            out=Yd[ex_ * CAP + st * 128:ex_ * CAP + (st + 1) * 128, d0:d0 + DW], in_=yo[:]), reads=[yo_b], writes=[Yd_b])

        with Phase(nc, S) as ph:
            cx = ph.cx
            acc_p = RR([cx.sb([128, D], F32, "acc") for _ in range(2)])
            yk_p = RR([cx.sb([128, D], BF16, "yk") for _ in range(3)])
            ii_p = RR([cx.sb([128, 4], I32, "ii") for _ in range(2)])
            gg_p = RR([cx.sb([128, 4], F32, "gg") for _ in range(2)])
            outb = Buf("out")
            for ti in range(OWN // 128):
                t0 = ti * 128
                acc, acc_b = acc_p.get()
                ii, ii_b = ii_p.get()
                gg, gg_b = gg_p.get()
                S.dma("sp", lambda e, acc=acc, t0=t0: e.dma_start(out=acc[:], in_=h1[t0:t0 + 128, :]), reads=[h1_b], writes=[acc_b])
                S.dma("sp", lambda e, ii=ii, t0=t0: e.dma_start(out=ii[:], in_=idxd[t0:t0 + 128, :]), reads=[idxd_b], writes=[ii_b])
                S.dma("sp", lambda e, gg=gg, t0=t0: e.dma_start(out=gg[:], in_=gated[t0:t0 + 128, :]), reads=[gated_b], writes=[gg_b])
                for k in range(4):
                    yk, yk_b = yk_p.get()
                    S.op("pool", lambda e, yk=yk: e.memset(yk[:], 0.0), writes=[yk_b])
                    S.dma("pool", lambda e, yk=yk, ii=ii, k=k: e.indirect_dma_start(
                        out=yk[:], out_offset=None, in_=Yd, in_offset=bass.IndirectOffsetOnAxis(ap=ii[:, k:k + 1], axis=0),
                        bounds_check=bchk(e), oob_is_err=False), reads=[Yd_b, ii_b], writes=[yk_b])
                    S.op("dve", lambda e, acc=acc, yk=yk, gg=gg, k=k: e.scalar_tensor_tensor(out=acc[:], in0=yk[:], scalar=gg[:, k:k + 1], in1=acc[:],
                                                                                         op0=ALU.mult, op1=ALU.add), reads=[yk_b, gg_b, acc_b], writes=[acc_b])
                S.dma("sp", lambda e, acc=acc, t0=t0: e.dma_start(out=out[t0:t0 + 128, :], in_=acc[:]), reads=[acc_b], writes=[outb])
    return nc


def make_in_maps(cfg, inp):
    SEQ, D = cfg["SEQ"], cfg["D"]
    OWN = SEQ // 2
    HALF = cfg["ROPE"] // 2
    invf = (cfg["THETA"] ** (-np.arange(HALF, dtype=np.float32) / HALF)).astype(np.float32).reshape(1, HALF)
    maps = []
    f = lambda a: np.ascontiguousarray(a)
    for core in range(N_CORES):
        b, half = core // 2, core % 2
        sl = slice(half * OWN, (half + 1) * OWN)
        m = {
            "x": f(inp["x"][b]),
            "pos": f(inp["positions"][b].reshape(SEQ, 1).astype(np.int32)),
            "half_sel": np.full((1, 1), half, np.int32),
            "invf": invf,
            "own_x": f(inp["x"][b, sl]),
            "own_pos": f(inp["positions"][b, sl].reshape(OWN, 1).astype(np.int32)),
        }
        m["halff"] = np.full((1, 1), float(half), np.float32)
        E, EL = cfg["E"], cfg["E"] // N_CORES
        m["w_gate"] = inp["w_gate"][0].reshape(E * D, cfg["DE"])
        m["w_up"] = inp["w_up"][0].reshape(E * D, cfg["DE"])
        m["w_down"] = inp["w_down"][0].reshape(E * cfg["DE"], D)
        m["b_gate"] = f(inp["b_gate"][0])
        m["b_up"] = f(inp["b_up"][0])
        m["b_down"] = f(inp["b_down"][0])
        m["ffn_norm_g"] = f(inp["ffn_norm_g"][0].reshape(1, -1))
        m["w_router"] = f(inp["w_router"][0])
        m["b_router"] = f(inp["b_router"][0].reshape(1, -1))
        for k in ("ssm_a_re", "ssm_a_im", "ssm_b_re", "ssm_b_im", "ssm_c_re", "ssm_c_im", "ssm_w_glu"):
            m[k] = f(inp[k][0])
        m["ssm_log_step"] = f(inp["ssm_log_step"][0].reshape(1, -1))
        m["ssm_d"] = f(inp["ssm_d"][0].reshape(1, -1))
        m["ssm_b_glu"] = f(inp["ssm_b_glu"][0].reshape(1, -1))
        for k in ("attn_norm_g", "w_in", "q_lora_norm_g", "w_q_up", "kv_lora_norm_g", "w_kv_up", "q_head_norm_g",
                  "k_head_norm_g", "attn_out_norm_g", "ssm_out_norm_g", "w_out"):
            a = inp[k][0]
            m[k] = f(a.reshape(1, -1) if a.ndim == 1 else a)
        maps.append(m)
    return maps


_NC_CACHE = {}


def kernel(**inputs):
    cfg = FULL_CFG
    inp = {k: np.asarray(v) for k, v in inputs.items()}
    if "nc" not in _NC_CACHE:
        _NC_CACHE["nc"] = build(cfg)
    nc = _NC_CACHE["nc"]
    maps = make_in_maps(cfg, inp)
    res = run_bass_kernel_spmd(nc, maps, core_ids=list(range(N_CORES)))
    B, SEQ, D = cfg["B"], cfg["SEQ"], cfg["D"]
    OWN = SEQ // 2
    try:
        print("MOE max routed tokens per (core, expert):", [int(r["cnt_out"][0].max()) for r in res.results], "capacity", cfg["CAP"], flush=True)
    except Exception:
        pass
    outp = np.empty((B, SEQ, D), np.float32)
    for core in range(N_CORES):
        b, half = core // 2, core % 2
        outp[b, half * OWN:(half + 1) * OWN] = res.results[core]["out"]
    return outp
```
